# Optimizing a Trainium2 kernel written in Bass

```python
import math
import jax, jax.numpy as jnp
from jax import lax
import numpy as np

D_MODEL = 1024
BATCH = 8
SEQ = 4096
DEPTH = 2

D_CONV = D_MODEL // 2
CONV_WIDTH = 3
D_SSM = D_MODEL // 2
SSM_GROUP_DIM = 16
SSM_GROUPS = D_SSM // SSM_GROUP_DIM
SSM_STATE = 64
SSM_DT_MIN = 1e-3
SSM_DT_MAX = 1e-1
MLA_HEADS = 8
QK_NOPE = 64
QK_ROPE = 32
V_HEAD = 64
Q_LORA = D_MODEL // 4
KV_LORA = D_MODEL // 8
D_ATTN = MLA_HEADS * V_HEAD
ROPE_THETA = 10000.0
Q_BLOCK = 128
N_BRANCHES = 3
IN_MIX = 3 * D_CONV + D_SSM + Q_LORA + KV_LORA + QK_ROPE
N_IN = IN_MIX + N_BRANCHES * D_MODEL
IN_SPLITS = (
    D_CONV,
    2 * D_CONV,
    3 * D_CONV,
    3 * D_CONV + D_SSM,
    3 * D_CONV + D_SSM + Q_LORA,
    3 * D_CONV + D_SSM + Q_LORA + KV_LORA,
    IN_MIX,
    IN_MIX + D_MODEL,
    IN_MIX + 2 * D_MODEL,
)
N_EXPERTS = 32
N_EXPERT_GROUPS = 8
EXPERTS_PER_GROUP = N_EXPERTS // N_EXPERT_GROUPS
TOPK_GROUPS = 1
TOP_K = 2
D_EXPERT = D_MODEL // 4
EXPERT_BLOCK = 128
DEEPNORM_ALPHA = (2 * DEPTH) ** 0.25
DEEPNORM_BETA = (8 * DEPTH) ** -0.25
LN_EPS = 1e-5
RMS_EPS = 1e-6

kernel_name = "hybrid_conv_s5_mla_grouped_moe_deepnorm_adaln"


def layer_norm_plain(x):
    xf = x.astype(jnp.float32)
    mu = jnp.mean(xf, axis=-1, keepdims=True)
    var = jnp.mean(jnp.square(xf - mu), axis=-1, keepdims=True)
    return ((xf - mu) * lax.rsqrt(var + LN_EPS)).astype(x.dtype)


def layer_norm(x, g, b):
    return (layer_norm_plain(x) * g + b).astype(x.dtype)


def rms_norm(x, g):
    xf = x.astype(jnp.float32)
    y = xf * lax.rsqrt(jnp.mean(jnp.square(xf), axis=-1, keepdims=True) + RMS_EPS)
    return (y * g).astype(x.dtype)


def modulate(x, shift, scale):
    return layer_norm_plain(x) * (1.0 + scale[:, None, :]) + shift[:, None, :]


def rope_tables(positions):
    inv_freq = ROPE_THETA ** (-jnp.arange(0, QK_ROPE, 2, dtype=jnp.float32) / QK_ROPE)
    ang = positions.astype(jnp.float32)[..., None] * inv_freq
    return jnp.cos(ang), jnp.sin(ang)


def apply_rope(x, cos, sin):
    x1, x2 = jnp.split(x, 2, axis=-1)
    cos = cos.astype(x.dtype)
    sin = sin.astype(x.dtype)
    return jnp.concatenate([x1 * cos - x2 * sin, x1 * sin + x2 * cos], axis=-1)


def short_conv_branch(xc, gc, gb, conv_w):
    u = gc * xc
    out = lax.conv_general_dilated(
        u, conv_w[:, None, :], window_strides=(1,),
        padding=[(CONV_WIDTH - 1, 0)],
        dimension_numbers=("NWC", "WIO", "NWC"),
        feature_group_count=D_CONV)
    return gb * out


def s5_branch(u, a_re, a_im, b_re, b_im, c_re, c_im, d_skip, log_dt, w_glu):
    Bsz, S, _ = u.shape
    f32 = jnp.float32
    uf = u.astype(f32).reshape(Bsz, S, SSM_GROUPS, SSM_GROUP_DIM)
    ar = a_re.astype(f32)
    ai = a_im.astype(f32)
    dt = jnp.exp(log_dt.astype(f32))[:, None]
    mag = jnp.exp(ar * dt)
    lb_re = mag * jnp.cos(ai * dt)
    lb_im = mag * jnp.sin(ai * dt)
    den = ar * ar + ai * ai
    nr = lb_re - 1.0
    ni = lb_im
    f_re = (nr * ar + ni * ai) / den
    f_im = (ni * ar - nr * ai) / den
    br = b_re.astype(f32)
    bi = b_im.astype(f32)
    bb_re = f_re[..., None] * br - f_im[..., None] * bi
    bb_im = f_re[..., None] * bi + f_im[..., None] * br
    bu_re = jnp.einsum("bsgp,gnp->bsgn", uf, bb_re)
    bu_im = jnp.einsum("bsgp,gnp->bsgn", uf, bb_im)
    la_re = jnp.broadcast_to(lb_re[None, None], (1, S, SSM_GROUPS, SSM_STATE))
    la_im = jnp.broadcast_to(lb_im[None, None], (1, S, SSM_GROUPS, SSM_STATE))

    def combine(e_i, e_j):
        ar_i, ai_i, br_i, bi_i = e_i
        ar_j, ai_j, br_j, bi_j = e_j
        return (ar_j * ar_i - ai_j * ai_i,
                ar_j * ai_i + ai_j * ar_i,
                ar_j * br_i - ai_j * bi_i + br_j,
                ar_j * bi_i + ai_j * br_i + bi_j)

    _, _, s_re, s_im = lax.associative_scan(combine, (la_re, la_im, bu_re, bu_im), axis=1)
    y = (jnp.einsum("bsgn,gpn->bsgp", s_re, c_re.astype(f32))
         - jnp.einsum("bsgn,gpn->bsgp", s_im, c_im.astype(f32))
         + d_skip.astype(f32).reshape(SSM_GROUPS, SSM_GROUP_DIM) * uf)
    y = jax.nn.gelu(y.reshape(Bsz, S, D_SSM))
    val, gate = jnp.split(y @ w_glu.astype(f32), 2, axis=-1)
    return (val * jax.nn.sigmoid(gate)).astype(u.dtype)


def causal_block_attention(q, k, v):
    Bsz, S, H, Dk = q.shape
    nb = S // Q_BLOCK
    qb = q.reshape(Bsz, nb, Q_BLOCK, H, Dk).transpose(1, 0, 2, 3, 4)
    key_idx = jnp.arange(S, dtype=jnp.int32)
    scale = Dk ** -0.5

    def one_block(args):
        qi, bi = args
        s = jnp.einsum("bqhd,bkhd->bhqk", qi, k, preferred_element_type=jnp.float32) * scale
        q_idx = bi * Q_BLOCK + jnp.arange(Q_BLOCK, dtype=jnp.int32)
        mask = key_idx[None, :] <= q_idx[:, None]
        s = jnp.where(mask[None, None], s, -jnp.inf)
        p = jax.nn.softmax(s, axis=-1).astype(v.dtype)
        return jnp.einsum("bhqk,bkhd->bqhd", p, v)

    o = lax.map(one_block, (qb, jnp.arange(nb, dtype=jnp.int32)))
    return o.transpose(1, 0, 2, 3, 4).reshape(Bsz, S, H * v.shape[-1])


def mla_branch(c_q, c_kv, k_r, cos, sin, q_norm, w_uq, kv_norm, w_uk, w_uv):
    Bsz, S, _ = c_q.shape
    cq = rms_norm(c_q, q_norm)
    ckv = rms_norm(c_kv, kv_norm)
    q = (cq @ w_uq).reshape(Bsz, S, MLA_HEADS, QK_NOPE + QK_ROPE)
    q_rope = apply_rope(q[..., QK_NOPE:], cos[:, :, None, :], sin[:, :, None, :])
    q = jnp.concatenate([q[..., :QK_NOPE], q_rope], axis=-1)
    k_rope = apply_rope(k_r, cos, sin)
    k_nope = (ckv @ w_uk).reshape(Bsz, S, MLA_HEADS, QK_NOPE)
    v = (ckv @ w_uv).reshape(Bsz, S, MLA_HEADS, V_HEAD)
    k = jnp.concatenate(
        [k_nope, jnp.broadcast_to(k_rope[:, :, None, :], (Bsz, S, MLA_HEADS, QK_ROPE))], axis=-1)
    return causal_block_attention(q, k, v)


def hybrid_mixer(h, cos, sin, w_in, conv_w, a_re, a_im, b_re, b_im, c_re, c_im, d_skip,
                 log_dt, w_glu, q_norm, w_uq, kv_norm, w_uk, w_uv,
                 w_up_conv, w_up_ssm, w_up_attn, w_o):
    proj = h @ w_in
    (xc, gc, gb, u_ssm, c_q, c_kv, k_r,
     g_conv, g_ssm, g_attn) = jnp.split(proj, IN_SPLITS, axis=-1)
    y_conv = short_conv_branch(xc, gc, gb, conv_w) @ w_up_conv
    y_ssm = s5_branch(u_ssm, a_re, a_im, b_re, b_im, c_re, c_im, d_skip, log_dt, w_glu) @ w_up_ssm
    y_attn = mla_branch(c_q, c_kv, k_r, cos, sin, q_norm, w_uq, kv_norm, w_uk, w_uv) @ w_up_attn
    merged = (jax.nn.sigmoid(g_conv) * y_conv
              + jax.nn.sigmoid(g_ssm) * y_ssm
              + jax.nn.sigmoid(g_attn) * y_attn)
    return merged @ w_o


def swiglu(x, w1, w3, w2):
    return (jax.nn.silu(x @ w1) * (x @ w3)) @ w2


def grouped_top2_route(h2d, router_w, router_bias):
    T = h2d.shape[0]
    s = jax.nn.sigmoid(h2d.astype(jnp.float32) @ router_w.astype(jnp.float32))
    sel = s + router_bias.astype(jnp.float32)
    grp = sel.reshape(T, N_EXPERT_GROUPS, EXPERTS_PER_GROUP)
    grp_score = lax.top_k(grp, 2)[0].sum(axis=-1)
    _, g_idx = lax.top_k(grp_score, TOPK_GROUPS)
    g_mask = jnp.any(g_idx[:, :, None] == jnp.arange(N_EXPERT_GROUPS)[None, None, :], axis=1)
    e_mask = jnp.repeat(g_mask, EXPERTS_PER_GROUP, axis=1)
    _, e_idx = lax.top_k(jnp.where(e_mask, sel, -jnp.inf), TOP_K)
    w = jnp.take_along_axis(s, e_idx, axis=1)
    w = w / jnp.sum(w, axis=-1, keepdims=True)
    return e_idx, w


def routed_experts(h2d, e_idx, e_w, w1, w3, w2):
    T, D = h2d.shape
    M = T * TOP_K
    flat_e = e_idx.reshape(M)
    flat_tok = jnp.arange(M, dtype=jnp.int32) // TOP_K
    flat_w = e_w.reshape(M)
    order = jnp.argsort(flat_e)
    e_sorted = flat_e[order]
    tok_sorted = flat_tok[order]
    w_sorted = flat_w[order]
    counts = jnp.bincount(flat_e, length=N_EXPERTS)
    padded = (counts + EXPERT_BLOCK - 1) // EXPERT_BLOCK * EXPERT_BLOCK
    pad_end = jnp.cumsum(padded)
    pad_start = pad_end - padded
    start = jnp.cumsum(counts) - counts
    dest = pad_start[e_sorted] + (jnp.arange(M, dtype=jnp.int32) - start[e_sorted])
    n_blocks = (M + EXPERT_BLOCK - 1) // EXPERT_BLOCK + N_EXPERTS
    n_rows = n_blocks * EXPERT_BLOCK
    row_tok = jnp.zeros((n_rows,), jnp.int32).at[dest].set(tok_sorted)
    row_w = jnp.zeros((n_rows,), h2d.dtype).at[dest].set(w_sorted)
    block_starts = jnp.arange(n_blocks, dtype=pad_end.dtype) * EXPERT_BLOCK
    block_e = jnp.minimum(jnp.searchsorted(pad_end, block_starts, side="right"), N_EXPERTS - 1)
    x_rows = h2d[row_tok].reshape(n_blocks, EXPERT_BLOCK, D)

    def one_block(args):
        xb, e = args
        return swiglu(xb, w1[e], w3[e], w2[e])

    y_rows = lax.map(one_block, (x_rows, block_e)).reshape(n_rows, D)
    return jax.ops.segment_sum(y_rows * row_w[:, None], row_tok, num_segments=T)


def moe_ffn(h, router_w, router_bias, w1, w3, w2, ws1, ws3, ws2):
    Bsz, S, D = h.shape
    h2d = h.reshape(Bsz * S, D)
    e_idx, e_w = grouped_top2_route(h2d, router_w, router_bias)
    y = routed_experts(h2d, e_idx, e_w.astype(h.dtype), w1, w3, w2) + swiglu(h2d, ws1, ws3, ws2)
    return y.reshape(Bsz, S, D)


def setup_inputs(seed: int = 0) -> dict:
    key = jax.random.key(seed)
    ks = iter(jax.random.split(key, 64))
    L = DEPTH
    beta = DEEPNORM_BETA

    def nrm(shape, scale):
        return scale * jax.random.normal(next(ks), shape, jnp.float32)

    x = nrm((BATCH, SEQ, D_MODEL), 1.0)
    c = nrm((BATCH, D_MODEL), 1.0)
    offset = jax.random.randint(next(ks), (BATCH, 1), 0, 2048, dtype=jnp.int32)
    positions = offset + jnp.arange(SEQ, dtype=jnp.int32)[None, :]
    n_idx = jnp.arange(SSM_STATE, dtype=jnp.float32)
    return {
        "x": x,
        "c": c,
        "positions": positions,
        "w_ada": nrm((L, D_MODEL, 6 * D_MODEL), 0.5 * D_MODEL ** -0.5),
        "b_ada": nrm((L, 6 * D_MODEL), 0.01),
        "w_in": nrm((L, D_MODEL, N_IN), D_MODEL ** -0.5),
        "conv_w": nrm((L, CONV_WIDTH, D_CONV), CONV_WIDTH ** -0.5),
        "ssm_a_re": -0.5 + nrm((L, SSM_GROUPS, SSM_STATE), 0.01),
        "ssm_a_im": math.pi * n_idx + nrm((L, SSM_GROUPS, SSM_STATE), 0.01),
        "ssm_b_re": nrm((L, SSM_GROUPS, SSM_STATE, SSM_GROUP_DIM), (2 * SSM_GROUP_DIM) ** -0.5),
        "ssm_b_im": nrm((L, SSM_GROUPS, SSM_STATE, SSM_GROUP_DIM), (2 * SSM_GROUP_DIM) ** -0.5),
        "ssm_c_re": nrm((L, SSM_GROUPS, SSM_GROUP_DIM, SSM_STATE), (2 * SSM_STATE) ** -0.5),
        "ssm_c_im": nrm((L, SSM_GROUPS, SSM_GROUP_DIM, SSM_STATE), (2 * SSM_STATE) ** -0.5),
        "ssm_d": nrm((L, D_SSM), 1.0),
        "ssm_log_dt": jax.random.uniform(next(ks), (L, SSM_GROUPS), jnp.float32,
                                         math.log(SSM_DT_MIN), math.log(SSM_DT_MAX)),
        "ssm_w_glu": nrm((L, D_SSM, 2 * D_SSM), D_SSM ** -0.5),
        "q_norm": 1.0 + nrm((L, Q_LORA), 0.01),
        "w_uq": nrm((L, Q_LORA, MLA_HEADS * (QK_NOPE + QK_ROPE)), Q_LORA ** -0.5),
        "kv_norm": 1.0 + nrm((L, KV_LORA), 0.01),
        "w_uk": nrm((L, KV_LORA, MLA_HEADS * QK_NOPE), KV_LORA ** -0.5),
        "w_uv": nrm((L, KV_LORA, MLA_HEADS * V_HEAD), beta * KV_LORA ** -0.5),
        "w_up_conv": nrm((L, D_CONV, D_MODEL), beta * D_CONV ** -0.5),
        "w_up_ssm": nrm((L, D_SSM, D_MODEL), beta * D_SSM ** -0.5),
        "w_up_attn": nrm((L, D_ATTN, D_MODEL), beta * D_ATTN ** -0.5),
        "w_o": nrm((L, D_MODEL, D_MODEL), beta * D_MODEL ** -0.5),
        "ln_g": 1.0 + nrm((L, 2, D_MODEL), 0.01),
        "ln_b": nrm((L, 2, D_MODEL), 0.01),
        "router_w": nrm((D_MODEL, N_EXPERTS), D_MODEL ** -0.5),
        "router_bias": nrm((N_EXPERTS,), 0.01),
        "exp_w1": nrm((L, N_EXPERTS, D_MODEL, D_EXPERT), D_MODEL ** -0.5),
        "exp_w3": nrm((L, N_EXPERTS, D_MODEL, D_EXPERT), D_MODEL ** -0.5),
        "exp_w2": nrm((L, N_EXPERTS, D_EXPERT, D_MODEL), beta * D_EXPERT ** -0.5),
        "shared_w1": nrm((L, D_MODEL, D_EXPERT), D_MODEL ** -0.5),
        "shared_w3": nrm((L, D_MODEL, D_EXPERT), D_MODEL ** -0.5),
        "shared_w2": nrm((L, D_EXPERT, D_MODEL), beta * D_EXPERT ** -0.5),
    }


def reference(x, c, positions, w_ada, b_ada, w_in, conv_w, ssm_a_re, ssm_a_im, ssm_b_re,
              ssm_b_im, ssm_c_re, ssm_c_im, ssm_d, ssm_log_dt, ssm_w_glu, q_norm, w_uq,
              kv_norm, w_uk, w_uv, w_up_conv, w_up_ssm, w_up_attn, w_o, ln_g, ln_b,
              router_w, router_bias, exp_w1, exp_w3, exp_w2, shared_w1, shared_w3, shared_w2):
    cos, sin = rope_tables(positions)
    cond = jax.nn.silu(c)
    for l in range(DEPTH):
        ada = cond @ w_ada[l] + b_ada[l]
        sh1, sc1, g1, sh2, sc2, g2 = jnp.split(ada, 6, axis=-1)
        h = modulate(x, sh1, sc1)
        y = hybrid_mixer(h, cos, sin, w_in[l], conv_w[l], ssm_a_re[l], ssm_a_im[l],
                         ssm_b_re[l], ssm_b_im[l], ssm_c_re[l], ssm_c_im[l], ssm_d[l],
                         ssm_log_dt[l], ssm_w_glu[l], q_norm[l], w_uq[l], kv_norm[l],
                         w_uk[l], w_uv[l], w_up_conv[l], w_up_ssm[l], w_up_attn[l], w_o[l])
        x = layer_norm(DEEPNORM_ALPHA * x + g1[:, None, :] * y, ln_g[l, 0], ln_b[l, 0])
        h = modulate(x, sh2, sc2)
        y = moe_ffn(h, router_w, router_bias, exp_w1[l], exp_w3[l], exp_w2[l],
                    shared_w1[l], shared_w3[l], shared_w2[l])
        x = layer_norm(DEEPNORM_ALPHA * x + g2[:, None, :] * y, ln_g[l, 1], ln_b[l, 1])
    return x
```

```python
import contextlib
import math
import numpy as np
import ml_dtypes
import concourse.bass as bass
import concourse.mybir as mybir
from concourse.bass_utils import run_bass_kernel_spmd

F32 = mybir.dt.float32
BF16 = mybir.dt.bfloat16
I32 = mybir.dt.int32
AF = mybir.ActivationFunctionType
ALU = mybir.AluOpType
AX = mybir.AxisListType

D = 1024
DEPTH = 2
DC = 512
G = 32
NST = 64
GD = 16
H = 8
NOPE = 64
ROPE = 32
DK = 96
DV = 64
QL = 256
KVL = 128
IN_MIX = 3 * DC + DC + QL + KVL + ROPE
NA = IN_MIX + ROPE
NE = 32
DE = 256
ALPHA = (2 * DEPTH) ** 0.25
LN_EPS = 1e-5
RMS_EPS = 1e-6
TWO_PI = 2.0 * math.pi

SEM_ROT = 12000
NDS = 40
SAME_ENG_SYNC = True
KSIM = False


class Reg:
    __slots__ = ("w", "r")

    def __init__(self):
        self.w = None
        self.r = {}


class Eng:
    def __init__(self, fw, name, h):
        self.name = name
        self.h = h
        self.sem = fw.new_sem()
        self.own = {id(self.sem)}
        self.cnt = 0
        self.seen = {}


class FW:
    def __init__(self, nc, es):
        self.nc = nc
        self.es = es
        self.nsem = 0
        self.E = {n: Eng(self, n, getattr(nc, n)) for n in ("tensor", "vector", "scalar", "gpsimd", "sync")}
        self.dsems = [[self.new_sem(), 0] for _ in range(NDS)]
        self.dsi = 0
        self.ntile = 0

    def new_sem(self):
        s = self.es.enter_context(self.nc.semaphore(f"sm{self.nsem}"))
        self.nsem += 1
        return s

    def sb(self, shape, dt, name=None):
        self.ntile += 1
        return self.es.enter_context(self.nc.sbuf_tensor(f"{name or 't'}{self.ntile}", list(shape), dt))

    def ps(self, es, name, shape, dt):
        self.ntile += 1
        return es.enter_context(self.nc.psum_tensor(f"{name}_{self.ntile}", list(shape), dt))

    @staticmethod
    def _flat(regs):
        out = []
        for t in regs:
            if isinstance(t, RegList):
                out.extend(t)
            else:
                out.append(t)
        return out

    def _deps(self, reads, writes):
        toks = []
        reads = self._flat(reads)
        writes = self._flat(writes)
        for t in reads:
            if t.w is not None:
                toks.append(t.w)
        for t in writes:
            if t.w is not None:
                toks.append(t.w)
            toks.extend(t.r.values())
        return toks

    def _wait(self, E, toks):
        need = {}
        for (s, v) in toks:
            k = id(s)
            if k not in need or need[k][1] < v:
                need[k] = (s, v)
        for k, (s, v) in need.items():
            if k in E.own and (E.name == "tensor" or not SAME_ENG_SYNC):
                continue
            if E.seen.get(k, 0) < v:
                E.h.wait_ge(s, v)
                E.seen[k] = v

    def op(self, eng, build, reads=(), writes=()):
        if self.cut():
            return None
        E = self.E[eng]
        self._wait(E, self._deps(reads, writes))
        if E.cnt >= SEM_ROT:
            E.sem = self.new_sem()
            E.own.add(id(E.sem))
            E.cnt = 0
        inst = build(E.h)
        E.cnt += 1
        inst.then_inc(E.sem, 1)
        tok = (E.sem, E.cnt)
        k = id(E.sem)
        for t in self._flat(reads):
            t.r[k] = tok
        for t in self._flat(writes):
            t.w = tok
            t.r = {}
        return inst

    def cut(self):
        import os
        kc = os.environ.get("KCUT")
        if kc is None or not getattr(self, "cut_on", False):
            return False
        self.cut_n = getattr(self, "cut_n", 0) + 1
        return self.cut_n > int(kc)

    def dma(self, q, out, in_, reads=(), writes=(), **kw):
        if self.cut():
            return None
        E = self.E[q]
        if KSIM and q == "gpsimd":
            ds = [self.new_sem(), 0]
            self.dsems.append(ds)
        else:
            ds = self.dsems[self.dsi]
            self.dsi = (self.dsi + 1) % NDS
        toks = self._deps(reads, writes)
        if ds[1] > 0:
            toks.append((ds[0], ds[1]))
        self._wait(E, toks)
        if ds[1] >= SEM_ROT:
            ds[0] = self.new_sem()
            ds[1] = 0
        ds[1] += 16
        E.h.dma_start(out=out, in_=in_, **kw).then_inc(ds[0], 16)
        tok = (ds[0], ds[1])
        k = id(ds[0])
        for t in self._flat(reads):
            t.r[k] = tok
        for t in self._flat(writes):
            t.w = tok
            t.r = {}

    def barrier(self):
        toks = [(E.sem, E.cnt) for E in self.E.values() if E.cnt > 0]
        toks += [(d[0], d[1]) for d in self.dsems if d[1] > 0]
        for E in self.E.values():
            need = [(s_, v) for (s_, v) in toks if id(s_) not in E.own]
            self._wait(E, need)

    def finish(self, regs=None):
        E = self.E["sync"]
        toks = [(e.sem, e.cnt) for e in self.E.values() if e.cnt > 0 and e is not E]
        toks += [(d[0], d[1]) for d in self.dsems if d[1] > 0]
        self._wait(E, toks)


class RegList(list):
    pass


class Rot:
    def __init__(self, tiles):
        self.t = [(a, Reg()) for a in tiles]
        self.i = 0

    def next(self):
        r = self.t[self.i]
        self.i = (self.i + 1) % len(self.t)
        return r


def build_program(S, L=DEPTH, stop=None, dbg=None):
    nc = bass.Bass("TRN2", target_bir_lowering=False)
    NTB = S // 128
    NTG = S // 512
    dbg = dbg or {}

    def din(name, shape, dt=F32):
        return nc.dram_tensor(name, list(shape), dt, kind="ExternalInput").ap()

    def dscr(name, shape, dt):
        return nc.dram_tensor(name, list(shape), dt, kind="Internal").ap()

    x_in = din("x", [S, D])
    c_in = din("c", [1, D])
    pos_in = din("positions", [1, S], I32)
    w_ada = din("w_ada", [L, D, 6 * D])
    b_ada = din("b_ada", [L, 6 * D])
    w_in_a = din("w_in_a", [L, D, NA])
    w_in_g = din("w_in_g", [L, D, 3 * D])
    conv_w = din("conv_w", [L, 3, DC])
    ssm_a_re = din("ssm_a_re", [L, G, NST])
    ssm_a_im = din("ssm_a_im", [L, G, NST])
    ssm_b_re = din("ssm_b_re", [L, G, NST, GD])
    ssm_b_im = din("ssm_b_im", [L, G, NST, GD])
    ssm_c_re = din("ssm_c_re", [L, G, GD, NST])
    ssm_c_im = din("ssm_c_im", [L, G, GD, NST])
    ssm_d = din("ssm_d", [L, DC])
    ssm_log_dt = din("ssm_log_dt", [L, G])
    ssm_w_glu = din("ssm_w_glu", [L, DC, 2 * DC])
    q_norm = din("q_norm", [L, QL])
    w_uq = din("w_uq", [L, QL, H * DK])
    w_uq_sw = din("w_uq_sw", [L, QL, H * DK])
    kv_norm = din("kv_norm", [L, KVL])
    w_uk = din("w_uk", [L, KVL, H * NOPE])
    w_uv = din("w_uv", [L, KVL, H * DV])
    w_up_conv = din("w_up_conv", [L, DC, D])
    w_up_ssm = din("w_up_ssm", [L, DC, D])
    w_up_attn = din("w_up_attn", [L, DC, D])
    w_o = din("w_o", [L, D, D])
    ln_g = din("ln_g", [L, 2, D])
    ln_b = din("ln_b", [L, 2, D])
    router_w = din("router_w", [D, NE])
    router_bias = din("router_bias", [1, NE])
    exp_w1p = din("exp_w1p", [L, NE, 128, 8 * DE])
    exp_w3p = din("exp_w3p", [L, NE, 128, 8 * DE])
    exp_w2p = din("exp_w2p", [L, NE, 128, 2 * D])
    shared_w1 = din("shared_w1", [L, D, DE])
    shared_w3 = din("shared_w3", [L, D, DE])
    shared_w2 = din("shared_w2", [L, DE, D])
    ident_bf = din("ident_bf", [128, 128], BF16)
    ident_f = din("ident_f", [128, 128])
    ones_bf = din("ones_bf", [128, 128], BF16)
    tri_bf = din("tri_bf", [128, 128], BF16)
    iota_f = din("iota_f", [128, 512])
    iota_p = din("iota_p", [128, 1])
    tris_bf = din("tris_bf", [128, 128], BF16)
    ropec = din("ropec", [32, 2])

    out_d = nc.dram_tensor("out", [S, D], F32, kind="ExternalOutput").ap()
    dbg_aps = {k: nc.dram_tensor(k, list(v[0]), v[1], kind="ExternalOutput").ap() for k, v in dbg.items()}

    ada_d = dscr("ada_d", [L, 6 * D], F32)
    cos_d = dscr("cos_d", [32, S], F32)
    sin_d = dscr("sin_d", [32, S], F32)
    hT_d = dscr("hT_d", [D, S], BF16)
    zc_d = dscr("zc_d", [DC, S], BF16)
    u_d = dscr("u_d", [DC, S], BF16)
    cq_d = dscr("cq_d", [QL, S], BF16)
    ckv_d = dscr("ckv_d", [KVL, S], BF16)
    kr_d = dscr("kr_d", [32, S], BF16)
    zs_d = dscr("zs_d", [DC, S], BF16)
    oT_d = dscr("oT_d", [DC, S], BF16)
    x1_d = dscr("x1_d", [S, D], F32)
    x2_d = dscr("x2_d", [S, D], F32)
    import os as _os3
    _cb = int(_os3.environ.get("KCAPB", "3"))
    NBLK_ = NE * _cb + (2 * ((S - _cb * 128 + 127) // 128) if S > _cb * 128 else 0)
    h2b_d = dscr("h2b_d", [S, 1032], BF16)
    xs_d = dscr("xs_d", [NBLK_ * 128, 1032], BF16)
    ys_d = dscr("ys_d", [NBLK_ * 128, D], BF16)
    ysh_d = dscr("ysh_d", [S, D], F32)

    R = {}

    def reg(name, i=None):
        key = name if i is None else (name, i)
        if key not in R:
            R[key] = Reg()
        return R[key]

    def regs_all(name):
        return [v for k, v in R.items() if (isinstance(k, tuple) and k[0] == name) or k == name]

    with contextlib.ExitStack() as es0:
        fw = FW(nc, es0)
        V, A, P, T, SY = "vector", "scalar", "gpsimd", "tensor", "sync"

        identb = fw.sb([128, 128], BF16, "identb")
        identf = fw.sb([128, 128], F32, "identf")
        onesb = fw.sb([128, 128], BF16, "onesb")
        trib = fw.sb([128, 128], BF16, "trib")
        rc_const = Reg()
        fw.dma(SY, identb[:], ident_bf[:, :], writes=[rc_const])
        fw.dma(SY, identf[:], ident_f[:, :], writes=[rc_const])
        fw.dma(SY, onesb[:], ones_bf[:, :], writes=[rc_const])
        fw.dma(SY, trib[:], tri_bf[:, :], writes=[rc_const])

        with contextlib.ExitStack() as es:
            fw.es = es
            ps_row = fw.ps(es, "ps_row", [1, 512], F32)
            r_ps_row = Reg()
            ccol = fw.sb([128, 8], F32, "ccol")
            cond = fw.sb([128, 8], F32, "cond")
            r_c = Reg()
            fw.dma(SY, ccol[:], c_in.rearrange("o (kt p) -> p (o kt)", p=128), writes=[r_c], allow_slow_non_contiguous=True)
            fw.op(A, lambda e: e.activation(out=cond[:], in_=ccol[:], func=AF.Silu), reads=[r_c], writes=[r_c])
            wa = Rot([fw.sb([128, 8, 512], F32, "wa") for _ in range(2)])
            arow = fw.sb([1, 6 * D], F32, "arow")
            brow = fw.sb([1, 6 * D], F32, "brow")
            r_arow = Reg()
            r_brow = Reg()
            for l in range(L):
                fw.dma(SY, brow[:], b_ada[l:l + 1, :], writes=[r_brow])
                for n in range(12):
                    wt, rw = wa.next()
                    fw.dma(SY, wt[:], w_ada[l, :, n * 512:(n + 1) * 512].rearrange("(kt p) n -> p kt n", p=128), writes=[rw])
                    for kt in range(8):
                        fw.op(T, lambda e, kt=kt, wt=wt: e.matmul(ps_row[:], lhsT=cond[:, kt:kt + 1], rhs=wt[:, kt, :], start=(kt == 0), stop=(kt == 7)),
                              reads=[r_c, rw], writes=[r_ps_row])
                    fw.op(V, lambda e, n=n: e.tensor_tensor(out=arow[:, n * 512:(n + 1) * 512], in0=ps_row[:], in1=brow[:, n * 512:(n + 1) * 512], op=ALU.add),
                          reads=[r_ps_row, r_brow], writes=[r_arow])
                fw.dma(SY, ada_d[l:l + 1, :], arow[:], reads=[r_arow], writes=[reg("ada")])

            posi = fw.sb([32, S], I32, "posi")
            ang = fw.sb([32, S], F32, "ang")
            kf = fw.sb([32, S], F32, "kf")
            ki = fw.sb([32, S], I32, "ki")
            red = fw.sb([32, S], F32, "red")
            tab = fw.sb([32, S], F32, "tab")
            rcs = fw.sb([32, 2], F32, "rcs")
            r_rcs = Reg()
            r_pos = Reg()
            r_ang = Reg()
            r_kf = Reg()
            r_ki = Reg()
            r_red = Reg()
            r_tab = Reg()
            fw.dma(SY, rcs[:], ropec[:, :], writes=[r_rcs])
            fw.dma(SY, posi[:], pos_in.partition_broadcast(32), writes=[r_pos])
            fw.op(V, lambda e: e.tensor_copy(out=ang[:], in_=posi[:]), reads=[r_pos], writes=[r_ang])
            fw.op(V, lambda e: e.tensor_scalar(out=ang[:], in0=ang[:], scalar1=rcs[:, 0:1], scalar2=None, op0=ALU.mult), reads=[r_ang, r_rcs], writes=[r_ang])
            fw.op(V, lambda e: e.tensor_scalar(out=kf[:], in0=ang[:], scalar1=1.0 / TWO_PI, scalar2=None, op0=ALU.mult), reads=[r_ang], writes=[r_kf])
            fw.op(V, lambda e: e.tensor_copy(out=ki[:], in_=kf[:]), reads=[r_kf], writes=[r_ki])
            fw.op(V, lambda e: e.tensor_copy(out=kf[:], in_=ki[:]), reads=[r_ki], writes=[r_kf])
            c1 = float(np.float32(6.28125))
            c2 = float(np.float32(TWO_PI - 6.28125))
            c3 = float(TWO_PI - 6.28125 - float(np.float32(TWO_PI - 6.28125)))
            for cc in (c1, c2, c3):
                fw.op(V, lambda e, cc=cc: e.scalar_tensor_tensor(out=ang[:], in0=kf[:], scalar=-cc, in1=ang[:], op0=ALU.mult, op1=ALU.add), reads=[r_ang, r_kf], writes=[r_ang])

            def wrap_pi(src, shift):
                fw.op(V, lambda e: e.tensor_scalar(out=tab[:], in0=src[:], scalar1=shift, scalar2=None, op0=ALU.add), reads=[r_ang, r_tab], writes=[r_tab])
                for _ in range(2):
                    fw.op(V, lambda e: e.tensor_scalar(out=red[:], in0=tab[:], scalar1=math.pi, scalar2=None, op0=ALU.is_gt), reads=[r_tab], writes=[r_red])
                    fw.op(V, lambda e: e.scalar_tensor_tensor(out=tab[:], in0=red[:], scalar=-TWO_PI, in1=tab[:], op0=ALU.mult, op1=ALU.add), reads=[r_red, r_tab], writes=[r_tab])
                    fw.op(V, lambda e: e.tensor_scalar(out=red[:], in0=tab[:], scalar1=-math.pi, scalar2=None, op0=ALU.is_lt), reads=[r_tab], writes=[r_red])
                    fw.op(V, lambda e: e.scalar_tensor_tensor(out=tab[:], in0=red[:], scalar=TWO_PI, in1=tab[:], op0=ALU.mult, op1=ALU.add), reads=[r_red, r_tab], writes=[r_tab])

            for which, shift, dst in (("sin", 0.0, sin_d), ("cos", math.pi / 2, cos_d)):
                wrap_pi(ang, shift)
                fw.op(A, lambda e: e.activation(out=tab[:], in_=tab[:], func=AF.Sin), reads=[r_tab], writes=[r_tab])
                if which == "sin":
                    fw.op(V, lambda e: e.tensor_scalar(out=tab[:], in0=tab[:], scalar1=rcs[:, 1:2], scalar2=None, op0=ALU.mult), reads=[r_tab, r_rcs], writes=[r_tab])
                fw.dma(SY, dst[:, :], tab[:], reads=[r_tab], writes=[reg("rope")])
            if "d_cos" in dbg_aps:
                pass
        fw.es = es0

        if stop == "p0":
            fw.barrier()
            if "d_ada" in dbg_aps:
                fw.dma(SY, dbg_aps["d_ada"][:, :], ada_d[:, :], reads=[reg("ada")], writes=[reg("dbg")])
            if "d_cos" in dbg_aps:
                fw.dma(SY, dbg_aps["d_cos"][:, :], cos_d[:, :], reads=[reg("rope")], writes=[reg("dbg")])
                fw.dma(SY, dbg_aps["d_sin"][:, :], sin_d[:, :], reads=[reg("rope")], writes=[reg("dbg")])
            fw.finish(list(R.values()))
            return nc


        def mm8(ps_ap, wtile, c0, c1, rhs_tile, regs_r, reg_w, nk=8):
            for kt in range(nk):
                fw.op(T, lambda e, kt=kt: e.matmul(ps_ap, lhsT=wtile[:, kt, c0:c1], rhs=rhs_tile[:, kt, :], start=(kt == 0), stop=(kt == nk - 1)),
                      reads=regs_r, writes=[reg_w])

        def load_cast(dst_tile, r_dst, src_rows_fn, nk, ncols, stg):
            for kt in range(nk):
                rg = Reg()
                r_dst.append(rg)
                fw.dma(P, dst_tile[:, kt, :], src_rows_fn(kt), writes=[rg])

        def layer_norm_stats(xt, r_xt, stats, mv, rstd, r_st, epst, r_eps):
            for hh in range(2):
                fw.op(V, lambda e, hh=hh: e.bn_stats(out=stats[:, hh, :], in_=xt[:, hh * 512:(hh + 1) * 512]), reads=[r_xt], writes=[r_st])
            fw.op(V, lambda e: e.bn_aggr(out=mv[:], in_=stats[:].rearrange("p a b -> p (a b)")), reads=[r_st], writes=[r_st])
            fw.op(A, lambda e: e.activation(out=rstd[:], in_=mv[:, 1:2], func=AF.Sqrt, bias=epst[:], scale=1.0), reads=[r_st, r_eps], writes=[r_st])
            fw.op(V, lambda e: e.reciprocal(out=rstd[:], in_=rstd[:]), reads=[r_st], writes=[r_st])

        def p1(l, x_src, r_xsrc):
            with contextlib.ExitStack() as es:
                fw.es = es
                fw.barrier()
                pst = Rot([fw.ps(es, "p1tp", [128, 8, 128], BF16) for i in range(2)])
                psm = Rot([fw.ps(es, "p1mm", [128, 512], F32) for i in range(5)])
                wA = fw.sb([128, 8, NA], BF16, "wA")
                r_wA = RegList()
                stg = None
                load_cast(wA, r_wA, lambda kt: w_in_a[l, kt * 128:(kt + 1) * 128, :], 8, NA, stg)
                sc = fw.sb([128, 8], F32, "sc")
                sh = fw.sb([128, 8], F32, "sh")
                cw = fw.sb([128, 4, 3], F32, "cw")
                gq = fw.sb([128, 2], F32, "gq")
                gkv = fw.sb([128, 1], F32, "gkv")
                epst = fw.sb([128, 1], F32, "epst")
                epsq = fw.sb([128, 1], F32, "epsq")
                r_small = Reg()
                fw.dma(SY, sh[:], ada_d[l, 0:D].rearrange("(kt p) -> p kt", p=128), reads=[reg("ada")], writes=[r_small], allow_slow_non_contiguous=True)
                fw.dma(SY, sc[:], ada_d[l, D:2 * D].rearrange("(kt p) -> p kt", p=128), reads=[reg("ada")], writes=[r_small], allow_slow_non_contiguous=True)
                for k3 in range(3):
                    fw.dma(SY, cw[:, :, k3], conv_w[l, k3].rearrange("(j p) -> p j", p=128), writes=[r_small], allow_slow_non_contiguous=True)
                fw.dma(SY, gq[:], q_norm[l].rearrange("(j p) -> p j", p=128), writes=[r_small], allow_slow_non_contiguous=True)
                fw.dma(SY, gkv[:], kv_norm[l].rearrange("(j p) -> p j", p=128), writes=[r_small], allow_slow_non_contiguous=True)
                fw.op(V, lambda e: e.tensor_scalar(out=sc[:], in0=sc[:], scalar1=1.0, scalar2=None, op0=ALU.add), reads=[r_small], writes=[r_small])
                fw.op(V, lambda e: e.memset(epst[:], LN_EPS), writes=[r_small])
                fw.op(V, lambda e: e.memset(epsq[:], RMS_EPS), writes=[r_small])
                cosT = fw.sb([32, S], F32, "cosT")
                sinT = fw.sb([32, S], F32, "sinT")
                r_tabs = Reg()
                fw.dma(SY, cosT[:], cos_d[:, :], reads=[reg("rope")], writes=[r_tabs])
                fw.dma(SY, sinT[:], sin_d[:, :], reads=[reg("rope")], writes=[r_tabs])
                ucv = [fw.sb([128, 514], F32, "ucv") for _ in range(4)]
                r_ucv = [Reg() for _ in range(4)]
                for j in range(4):
                    fw.op(V, lambda e, j=j: e.memset(ucv[j][:, 0:2], 0.0), writes=[r_ucv[j]])
                xts = Rot([fw.sb([128, D], F32, "xt") for _ in range(3)])
                xns = Rot([fw.sb([128, D], BF16, "xn") for _ in range(2)])
                hTs = Rot([fw.sb([128, 8, 512], BF16, "hT") for _ in range(2)])
                stats = fw.sb([128, 2, 6], F32, "stats")
                mv = fw.sb([128, 2], F32, "mv")
                rstd = fw.sb([128, 1], F32, "rstd")
                r_st = Reg()
                gcs = Rot([fw.sb([128, 512], F32, "gcs") for _ in range(2)])
                t1s = Rot([fw.sb([128, 512], F32, "t1s") for _ in range(2)])
                zcs = Rot([fw.sb([128, 4, 512], BF16, "zcs") for _ in range(2)])
                ugs = Rot([fw.sb([128, 4, 512], BF16, "ugs") for _ in range(2)])
                sqs = Rot([fw.sb([128, 512], BF16, "sqs") for _ in range(3)])
                rsts = Rot([fw.sb([128, 512], F32, "rsts") for _ in range(2)])
                cqs = Rot([fw.sb([128, 2, 512], BF16, "cqs") for _ in range(2)])
                ckvs = Rot([fw.sb([128, 512], BF16, "ckvs") for _ in range(2)])
                krs = Rot([fw.sb([32, 512], BF16, "krs") for _ in range(2)])
                ta = fw.sb([32, 512], F32, "ta")
                tb_ = fw.sb([32, 512], F32, "tb")
                r_ta = Reg()
                r_tb = Reg()
                hts1 = {}

                def P1_A(tg):
                    t0 = tg * 512
                    hT, r_hT = hTs.next()
                    for tb in range(4):
                        xt, r_xt = xts.next()
                        row0 = t0 + tb * 128
                        fw.dma(SY, xt[:], x_src[row0:row0 + 128, :], reads=[r_xsrc], writes=[r_xt])
                        layer_norm_stats(xt, r_xt, stats, mv, rstd, r_st, epst, r_small)
                        xn, r_xn = xns.next()
                        fw.op(V, lambda e, xn=xn, xt=xt: e.tensor_scalar(out=xn[:], in0=xt[:], scalar1=mv[:, 0:1], scalar2=rstd[:], op0=ALU.subtract, op1=ALU.mult),
                              reads=[r_xt, r_st], writes=[r_xn])
                        tp, r_tp = pst.next()
                        for kt in range(8):
                            fw.op(T, lambda e, kt=kt, tp=tp, xn=xn: e.transpose(out=tp[:, kt, :], in_=xn[:, kt * 128:(kt + 1) * 128], identity=identb[:]),
                                  reads=[r_xn, rc_const], writes=[r_tp])
                        for kt in range(8):
                            fw.op(A, lambda e, kt=kt, tp=tp, hT=hT, tb=tb: e.activation(out=hT[:, kt, tb * 128:(tb + 1) * 128], in_=tp[:, kt, :], func=AF.Identity,
                                                                                  scale=sc[:, kt:kt + 1], bias=sh[:, kt:kt + 1]),
                                  reads=[r_tp, r_small], writes=[r_hT])
                    fw.dma(P, hT_d.rearrange("(kt p) s -> p kt s", p=128)[:, :, t0:t0 + 512], hT[:], reads=[r_hT], writes=[Reg()])
                    hts1[tg] = (hT, r_hT)

                def P1_B(tg):
                    t0 = tg * 512
                    hT, r_hT = hts1.pop(tg)
                    rd = [r_wA, r_hT]
                    zc, r_zc = zcs.next()
                    for j in range(4):
                        xc_ps, r_xc = psm.next()
                        gc_ps, r_gc = psm.next()
                        gb_ps, r_gb = psm.next()
                        mm8(xc_ps[:], wA, j * 128, (j + 1) * 128, hT, rd, r_xc)
                        mm8(gc_ps[:], wA, DC + j * 128, DC + (j + 1) * 128, hT, rd, r_gc)
                        mm8(gb_ps[:], wA, 2 * DC + j * 128, 2 * DC + (j + 1) * 128, hT, rd, r_gb)
                        gc, r_gcs = gcs.next()
                        fw.op(A, lambda e, gc=gc, gc_ps=gc_ps: e.activation(out=gc[:], in_=gc_ps[:], func=AF.Identity), reads=[r_gc], writes=[r_gcs])
                        fw.op(V, lambda e, j=j, gc=gc, xc_ps=xc_ps: e.tensor_tensor(out=ucv[j][:, 2:514], in0=xc_ps[:], in1=gc[:], op=ALU.mult),
                              reads=[r_xc, r_gcs], writes=[r_ucv[j]])
                        t1, r_t1 = t1s.next()
                        fw.op(V, lambda e, j=j, t1=t1: e.tensor_scalar(out=t1[:], in0=ucv[j][:, 0:512], scalar1=cw[:, j, 0:1], scalar2=None, op0=ALU.mult),
                              reads=[r_ucv[j], r_small], writes=[r_t1])
                        fw.op(V, lambda e, j=j, t1=t1: e.scalar_tensor_tensor(out=t1[:], in0=ucv[j][:, 1:513], scalar=cw[:, j, 1:2], in1=t1[:], op0=ALU.mult, op1=ALU.add),
                              reads=[r_ucv[j], r_small, r_t1], writes=[r_t1])
                        fw.op(V, lambda e, j=j, t1=t1: e.scalar_tensor_tensor(out=t1[:], in0=ucv[j][:, 2:514], scalar=cw[:, j, 2:3], in1=t1[:], op0=ALU.mult, op1=ALU.add),
                              reads=[r_ucv[j], r_small, r_t1], writes=[r_t1])
                        fw.op(V, lambda e, j=j, t1=t1, zc=zc, gb_ps=gb_ps: e.tensor_tensor(out=zc[:, j, :], in0=gb_ps[:], in1=t1[:], op=ALU.mult),
                              reads=[r_gb, r_t1], writes=[r_zc])
                        fw.op(A, lambda e, j=j: e.activation(out=ucv[j][:, 0:2], in_=ucv[j][:, 512:514], func=AF.Identity), reads=[r_ucv[j]], writes=[r_ucv[j]])
                    fw.dma(P, zc_d.rearrange("(j p) s -> p j s", p=128)[:, :, t0:t0 + 512], zc[:], reads=[r_zc], writes=[Reg()])
                    ug, r_ug = ugs.next()
                    for j in range(4):
                        u_ps, r_u = psm.next()
                        mm8(u_ps[:], wA, 3 * DC + j * 128, 3 * DC + (j + 1) * 128, hT, rd, r_u)
                        fw.op(A, lambda e, j=j, ug=ug, u_ps=u_ps: e.activation(out=ug[:, j, :], in_=u_ps[:], func=AF.Identity), reads=[r_u], writes=[r_ug])
                    fw.dma(P, u_d.rearrange("(j p) s -> p j s", p=128)[:, :, t0:t0 + 512], ug[:], reads=[r_ug], writes=[Reg()])
                    for (nj, c0, gvec, dst_rot, dst_d, dname) in ((2, 4 * DC, gq, cqs, cq_d, "cq"), (1, 4 * DC + QL, gkv, ckvs, ckv_d, "ckv")):
                        dst, r_dst = dst_rot.next()
                        pss = []
                        ssq_ps, r_ssq = psm.next()
                        for j in range(nj):
                            c_ps, r_cps = psm.next()
                            mm8(c_ps[:], wA, c0 + j * 128, c0 + (j + 1) * 128, hT, rd, r_cps)
                            sq, r_sq = sqs.next()
                            fw.op(A, lambda e, sq=sq, c_ps=c_ps: e.activation(out=sq[:], in_=c_ps[:], func=AF.Square), reads=[r_cps], writes=[r_sq])
                            fw.op(T, lambda e, sq=sq, j=j, ssq_ps=ssq_ps, nj=nj: e.matmul(ssq_ps[:], lhsT=onesb[:], rhs=sq[:], start=(j == 0), stop=(j == nj - 1)),
                                  reads=[r_sq, rc_const], writes=[r_ssq])
                            pss.append((c_ps, r_cps))
                        rst, r_rst = rsts.next()
                        fw.op(A, lambda e, rst=rst, ssq_ps=ssq_ps, nj=nj: e.activation(out=rst[:], in_=ssq_ps[:], func=AF.Sqrt, bias=epsq[:], scale=1.0 / (128 * nj)),
                              reads=[r_ssq, r_small], writes=[r_rst])
                        fw.op(V, lambda e, rst=rst: e.reciprocal(out=rst[:], in_=rst[:]), reads=[r_rst], writes=[r_rst])
                        for j in range(nj):
                            c_ps, r_cps = pss[j]
                            o_ap = dst[:, j, :] if nj == 2 else dst[:]
                            fw.op(V, lambda e, o_ap=o_ap, c_ps=c_ps, j=j, rst=rst, gvec=gvec: e.scalar_tensor_tensor(out=o_ap, in0=c_ps[:], scalar=gvec[:, j:j + 1], in1=rst[:], op0=ALU.mult, op1=ALU.mult),
                                  reads=[r_cps, r_rst, r_small], writes=[r_dst])
                        if nj == 2:
                            fw.dma(P, dst_d.rearrange("(j p) s -> p j s", p=128)[:, :, t0:t0 + 512], dst[:], reads=[r_dst], writes=[Reg()])
                        else:
                            fw.dma(P, dst_d[:, t0:t0 + 512], dst[:], reads=[r_dst], writes=[Reg()])
                    kr_ps, r_kr = psm.next()
                    ksw_ps, r_ksw = psm.next()
                    mm8(kr_ps[0:32, :], wA, IN_MIX - ROPE, IN_MIX, hT, rd, r_kr)
                    mm8(ksw_ps[0:32, :], wA, IN_MIX, IN_MIX + ROPE, hT, rd, r_ksw)
                    kr, r_krs = krs.next()
                    fw.op(V, lambda e, kr_ps=kr_ps: e.tensor_tensor(out=ta[:], in0=kr_ps[0:32, :], in1=cosT[:, t0:t0 + 512], op=ALU.mult), reads=[r_kr, r_tabs], writes=[r_ta])
                    fw.op(V, lambda e, ksw_ps=ksw_ps: e.tensor_tensor(out=tb_[:], in0=ksw_ps[0:32, :], in1=sinT[:, t0:t0 + 512], op=ALU.mult), reads=[r_ksw, r_tabs], writes=[r_tb])
                    fw.op(V, lambda e, kr=kr: e.tensor_tensor(out=kr[:], in0=ta[:], in1=tb_[:], op=ALU.add), reads=[r_ta, r_tb], writes=[r_krs])
                    fw.dma(P, kr_d[:, t0:t0 + 512], kr[:], reads=[r_krs], writes=[Reg()])

                P1_A(0)
                for tg in range(NTG):
                    if tg + 1 < NTG:
                        P1_A(tg + 1)
                    P1_B(tg)
            fw.es = es0


        def p3(l):
            with contextlib.ExitStack() as es:
                fw.es = es
                fw.barrier()
                ps_s = Rot([fw.ps(es, "p3s", [128, 512], F32) for i in range(3)])
                ps_o = Rot([fw.ps(es, "p3o", [128, 4, DV + 1], F32) for i in range(2)])
                ps_q = Rot([fw.ps(es, "p3q", [128, 512], F32) for i in range(2)])
                ps_t = Rot([fw.ps(es, "p3t", [128, 4, 128], BF16) for i in range(1)])
                stg = None
                wq = fw.sb([128, 2, H * DK], BF16, "wq")
                wqs = fw.sb([128, 2, H * DK], BF16, "wqs")
                wk = fw.sb([128, 1, H * NOPE], BF16, "wk")
                wv = fw.sb([128, 1, H * DV], BF16, "wv")
                r_w = RegList()
                load_cast(wq, r_w, lambda kt: w_uq[l, kt * 128:(kt + 1) * 128, :], 2, H * DK, stg)
                load_cast(wqs, r_w, lambda kt: w_uq_sw[l, kt * 128:(kt + 1) * 128, :], 2, H * DK, stg)
                load_cast(wk, r_w, lambda kt: w_uk[l, kt * 128:(kt + 1) * 128, :], 1, H * NOPE, stg)
                load_cast(wv, r_w, lambda kt: w_uv[l, kt * 128:(kt + 1) * 128, :], 1, H * DV, stg)
                ckv = fw.sb([128, S], BF16, "ckv")
                r_ckv = Reg()
                fw.dma(SY, ckv[:], ckv_d[:, :], reads=[reg("ckv")], writes=[r_ckv])
                KT = fw.sb([DK, H, S], BF16, "KT")
                r_KT = Reg()
                Vt = fw.sb([128, NTB, H, DV + 1], BF16, "Vt")
                r_V = Reg()
                for h in range(H):
                    fw.dma(SY, KT[NOPE:DK, h, :], kr_d[:, :], reads=[reg("kr")], writes=[r_KT])
                fw.op(P, lambda e: e.memset(Vt[:, :, :, DV:DV + 1], 1.0), writes=[r_V])
                cnt = 0
                for tg in range(NTG):
                    t0 = tg * 512
                    for h in range(H):
                        kp, r_kp = ps_q.next()
                        fw.op(T, lambda e, kp=kp, h=h, t0=t0: e.matmul(kp[0:NOPE, :], lhsT=wk[:, 0, h * NOPE:(h + 1) * NOPE], rhs=ckv[:, t0:t0 + 512], start=True, stop=True),
                              reads=[r_w, r_ckv], writes=[r_kp])
                        if cnt % 2 == 0:
                            fw.op(A, lambda e, kp=kp, h=h, t0=t0: e.activation(out=KT[0:NOPE, h, t0:t0 + 512], in_=kp[0:NOPE, :], func=AF.Identity), reads=[r_kp], writes=[r_KT])
                        else:
                            fw.op(V, lambda e, kp=kp, h=h, t0=t0: e.tensor_copy(out=KT[0:NOPE, h, t0:t0 + 512], in_=kp[0:NOPE, :]), reads=[r_kp], writes=[r_KT])
                        cnt += 1
                    for tb in range(4):
                        tbg = tg * 4 + tb
                        vp, r_vp = ps_q.next()
                        fw.op(T, lambda e, vp=vp, tbg=tbg: e.matmul(vp[:], lhsT=ckv[:, tbg * 128:(tbg + 1) * 128], rhs=wv[:, 0, :], start=True, stop=True),
                              reads=[r_w, r_ckv], writes=[r_vp])
                        if cnt % 2 == 0:
                            fw.op(A, lambda e, vp=vp, tbg=tbg: e.activation(out=Vt[:, tbg, :, 0:DV], in_=vp[:].rearrange("p (h d) -> p h d", h=H), func=AF.Identity), reads=[r_vp], writes=[r_V])
                        else:
                            fw.op(V, lambda e, vp=vp, tbg=tbg: e.tensor_copy(out=Vt[:, tbg, :, 0:DV], in_=vp[:].rearrange("p (h d) -> p h d", h=H)), reads=[r_vp], writes=[r_V])
                        cnt += 1
                cqgs = Rot([fw.sb([128, 2, 512], BF16, "cqg") for _ in range(2)])
                css = Rot([fw.sb([DK, 512], F32, "cs") for _ in range(2)])
                sns = Rot([fw.sb([DK, 512], F32, "sn") for _ in range(2)])
                qTs = Rot([fw.sb([DK, 512], BF16, "qT") for _ in range(3)])
                pTs = Rot([fw.sb([128, 512], BF16, "pT") for _ in range(4)])
                tas = Rot([fw.sb([DK, 512], F32, "ta3") for _ in range(2)])
                tbs = Rot([fw.sb([DK, 512], F32, "tb3") for _ in range(2)])
                otoks = Rot([fw.sb([128, 4, DC], BF16, "otok") for _ in range(2)])
                oTgs = Rot([fw.sb([128, 4, 512], BF16, "oTg") for _ in range(2)])
                rinvs = Rot([fw.sb([128, 4], F32, "rinv") for _ in range(2)])
                sm_scale = float(DK) ** -0.5
                for qg in range(NTG):
                    t0 = qg * 512
                    cqg, r_cqg = cqgs.next()
                    cs, r_cs = css.next()
                    sn, r_sn = sns.next()
                    fw.dma(SY, cqg[:], cq_d.rearrange("(j p) s -> p j s", p=128)[:, :, t0:t0 + 512], reads=[reg("cq")], writes=[r_cqg])
                    fw.dma(SY, cs[NOPE:DK, :], cos_d[:, t0:t0 + 512], reads=[reg("rope")], writes=[r_cs])
                    fw.dma(SY, sn[NOPE:DK, :], sin_d[:, t0:t0 + 512], reads=[reg("rope")], writes=[r_sn])
                    otok, r_otok = otoks.next()
                    qst = {}

                    def Q3(h):
                        q_ps, r_qps = ps_q.next()
                        qs_ps, r_qsps = ps_q.next()
                        for kt in range(2):
                            fw.op(T, lambda e, kt=kt, q_ps=q_ps, h=h, cqg=cqg: e.matmul(q_ps[0:DK, :], lhsT=wq[:, kt, h * DK:(h + 1) * DK], rhs=cqg[:, kt, :], start=(kt == 0), stop=(kt == 1)),
                                  reads=[r_w, r_cqg], writes=[r_qps])
                        for kt in range(2):
                            fw.op(T, lambda e, kt=kt, qs_ps=qs_ps, h=h, cqg=cqg: e.matmul(qs_ps[0:DK, :], lhsT=wqs[:, kt, h * DK:(h + 1) * DK], rhs=cqg[:, kt, :], start=(kt == 0), stop=(kt == 1)),
                                  reads=[r_w, r_cqg], writes=[r_qsps])
                        qT, r_qT = qTs.next()
                        ta3, r_ta3 = tas.next()
                        tb3, r_tb3 = tbs.next()
                        fw.op(A, lambda e, qT=qT, q_ps=q_ps: e.activation(out=qT[0:NOPE, :], in_=q_ps[0:NOPE, :], func=AF.Identity), reads=[r_qps], writes=[r_qT])
                        fw.op(V, lambda e, ta3=ta3, q_ps=q_ps, cs=cs: e.tensor_tensor(out=ta3[NOPE:DK, :], in0=q_ps[NOPE:DK, :], in1=cs[NOPE:DK, :], op=ALU.mult), reads=[r_qps, r_cs], writes=[r_ta3])
                        fw.op(V, lambda e, tb3=tb3, qs_ps=qs_ps, sn=sn: e.tensor_tensor(out=tb3[NOPE:DK, :], in0=qs_ps[NOPE:DK, :], in1=sn[NOPE:DK, :], op=ALU.mult), reads=[r_qsps, r_sn], writes=[r_tb3])
                        fw.op(V, lambda e, qT=qT, ta3=ta3, tb3=tb3: e.tensor_tensor(out=qT[NOPE:DK, :], in0=ta3[NOPE:DK, :], in1=tb3[NOPE:DK, :], op=ALU.add), reads=[r_ta3, r_tb3], writes=[r_qT])
                        qst[h] = (qT, r_qT)

                    def KB3(h):
                        qT, r_qT = qst.pop(h)
                        o_ps, r_ops = ps_o.next()
                        nkb = 4 * qg + 4

                        def emit_s(kb):
                            m = kb - 4 * qg
                            c0 = max(0, m) * 128
                            s_ps, r_sps = ps_s.next()
                            fw.op(T, lambda e: e.matmul(s_ps[:, c0:512], lhsT=KT[0:DK, h, kb * 128:(kb + 1) * 128], rhs=qT[0:DK, c0:512], start=True, stop=True),
                                  reads=[r_KT, r_qT], writes=[r_sps])
                            return (s_ps, r_sps, m, c0)

                        pend = [emit_s(0)]
                        if nkb > 1:
                            pend.append(emit_s(1))
                        for kb in range(nkb):
                            s_ps, r_sps, m, c0 = pend.pop(0)
                            if kb + 2 < nkb:
                                pend.append(emit_s(kb + 2))
                            pT, r_pT = pTs.next()
                            fw.op(A, lambda e, pT=pT, s_ps=s_ps, c0=c0: e.activation(out=pT[:, c0:512], in_=s_ps[:, c0:512], func=AF.Exp, scale=sm_scale), reads=[r_sps], writes=[r_pT])
                            if m >= 0:
                                fw.op(V, lambda e, pT=pT, c0=c0: e.tensor_tensor(out=pT[:, c0:c0 + 128], in0=pT[:, c0:c0 + 128], in1=trib[:], op=ALU.mult), reads=[r_pT, rc_const], writes=[r_pT])
                            for qb in range(max(0, m), 4):
                                fw.op(T, lambda e, qb=qb, pT=pT, kb=kb: e.matmul(o_ps[:, qb, :], lhsT=pT[:, qb * 128:(qb + 1) * 128], rhs=Vt[:, kb, h, :], start=(kb == 0 and qb == 0), stop=(kb == 4 * qg + qb), skip_group_check=True),
                                      reads=[r_pT, r_V], writes=[r_ops])
                        rinv, r_rinv = rinvs.next()
                        fw.op(V, lambda e, rinv=rinv, o_ps=o_ps: e.reciprocal(out=rinv[:], in_=o_ps[:, :, DV]), reads=[r_ops], writes=[r_rinv])
                        for qb in range(4):
                            fw.op(V, lambda e, qb=qb, rinv=rinv, o_ps=o_ps, otok=otok, h=h: e.tensor_scalar(out=otok[:, qb, h * DV:(h + 1) * DV], in0=o_ps[:, qb, 0:DV], scalar1=rinv[:, qb:qb + 1], scalar2=None, op0=ALU.mult),
                                  reads=[r_ops, r_rinv], writes=[r_otok])

                    Q3(0)
                    for h in range(H):
                        if h + 1 < H:
                            Q3(h + 1)
                        KB3(h)
                    oTg, r_oTg = oTgs.next()
                    for qb in range(4):
                        tp, r_tp = ps_t.next()
                        for j in range(4):
                            fw.op(T, lambda e, j=j, qb=qb, tp=tp, otok=otok: e.transpose(out=tp[:, j, :], in_=otok[:, qb, j * 128:(j + 1) * 128], identity=identb[:]), reads=[r_otok, rc_const], writes=[r_tp])
                        fw.op(A, lambda e, qb=qb, tp=tp, oTg=oTg: e.activation(out=oTg[:, :, qb * 128:(qb + 1) * 128], in_=tp[:], func=AF.Identity), reads=[r_tp], writes=[r_oTg])
                    fw.dma(P, oT_d.rearrange("(j p) s -> p j s", p=128)[:, :, t0:t0 + 512], oTg[:], reads=[r_oTg], writes=[Reg()])
                if "d_KT" in dbg_aps:
                    fw.dma(SY, dbg_aps["d_KT"][:, :], KT[:].rearrange("p h s -> p (h s)"), reads=[r_KT], writes=[reg("dbg")])
                    fw.dma(SY, dbg_aps["d_V"][:, :], Vt[:].rearrange("p a h d -> p (a h d)"), reads=[r_V], writes=[reg("dbg")])
                    fw.dma(SY, dbg_aps["d_qT"][:, :], qT[:], reads=[r_qT], writes=[reg("dbg")])
                    fw.dma(SY, dbg_aps["d_pT"][:, :], pT[:], reads=[r_pT], writes=[reg("dbg")])
                    fw.dma(SY, dbg_aps["d_otok"][:, :], otok[:].rearrange("p a d -> p (a d)"), reads=[r_otok], writes=[reg("dbg")])
            fw.es = es0


        C1 = float(np.float32(6.28125))
        C2 = float(np.float32(TWO_PI - 6.28125))
        C3 = float(TWO_PI - 6.28125 - float(np.float32(TWO_PI - 6.28125)))
        GELU_K = 2.0 * math.sqrt(2.0 / math.pi)

        def p2(l):
            TC = 512
            with contextlib.ExitStack() as es:
                fw.es = es
                fw.barrier()
                ps_b = Rot([fw.ps(es, "p2b", [128, 512], F32) for i in range(4)])
                ps_y = Rot([fw.ps(es, "p2y", [128, 512], F32) for i in range(2)])
                ps_g = Rot([fw.ps(es, "p2g", [128, 512], F32) for i in range(2)])
                r_pre = Reg()
                sh16 = [128, 16]
                are = fw.sb(sh16, F32, "are")
                aim = fw.sb(sh16, F32, "aim")
                dtl = fw.sb(sh16, F32, "dtl")
                rr = fw.sb(sh16, F32, "rr")
                th = fw.sb(sh16, F32, "th")
                thT = fw.sb(sh16, F32, "thT")
                lre = fw.sb(sh16, F32, "lre")
                lim = fw.sb(sh16, F32, "lim")
                cTc = fw.sb(sh16, F32, "cTc")
                sTc = fw.sb(sh16, F32, "sTc")
                nsTc = fw.sb(sh16, F32, "nsTc")
                fre = fw.sb(sh16, F32, "fre")
                fim = fw.sb(sh16, F32, "fim")
                nfim = fw.sb(sh16, F32, "nfim")
                den = fw.sb(sh16, F32, "den")
                t16a = fw.sb(sh16, F32, "t16a")
                t16b = fw.sb(sh16, F32, "t16b")
                t16i = fw.sb(sh16, I32, "t16i")
                dsk = fw.sb([128, 4], F32, "dsk")
                iot = fw.sb([128, TC], F32, "iot")
                fw.dma(SY, are[:], ssm_a_re[l].rearrange("(gp g2) n -> (g2 n) gp", g2=2), writes=[r_pre], allow_slow_non_contiguous=True)
                fw.dma(SY, aim[:], ssm_a_im[l].rearrange("(gp g2) n -> (g2 n) gp", g2=2), writes=[r_pre], allow_slow_non_contiguous=True)
                ldt2 = ssm_log_dt[l].rearrange("(gp g2) -> g2 gp", g2=2)
                for g2 in range(2):
                    fw.dma(SY, dtl[g2 * 64:(g2 + 1) * 64, :], ldt2[g2:g2 + 1, :].partition_broadcast(64), writes=[r_pre], allow_slow_non_contiguous=True)
                fw.dma(SY, dsk[:], ssm_d[l].rearrange("(ct p) -> p ct", p=128), writes=[r_pre], allow_slow_non_contiguous=True)
                fw.dma(SY, iot[:], iota_f[:, 0:TC], writes=[r_pre])

                def vop(f, reads=(r_pre,), writes=(r_pre,), eng=V):
                    fw.op(eng, f, reads=list(reads), writes=list(writes))

                def sincos(src, sin_dst, cos_dst, shape, tf, ti, tm):
                    vop(lambda e: e.tensor_scalar(out=tf, in0=src, scalar1=1.0 / TWO_PI, scalar2=None, op0=ALU.mult))
                    vop(lambda e: e.tensor_copy(out=ti, in_=tf))
                    vop(lambda e: e.tensor_copy(out=tf, in_=ti))
                    vop(lambda e: e.scalar_tensor_tensor(out=tm, in0=tf, scalar=-C1, in1=src, op0=ALU.mult, op1=ALU.add))
                    vop(lambda e: e.scalar_tensor_tensor(out=tm, in0=tf, scalar=-C2, in1=tm, op0=ALU.mult, op1=ALU.add))
                    vop(lambda e: e.scalar_tensor_tensor(out=tm, in0=tf, scalar=-C3, in1=tm, op0=ALU.mult, op1=ALU.add))
                    for dst, shift in ((sin_dst, 0.0), (cos_dst, math.pi / 2)):
                        vop(lambda e, dst=dst, shift=shift: e.tensor_scalar(out=dst, in0=tm, scalar1=shift, scalar2=None, op0=ALU.add))
                        vop(lambda e, dst=dst: e.tensor_scalar(out=tf, in0=dst, scalar1=math.pi, scalar2=None, op0=ALU.is_gt))
                        vop(lambda e, dst=dst: e.scalar_tensor_tensor(out=dst, in0=tf, scalar=-TWO_PI, in1=dst, op0=ALU.mult, op1=ALU.add))
                        vop(lambda e, dst=dst: e.tensor_scalar(out=tf, in0=dst, scalar1=-math.pi, scalar2=None, op0=ALU.is_lt))
                        vop(lambda e, dst=dst: e.scalar_tensor_tensor(out=dst, in0=tf, scalar=TWO_PI, in1=dst, op0=ALU.mult, op1=ALU.add))
                        vop(lambda e, dst=dst: e.tensor_scalar(out=dst, in0=dst, scalar1=math.pi, scalar2=-math.pi, op0=ALU.min, op1=ALU.max))
                        vop(lambda e, dst=dst: e.activation(out=dst, in_=dst, func=AF.Sin), eng=A)

                vop(lambda e: e.activation(out=dtl[:], in_=dtl[:], func=AF.Exp), eng=A)
                vop(lambda e: e.tensor_tensor(out=rr[:], in0=are[:], in1=dtl[:], op=ALU.mult))
                vop(lambda e: e.activation(out=rr[:], in_=rr[:], func=AF.Exp), eng=A)
                vop(lambda e: e.tensor_tensor(out=th[:], in0=aim[:], in1=dtl[:], op=ALU.mult))
                sincos(th[:], lim[:], lre[:], sh16, t16a[:], t16i[:], t16b[:])
                vop(lambda e: e.tensor_tensor(out=lre[:], in0=lre[:], in1=rr[:], op=ALU.mult))
                vop(lambda e: e.tensor_tensor(out=lim[:], in0=lim[:], in1=rr[:], op=ALU.mult))
                vop(lambda e: e.tensor_scalar(out=thT[:], in0=th[:], scalar1=float(TC), scalar2=None, op0=ALU.mult))
                sincos(thT[:], sTc[:], cTc[:], sh16, t16a[:], t16i[:], t16b[:])
                vop(lambda e: e.tensor_scalar(out=nsTc[:], in0=sTc[:], scalar1=-1.0, scalar2=None, op0=ALU.mult))
                vop(lambda e: e.tensor_tensor(out=den[:], in0=are[:], in1=are[:], op=ALU.mult))
                vop(lambda e: e.tensor_tensor(out=t16a[:], in0=aim[:], in1=aim[:], op=ALU.mult))
                vop(lambda e: e.tensor_tensor(out=den[:], in0=den[:], in1=t16a[:], op=ALU.add))
                vop(lambda e: e.reciprocal(out=den[:], in_=den[:]))
                vop(lambda e: e.tensor_scalar(out=t16a[:], in0=lre[:], scalar1=-1.0, scalar2=None, op0=ALU.add))
                vop(lambda e: e.tensor_tensor(out=fre[:], in0=t16a[:], in1=are[:], op=ALU.mult))
                vop(lambda e: e.tensor_tensor(out=t16b[:], in0=lim[:], in1=aim[:], op=ALU.mult))
                vop(lambda e: e.tensor_tensor(out=fre[:], in0=fre[:], in1=t16b[:], op=ALU.add))
                vop(lambda e: e.tensor_tensor(out=fre[:], in0=fre[:], in1=den[:], op=ALU.mult))
                vop(lambda e: e.tensor_tensor(out=fim[:], in0=lim[:], in1=are[:], op=ALU.mult))
                vop(lambda e: e.tensor_tensor(out=t16b[:], in0=t16a[:], in1=aim[:], op=ALU.mult))
                vop(lambda e: e.tensor_tensor(out=fim[:], in0=fim[:], in1=t16b[:], op=ALU.subtract))
                vop(lambda e: e.tensor_tensor(out=fim[:], in0=fim[:], in1=den[:], op=ALU.mult))
                vop(lambda e: e.tensor_scalar(out=nfim[:], in0=fim[:], scalar1=-1.0, scalar2=None, op0=ALU.mult))

                BT = [fw.sb([128, 16, 128], BF16, "BTre"), fw.sb([128, 16, 128], BF16, "BTim")]
                CT = [fw.sb([128, 16, 128], BF16, "CTre"), fw.sb([128, 16, 128], BF16, "CTim"), fw.sb([128, 16, 128], BF16, "CTnre")]
                r_BT = Reg()
                r_CT = Reg()
                with contextlib.ExitStack() as es_pre:
                    fw.es = es_pre
                    BD = [fw.sb([128, 16, 128], F32, "BDre"), fw.sb([128, 16, 128], F32, "BDim")]
                    BB = [fw.sb([128, 16, 128], F32, "BBre"), fw.sb([128, 16, 128], F32, "BBim")]
                    CBD = [fw.sb([32, 16, 128], F32, "CBDre"), fw.sb([32, 16, 128], F32, "CBDim")]
                    r_BD = Reg()
                    r_BB = Reg()
                    r_CBD = Reg()
                    for ri, src in enumerate((ssm_b_re, ssm_b_im)):
                        fw.op(P, lambda e, ri=ri: e.memset(BD[ri][:], 0.0), writes=[r_BD])
                        v = src[l].rearrange("(ct j g2) n q -> j g2 n ct q", j=4, g2=2)
                        for j in range(4):
                            for g2 in range(2):
                                dstv = BD[ri][g2 * 64:(g2 + 1) * 64, :, j * 32 + g2 * 16:j * 32 + g2 * 16 + 16].rearrange("p (ct jj) q -> p ct jj q", jj=4)[:, :, j, :]
                                fw.dma(SY, dstv, v[j, g2], writes=[r_BD])
                    for ri, src in enumerate((ssm_c_re, ssm_c_im)):
                        fw.op(P, lambda e, ri=ri: e.memset(CBD[ri][:], 0.0), writes=[r_CBD])
                        fw.op(P, lambda e, ri=ri: e.memset(CT[ri][:], 0.0), writes=[r_CT])
                        if ri == 0:
                            fw.op(P, lambda e: e.memset(CT[2][:], 0.0), writes=[r_CT])
                        v = src[l].rearrange("(gp g2) p n -> g2 p gp n", g2=2)
                        for g2 in range(2):
                            fw.dma(SY, CBD[ri][g2 * 16:(g2 + 1) * 16, :, g2 * 64:(g2 + 1) * 64], v[g2], writes=[r_CBD])
                    bc16 = lambda t: t[:].unsqueeze(2).to_broadcast([128, 16, 128])
                    BBt = fw.sb([128, 16, 128], F32, "BBt")
                    fw.op(V, lambda e: e.tensor_tensor(out=BB[0][:], in0=BD[0][:], in1=bc16(fre), op=ALU.mult), reads=[r_BD, r_pre], writes=[r_BB])
                    fw.op(V, lambda e: e.tensor_tensor(out=BBt[:], in0=BD[1][:], in1=bc16(nfim), op=ALU.mult), reads=[r_BD, r_pre], writes=[r_BB])
                    fw.op(V, lambda e: e.tensor_tensor(out=BB[0][:], in0=BB[0][:], in1=BBt[:], op=ALU.add), reads=[r_BB], writes=[r_BB])
                    fw.op(V, lambda e: e.tensor_tensor(out=BB[1][:], in0=BD[1][:], in1=bc16(fre), op=ALU.mult), reads=[r_BD, r_pre], writes=[r_BB])
                    fw.op(V, lambda e: e.tensor_tensor(out=BBt[:], in0=BD[0][:], in1=bc16(fim), op=ALU.mult), reads=[r_BD, r_pre], writes=[r_BB])
                    fw.op(V, lambda e: e.tensor_tensor(out=BB[1][:], in0=BB[1][:], in1=BBt[:], op=ALU.add), reads=[r_BB], writes=[r_BB])
                    for gp in range(16):
                        for ri in range(2):
                            tp, r_tp = ps_b.next()
                            fw.op(T, lambda e, gp=gp, ri=ri, tp=tp: e.transpose(out=tp[:, 0:128], in_=BB[ri][:, gp, :], identity=identf[:]), reads=[r_BB, rc_const], writes=[r_tp])
                            fw.op(A, lambda e, gp=gp, ri=ri, tp=tp: e.activation(out=BT[ri][:, gp, :], in_=tp[:, 0:128], func=AF.Identity), reads=[r_tp], writes=[r_BT])
                            tp2, r_tp2 = ps_b.next()
                            fw.op(T, lambda e, gp=gp, ri=ri, tp2=tp2: e.transpose(out=tp2[:, 0:32], in_=CBD[ri][:, gp, :], identity=identf[0:32, 0:32]), reads=[r_CBD, rc_const], writes=[r_tp2])
                            jj = gp % 4
                            fw.op(A, lambda e, gp=gp, ri=ri, tp2=tp2, jj=jj: e.activation(out=CT[ri][:, gp, jj * 32:(jj + 1) * 32], in_=tp2[:, 0:32], func=AF.Identity, scale=(1.0 if ri == 0 else -1.0)),
                                  reads=[r_tp2], writes=[r_CT])
                            if ri == 0:
                                fw.op(A, lambda e, gp=gp, tp2=tp2, jj=jj: e.activation(out=CT[2][:, gp, jj * 32:(jj + 1) * 32], in_=tp2[:, 0:32], func=AF.Identity, scale=-1.0),
                                      reads=[r_tp2], writes=[r_CT])
                fw.es = es
                fw.barrier()
                cosT = fw.sb([128, 16, TC], F32, "cosT2")
                sinT = fw.sb([128, 16, TC], F32, "sinT2")
                with contextlib.ExitStack() as es_t:
                    fw.es = es_t
                    NT = 8 * TC
                    angt = fw.sb([128, NT], F32, "angt")
                    tft = fw.sb([128, NT], F32, "tft")
                    tit = fw.sb([128, NT], I32, "tit")
                    tmt = fw.sb([128, NT], F32, "tmt")
                    g3 = lambda t: t[:].rearrange("p (g t) -> p g t", g=8)
                    for hf in range(2):
                        gs_ = slice(hf * 8, hf * 8 + 8)
                        vop(lambda e, gs_=gs_: e.tensor_tensor(out=g3(angt), in0=iot[:].unsqueeze(1).to_broadcast([128, 8, TC]), in1=th[:, gs_].unsqueeze(2).to_broadcast([128, 8, TC]), op=ALU.mult))
                        sincos(angt[:], sinT[:, gs_, :].rearrange("p g t -> p (g t)"), cosT[:, gs_, :].rearrange("p g t -> p (g t)"), [128, NT], tft[:], tit[:], tmt[:])
                fw.es = es
                fw.barrier()
                wg = fw.sb([128, 4, 2 * DC], BF16, "wg")
                r_wg = RegList()
                stg = None
                load_cast(wg, r_wg, lambda kt: ssm_w_glu[l, kt * 128:(kt + 1) * 128, :], 4, 2 * DC, stg)
                car = [fw.sb([128, 16], F32, "car_re"), fw.sb([128, 16], F32, "car_im")]
                r_car = [Reg() for _ in range(16)]
                fw.op(V, lambda e: e.memset(car[0][:], 0.0), writes=r_car)
                fw.op(V, lambda e: e.memset(car[1][:], 0.0), writes=r_car)
                uts = Rot([fw.sb([128, 4, TC], BF16, "uT2") for _ in range(3)])
                f32t = lambda nm, n: Rot([fw.sb([128, TC], F32, nm) for _ in range(n)])
                t1s, t2s, t3s, t4s = f32t("s1", 2), f32t("s2", 2), f32t("s3", 2), f32t("s4", 2)
                wres, wims = f32t("wre", 3), f32t("wim", 3)
                vres, vims = f32t("vre", 3), f32t("vim", 3)
                bft = lambda nm, n: Rot([fw.sb([128, TC], BF16, nm) for _ in range(n)])
                p1s, p2s, p3s, p4s = bft("q1", 3), bft("q2", 3), bft("q3", 3), bft("q4", 3)
                sres = Rot([fw.sb([128, TC], BF16, "sre") for _ in range(3)])
                sims = Rot([fw.sb([128, TC], BF16, "sim") for _ in range(3)])
                ygs = Rot([fw.sb([128, 4, TC], BF16, "yg") for _ in range(2)])
                yfs = f32t("yf", 2)
                ysq = f32t("ysq", 2)
                sgs = f32t("sg2", 2)
                zss = Rot([fw.sb([128, 4, TC], BF16, "zs") for _ in range(2)])
                ctmp = fw.sb([128, 2], F32, "ctmp")
                r_ctmp = Reg()
                upre = {}

                def load_u(c):
                    ut, r_ut = uts.next()
                    fw.dma(SY, ut[:], u_d.rearrange("(j p) s -> p j s", p=128)[:, :, c * TC:(c + 1) * TC], reads=[reg("u")], writes=[r_ut])
                    upre[c] = (ut, r_ut)

                load_u(0)
                for c in range(S // TC):
                    t0 = c * TC
                    if c + 1 < S // TC:
                        load_u(c + 1)
                    ut, r_ut = upre.pop(c)
                    yg, r_yg = ygs.next()
                    st2 = {}

                    def P2A(it):
                        ct, j = divmod(it, 4)
                        gp = it
                        cs_ = cosT[:, gp, :]
                        sn_ = sinT[:, gp, :]
                        bre, r_bre = ps_b.next()
                        bim, r_bim = ps_b.next()
                        fw.op(T, lambda e, gp=gp, bre=bre, ct=ct, ut=ut: e.matmul(bre[:, 0:TC], lhsT=BT[0][:, gp, :], rhs=ut[:, ct, :], start=True, stop=True), reads=[r_BT, r_ut], writes=[r_bre])
                        fw.op(T, lambda e, gp=gp, bim=bim, ct=ct, ut=ut: e.matmul(bim[:, 0:TC], lhsT=BT[1][:, gp, :], rhs=ut[:, ct, :], start=True, stop=True), reads=[r_BT, r_ut], writes=[r_bim])
                        cs_ = cosT[:, gp, :]
                        sn_ = sinT[:, gp, :]
                        (t1, r1), (t2, r2), (t3, r3), (t4, r4) = t1s.next(), t2s.next(), t3s.next(), t4s.next()
                        fw.op(V, lambda e, t1=t1, bre=bre, cs_=cs_: e.tensor_tensor(out=t1[:], in0=bre[:, 0:TC], in1=cs_, op=ALU.mult), reads=[r_bre, r_pre], writes=[r1])
                        fw.op(V, lambda e, t2=t2, bim=bim, sn_=sn_: e.tensor_tensor(out=t2[:], in0=bim[:, 0:TC], in1=sn_, op=ALU.mult), reads=[r_bim, r_pre], writes=[r2])
                        fw.op(V, lambda e, t3=t3, bim=bim, cs_=cs_: e.tensor_tensor(out=t3[:], in0=bim[:, 0:TC], in1=cs_, op=ALU.mult), reads=[r_bim, r_pre], writes=[r3])
                        fw.op(V, lambda e, t4=t4, bre=bre, sn_=sn_: e.tensor_tensor(out=t4[:], in0=bre[:, 0:TC], in1=sn_, op=ALU.mult), reads=[r_bre, r_pre], writes=[r4])
                        (wre, r_wre), (wim, r_wim) = wres.next(), wims.next()
                        fw.op(P, lambda e, wre=wre, t1=t1, t2=t2: e.tensor_tensor(out=wre[:], in0=t1[:], in1=t2[:], op=ALU.add), reads=[r1, r2], writes=[r_wre])
                        fw.op(P, lambda e, wim=wim, t3=t3, t4=t4: e.tensor_tensor(out=wim[:], in0=t3[:], in1=t4[:], op=ALU.subtract), reads=[r3, r4], writes=[r_wim])
                        st2[it] = dict(wre=wre, r_wre=r_wre, wim=wim, r_wim=r_wim)

                    def P2B(it):
                        ct, j = divmod(it, 4)
                        gp = it
                        cs_ = cosT[:, gp, :]
                        sn_ = sinT[:, gp, :]
                        d_ = st2[it]
                        wre, r_wre, wim, r_wim = d_['wre'], d_['r_wre'], d_['wim'], d_['r_wim']
                        (vre, r_vre), (vim, r_vim) = vres.next(), vims.next()
                        fw.op(V, lambda e, vre=vre, wre=wre, gp=gp: e.tensor_tensor_scan(out=vre[:], data0=rr[:, gp:gp + 1].to_broadcast([128, TC]), data1=wre[:], initial=car[0][:, gp:gp + 1], op0=ALU.mult, op1=ALU.add),
                              reads=[r_wre, r_pre, r_car[gp]], writes=[r_vre])
                        fw.op(V, lambda e, vim=vim, wim=wim, gp=gp: e.tensor_tensor_scan(out=vim[:], data0=rr[:, gp:gp + 1].to_broadcast([128, TC]), data1=wim[:], initial=car[1][:, gp:gp + 1], op0=ALU.mult, op1=ALU.add),
                              reads=[r_wim, r_pre, r_car[gp]], writes=[r_vim])
                        fw.op(A, lambda e, vre=vre, gp=gp: e.activation(out=ctmp[:, 0:1], in_=vre[:, TC - 1:TC], func=AF.Identity, scale=cTc[:, gp:gp + 1]), reads=[r_vre, r_pre], writes=[r_ctmp])
                        fw.op(A, lambda e, vre=vre, gp=gp: e.activation(out=ctmp[:, 1:2], in_=vre[:, TC - 1:TC], func=AF.Identity, scale=sTc[:, gp:gp + 1]), reads=[r_vre, r_pre], writes=[r_ctmp])
                        fw.op(A, lambda e, vim=vim, gp=gp: e.activation(out=car[0][:, gp:gp + 1], in_=vim[:, TC - 1:TC], func=AF.Identity, scale=nsTc[:, gp:gp + 1], bias=ctmp[:, 0:1]),
                              reads=[r_vim, r_pre, r_ctmp], writes=[r_car[gp]])
                        fw.op(A, lambda e, vim=vim, gp=gp: e.activation(out=car[1][:, gp:gp + 1], in_=vim[:, TC - 1:TC], func=AF.Identity, scale=cTc[:, gp:gp + 1], bias=ctmp[:, 1:2]),
                              reads=[r_vim, r_pre, r_ctmp], writes=[r_car[gp]])
                        (q1, rq1), (q2, rq2), (q3, rq3), (q4, rq4) = p1s.next(), p2s.next(), p3s.next(), p4s.next()
                        fw.op(P, lambda e, q1=q1, vre=vre, cs_=cs_: e.tensor_tensor(out=q1[:], in0=vre[:], in1=cs_, op=ALU.mult), reads=[r_vre, r_pre], writes=[rq1])
                        fw.op(P, lambda e, q2=q2, vim=vim, sn_=sn_: e.tensor_tensor(out=q2[:], in0=vim[:], in1=sn_, op=ALU.mult), reads=[r_vim, r_pre], writes=[rq2])
                        fw.op(P, lambda e, q3=q3, vre=vre, sn_=sn_: e.tensor_tensor(out=q3[:], in0=vre[:], in1=sn_, op=ALU.mult), reads=[r_vre, r_pre], writes=[rq3])
                        fw.op(V, lambda e, q4=q4, vim=vim, cs_=cs_: e.tensor_tensor(out=q4[:], in0=vim[:], in1=cs_, op=ALU.mult), reads=[r_vim, r_pre], writes=[rq4])
                        st2[it] = dict(q1=q1, rq1=rq1, q2=q2, rq2=rq2, q3=q3, rq3=rq3, q4=q4, rq4=rq4)

                    def P2C(it):
                        ct, j = divmod(it, 4)
                        gp = it
                        d_ = st2.pop(it)
                        q1, rq1, q2, rq2, q3, rq3, q4, rq4 = d_['q1'], d_['rq1'], d_['q2'], d_['rq2'], d_['q3'], d_['rq3'], d_['q4'], d_['rq4']
                        if j == 0:
                            st2['y', ct] = ps_y.next()
                        y_ps, r_yps = st2['y', ct]
                        fw.op(T, lambda e, gp=gp, q1=q1, j=j, y_ps=y_ps: e.matmul(y_ps[:, 0:TC], lhsT=CT[0][:, gp, :], rhs=q1[:], start=(j == 0), stop=False), reads=[r_CT, rq1], writes=[r_yps])
                        fw.op(T, lambda e, gp=gp, q2=q2, y_ps=y_ps: e.matmul(y_ps[:, 0:TC], lhsT=CT[2][:, gp, :], rhs=q2[:], start=False, stop=False), reads=[r_CT, rq2], writes=[r_yps])
                        fw.op(T, lambda e, gp=gp, q3=q3, y_ps=y_ps: e.matmul(y_ps[:, 0:TC], lhsT=CT[1][:, gp, :], rhs=q3[:], start=False, stop=False), reads=[r_CT, rq3], writes=[r_yps])
                        fw.op(T, lambda e, gp=gp, q4=q4, j=j, y_ps=y_ps: e.matmul(y_ps[:, 0:TC], lhsT=CT[1][:, gp, :], rhs=q4[:], start=False, stop=(j == 3)), reads=[r_CT, rq4], writes=[r_yps])
                        if j == 3:
                            (yf, r_yf), (yq, r_yq), (sg, r_sg) = yfs.next(), ysq.next(), sgs.next()
                            fw.op(V, lambda e, yf=yf, ut=ut, ct=ct, y_ps=y_ps: e.scalar_tensor_tensor(out=yf[:], in0=ut[:, ct, :], scalar=dsk[:, ct:ct + 1], in1=y_ps[:, 0:TC], op0=ALU.mult, op1=ALU.add),
                                  reads=[r_ut, r_pre, r_yps], writes=[r_yf])
                            fw.op(A, lambda e, yq=yq, yf=yf: e.activation(out=yq[:], in_=yf[:], func=AF.Square), reads=[r_yf], writes=[r_yq])
                            fw.op(V, lambda e, yq=yq: e.tensor_scalar(out=yq[:], in0=yq[:], scalar1=0.044715, scalar2=1.0, op0=ALU.mult, op1=ALU.add), reads=[r_yq], writes=[r_yq])
                            fw.op(V, lambda e, yq=yq, yf=yf: e.tensor_tensor(out=yq[:], in0=yq[:], in1=yf[:], op=ALU.mult), reads=[r_yq, r_yf], writes=[r_yq])
                            fw.op(A, lambda e, sg=sg, yq=yq: e.activation(out=sg[:], in_=yq[:], func=AF.Sigmoid, scale=GELU_K), reads=[r_yq], writes=[r_sg])
                            fw.op(P, lambda e, yg=yg, ct=ct, yf=yf, sg=sg: e.tensor_tensor(out=yg[:, ct, :], in0=yf[:], in1=sg[:], op=ALU.mult), reads=[r_yf, r_sg], writes=[r_yg])

                    for it2 in range(16 + 2):
                        if it2 < 16:
                            P2A(it2)
                        if 0 <= it2 - 1 < 16:
                            P2B(it2 - 1)
                        if 0 <= it2 - 2 < 16:
                            P2C(it2 - 2)
                    zs, r_zs = zss.next()
                    for m in range(4):
                        v_ps, r_vps = ps_g.next()
                        g_ps, r_gps = ps_g.next()
                        for kt in range(4):
                            fw.op(T, lambda e, kt=kt, m=m, v_ps=v_ps, yg=yg: e.matmul(v_ps[:, 0:TC], lhsT=wg[:, kt, m * 128:(m + 1) * 128], rhs=yg[:, kt, :], start=(kt == 0), stop=(kt == 3)), reads=[r_wg, r_yg], writes=[r_vps])
                        for kt in range(4):
                            fw.op(T, lambda e, kt=kt, m=m, g_ps=g_ps, yg=yg: e.matmul(g_ps[:, 0:TC], lhsT=wg[:, kt, DC + m * 128:DC + (m + 1) * 128], rhs=yg[:, kt, :], start=(kt == 0), stop=(kt == 3)), reads=[r_wg, r_yg], writes=[r_gps])
                        sg, r_sg = sgs.next()
                        fw.op(A, lambda e, sg=sg, g_ps=g_ps: e.activation(out=sg[:], in_=g_ps[:, 0:TC], func=AF.Sigmoid), reads=[r_gps], writes=[r_sg])
                        fw.op(V, lambda e, zs=zs, m=m, v_ps=v_ps, sg=sg: e.tensor_tensor(out=zs[:, m, :], in0=v_ps[:, 0:TC], in1=sg[:], op=ALU.mult), reads=[r_vps, r_sg], writes=[r_zs])
                    fw.dma(SY, zs_d.rearrange("(j p) s -> p j s", p=128)[:, :, t0:t0 + TC], zs[:], reads=[r_zs], writes=[Reg()])
            fw.es = es0


        def load_cast2(dst_fn, r_dst, src_fn, nk, ncols, stg):
            for kt in range(nk):
                rg = Reg()
                r_dst.append(rg)
                fw.dma(P, dst_fn(kt), src_fn(kt), writes=[rg])

        def final_ln(tt, r_tt, stats, mv, rstd, r_st, epst, r_c, lng, lnb, xo, r_xo, eng2=P):
            layer_norm_stats(tt, r_tt, stats, mv, rstd, r_st, epst, r_c)
            fw.op(V, lambda e: e.tensor_scalar(out=tt[:], in0=tt[:], scalar1=mv[:, 0:1], scalar2=rstd[:], op0=ALU.subtract, op1=ALU.mult), reads=[r_tt, r_st], writes=[r_tt])
            fw.op(eng2, lambda e: e.tensor_tensor(out=tt[:], in0=tt[:], in1=lng[:], op=ALU.mult), reads=[r_tt, r_c], writes=[r_tt])
            fw.op(eng2, lambda e: e.tensor_tensor(out=xo[:], in0=tt[:], in1=lnb[:], op=ALU.add), reads=[r_tt, r_c], writes=[r_xo])

        def p4(l, x_src, r_xsrc):
            with contextlib.ExitStack() as es:
                fw.es = es
                fw.barrier()
                ps_yb = [Rot([fw.ps(es, "p4y", [128, 512], F32)]) for i in range(3)]
                ps_g = Rot([fw.ps(es, "p4g", [128, 512], F32) for i in range(3)])
                ps_o = Rot([fw.ps(es, "p4o", [128, 512], F32) for i in range(2)])
                stg = None
                wgate = fw.sb([128, 8, 3 * D], BF16, "wgate")
                wup = [fw.sb([128, 4, D], BF16, f"wup{i}") for i in range(3)]
                wo = fw.sb([128, 8, D], BF16, "wo")
                r_w = RegList()
                for i, src in enumerate((w_up_conv, w_up_ssm, w_up_attn)):
                    load_cast2(lambda kt, i=i: wup[i][:, kt, :], r_w, lambda kt, src=src: src[l, kt * 128:(kt + 1) * 128, :], 4, D, stg)
                for cch in range(3):
                    load_cast2(lambda kt, cch=cch: wgate[:, kt, cch * D:(cch + 1) * D], r_w, lambda kt, cch=cch: w_in_g[l, kt * 128:(kt + 1) * 128, cch * D:(cch + 1) * D], 8, D, stg)
                load_cast2(lambda kt: wo[:, kt, :], r_w, lambda kt: w_o[l, kt * 128:(kt + 1) * 128, :], 8, D, stg)
                g1b = fw.sb([128, D], F32, "g1b")
                lng = fw.sb([128, D], F32, "lng")
                lnb = fw.sb([128, D], F32, "lnb")
                epst = fw.sb([128, 1], F32, "epst4")
                r_c = Reg()
                fw.dma(SY, g1b[:], ada_d[l:l + 1, 2 * D:3 * D].partition_broadcast(128), reads=[reg("ada")], writes=[r_c])
                fw.dma(SY, lng[:], ln_g[l, 0:1, :].partition_broadcast(128), writes=[r_c])
                fw.dma(SY, lnb[:], ln_b[l, 0:1, :].partition_broadcast(128), writes=[r_c])
                fw.op(V, lambda e: e.memset(epst[:], LN_EPS), writes=[r_c])
                hTs = Rot([fw.sb([128, 8, 512], BF16, "hT4") for _ in range(2)])
                zts = [Rot([fw.sb([128, 4, 512], BF16, f"z4{i}") for _ in range(2)]) for i in range(3)]
                mgs = Rot([fw.sb([128, 8, 512], BF16, "mg") for _ in range(2)])
                sgs = Rot([fw.sb([128, 512], F32, "sg4") for _ in range(4)])
                accs = Rot([fw.sb([128, 512], F32, "acc4") for _ in range(2)])
                tts = Rot([fw.sb([128, 512], F32, "tt4") for _ in range(3)])
                xts = Rot([fw.sb([128, D], F32, "xt4") for _ in range(2)])
                ybs = Rot([fw.sb([128, D], F32, "yb4") for _ in range(2)])
                xos = Rot([fw.sb([128, D], F32, "xo4") for _ in range(2)])
                stats = fw.sb([128, 2, 6], F32, "stats4")
                mv = fw.sb([128, 2], F32, "mv4")
                rstd = fw.sb([128, 1], F32, "rstd4")
                r_st = Reg()
                zsrc = ((zc_d, "zc"), (zs_d, "zs"), (oT_d, "oT"))
                for tg in range(NTG):
                    t0 = tg * 512
                    hT, r_hT = hTs.next()
                    fw.dma(SY, hT[:], hT_d.rearrange("(kt p) s -> p kt s", p=128)[:, :, t0:t0 + 512], reads=[reg("hT")], writes=[r_hT])
                    zt = []
                    for i in range(3):
                        z, r_z = zts[i].next()
                        fw.dma(SY, z[:], zsrc[i][0].rearrange("(j p) s -> p j s", p=128)[:, :, t0:t0 + 512], reads=[reg(zsrc[i][1])], writes=[r_z])
                        zt.append((z, r_z))
                    mg, r_mg = mgs.next()
                    for m in range(8):
                        yps = []
                        for i in range(3):
                            y_ps, r_yps = ps_yb[i].next()
                            z, r_z = zt[i]
                            for kt in range(4):
                                fw.op(T, lambda e, kt=kt, i=i, m=m, y_ps=y_ps, z=z: e.matmul(y_ps[:], lhsT=wup[i][:, kt, m * 128:(m + 1) * 128], rhs=z[:, kt, :], start=(kt == 0), stop=(kt == 3)),
                                      reads=[r_w, r_z], writes=[r_yps])
                            yps.append((y_ps, r_yps))
                        sg_l = []
                        for i in range(3):
                            g_ps, r_gps = ps_g.next()
                            for kt in range(8):
                                fw.op(T, lambda e, kt=kt, i=i, m=m, g_ps=g_ps, hT=hT: e.matmul(g_ps[:], lhsT=wgate[:, kt, i * D + m * 128:i * D + (m + 1) * 128], rhs=hT[:, kt, :], start=(kt == 0), stop=(kt == 7)),
                                      reads=[r_w, r_hT], writes=[r_gps])
                            sg, r_sg = sgs.next()
                            fw.op(A, lambda e, sg=sg, g_ps=g_ps: e.activation(out=sg[:], in_=g_ps[:], func=AF.Sigmoid), reads=[r_gps], writes=[r_sg])
                            sg_l.append((sg, r_sg))
                        acc, r_acc = accs.next()
                        ta_, r_ta_ = tts.next()
                        tb2, r_tb2 = tts.next()
                        fw.op(V, lambda e, acc=acc: e.tensor_tensor(out=acc[:], in0=yps[0][0][:], in1=sg_l[0][0][:], op=ALU.mult), reads=[yps[0][1], sg_l[0][1]], writes=[r_acc])
                        fw.op(V, lambda e, ta_=ta_: e.tensor_tensor(out=ta_[:], in0=yps[1][0][:], in1=sg_l[1][0][:], op=ALU.mult), reads=[yps[1][1], sg_l[1][1]], writes=[r_ta_])
                        fw.op(V, lambda e, tb2=tb2: e.tensor_tensor(out=tb2[:], in0=yps[2][0][:], in1=sg_l[2][0][:], op=ALU.mult), reads=[yps[2][1], sg_l[2][1]], writes=[r_tb2])
                        fw.op(P, lambda e, acc=acc, ta_=ta_: e.tensor_tensor(out=acc[:], in0=acc[:], in1=ta_[:], op=ALU.add), reads=[r_acc, r_ta_], writes=[r_acc])
                        fw.op(P, lambda e, acc=acc, tb2=tb2, mg=mg, m=m: e.tensor_tensor(out=mg[:, m, :], in0=acc[:], in1=tb2[:], op=ALU.add), reads=[r_acc, r_tb2], writes=[r_mg])
                    for tb in range(4):
                        row0 = t0 + tb * 128
                        xt, r_xt = xts.next()
                        fw.dma(SY, xt[:], x_src[row0:row0 + 128, :], reads=[r_xsrc], writes=[r_xt])
                        yb, r_yb = ybs.next()
                        for hh in range(2):
                            o_ps, r_ops = ps_o.next()
                            for kt in range(8):
                                fw.op(T, lambda e, kt=kt, hh=hh, tb=tb, o_ps=o_ps, mg=mg: e.matmul(o_ps[:], lhsT=mg[:, kt, tb * 128:(tb + 1) * 128], rhs=wo[:, kt, hh * 512:(hh + 1) * 512], start=(kt == 0), stop=(kt == 7)),
                                      reads=[r_w, r_mg], writes=[r_ops])
                            fw.op(V, lambda e, hh=hh, o_ps=o_ps, yb=yb: e.tensor_tensor(out=yb[:, hh * 512:(hh + 1) * 512], in0=o_ps[:], in1=g1b[:, hh * 512:(hh + 1) * 512], op=ALU.mult), reads=[r_ops, r_c], writes=[r_yb])
                        fw.op(V, lambda e, xt=xt, yb=yb: e.scalar_tensor_tensor(out=yb[:], in0=xt[:], scalar=ALPHA, in1=yb[:], op0=ALU.mult, op1=ALU.add), reads=[r_xt, r_yb], writes=[r_yb])
                        xo, r_xo = xos.next()
                        final_ln(yb, r_yb, stats, mv, rstd, r_st, epst, r_c, lng, lnb, xo, r_xo)
                        fw.dma(P, x1_d[row0:row0 + 128, :], xo[:], reads=[r_xo], writes=[Reg()])
            fw.es = es0

        def p5_dense(l, x_dst, r_xdst):
            NH = max(1, S // 2048)
            HALF = S // NH
            NTBH = HALF // 128
            NTGH = HALF // 512
            with contextlib.ExitStack() as es:
                fw.es = es
                fw.barrier()
                sc = fw.sb([128, 8], F32, "sc5")
                sh = fw.sb([128, 8], F32, "sh5")
                g2b = fw.sb([128, D], F32, "g2b")
                lng = fw.sb([128, D], F32, "lng5")
                lnb = fw.sb([128, D], F32, "lnb5")
                rbb = fw.sb([128, NE], F32, "rbb")
                rw32 = fw.sb([128, 8, NE], F32, "rw32")
                epst = fw.sb([128, 1], F32, "epst5")
                r_c = Reg()
                fw.dma(SY, sh[:], ada_d[l, 3 * D:4 * D].rearrange("(kt p) -> p kt", p=128), reads=[reg("ada")], writes=[r_c], allow_slow_non_contiguous=True)
                fw.dma(SY, sc[:], ada_d[l, 4 * D:5 * D].rearrange("(kt p) -> p kt", p=128), reads=[reg("ada")], writes=[r_c], allow_slow_non_contiguous=True)
                fw.dma(SY, g2b[:], ada_d[l:l + 1, 5 * D:6 * D].partition_broadcast(128), reads=[reg("ada")], writes=[r_c])
                fw.dma(SY, lng[:], ln_g[l, 1:2, :].partition_broadcast(128), writes=[r_c])
                fw.dma(SY, lnb[:], ln_b[l, 1:2, :].partition_broadcast(128), writes=[r_c])
                fw.dma(SY, rbb[:], router_bias[0:1, :].partition_broadcast(128), writes=[r_c])
                fw.dma(SY, rw32[:], router_w.rearrange("(kt p) e -> p kt e", p=128), writes=[r_c])
                fw.op(V, lambda e: e.tensor_scalar(out=sc[:], in0=sc[:], scalar1=1.0, scalar2=None, op0=ALU.add), reads=[r_c], writes=[r_c])
                fw.op(V, lambda e: e.memset(epst[:], LN_EPS), writes=[r_c])
                h2T = fw.sb([128, 8, HALF], BF16, "h2T")
                r_h2T = Reg()
                Gd = fw.sb([128, NTBH, NE], F32, "Gd")
                r_Gd = Reg()
                yacc = fw.sb([128, NTBH, D], F32, "yacc")
                r_yacc = [Reg() for _ in range(NTBH)]
                stats = fw.sb([128, 2, 6], F32, "stats5")
                mv = fw.sb([128, 2], F32, "mv5")
                rstd = fw.sb([128, 1], F32, "rstd5")
                r_st = Reg()
                for hf in range(NH):
                    tok0 = hf * HALF
                    with contextlib.ExitStack() as esA:
                        fw.es = esA
                        fw.barrier()
                        ps_tp = Rot([fw.ps(esA, "p5tp", [128, 8, 128], F32) for i in range(2)])
                        ps_lg = Rot([fw.ps(esA, "p5lg", [128, NE], F32) for i in range(2)])
                        xts = Rot([fw.sb([128, D], F32, "xt5") for _ in range(3)])
                        xns = Rot([fw.sb([128, D], F32, "xn5") for _ in range(2)])
                        h32s = Rot([fw.sb([128, 8, 128], F32, "h32") for _ in range(2)])
                        rt = {k: fw.sb([128, NE], F32, "rt" + k) for k in ("s", "sel", "eq", "sel2", "ge2", "mask", "ws")}
                        r8 = {k: fw.sb([128, 8], F32, "r8" + k) for k in ("m1", "m2", "gs", "gsel")}
                        r1 = {k: fw.sb([128, 1], F32, "r1" + k) for k in ("gmax", "den")}
                        r_rt = Reg()
                        for tbh in range(NTBH):
                            row0 = tok0 + tbh * 128
                            xt, r_xt = xts.next()
                            fw.dma(SY, xt[:], x1_d[row0:row0 + 128, :], reads=[reg("x1")], writes=[r_xt])
                            layer_norm_stats(xt, r_xt, stats, mv, rstd, r_st, epst, r_c)
                            xn, r_xn = xns.next()
                            fw.op(V, lambda e, xn=xn, xt=xt: e.tensor_scalar(out=xn[:], in0=xt[:], scalar1=mv[:, 0:1], scalar2=rstd[:], op0=ALU.subtract, op1=ALU.mult), reads=[r_xt, r_st], writes=[r_xn])
                            tp, r_tp = ps_tp.next()
                            for kt in range(8):
                                fw.op(T, lambda e, kt=kt, tp=tp, xn=xn: e.transpose(out=tp[:, kt, :], in_=xn[:, kt * 128:(kt + 1) * 128], identity=identf[:]), reads=[r_xn, rc_const], writes=[r_tp])
                            h32, r_h32 = h32s.next()
                            for kt in range(8):
                                fw.op(A, lambda e, kt=kt, tp=tp, h32=h32: e.activation(out=h32[:, kt, :], in_=tp[:, kt, :], func=AF.Identity, scale=sc[:, kt:kt + 1], bias=sh[:, kt:kt + 1]), reads=[r_tp, r_c], writes=[r_h32])
                            fw.op(P, lambda e, h32=h32, tbh=tbh: e.tensor_copy(out=h2T[:, :, tbh * 128:(tbh + 1) * 128], in_=h32[:]), reads=[r_h32], writes=[r_h2T])
                            lg, r_lg = ps_lg.next()
                            for kt in range(8):
                                fw.op(T, lambda e, kt=kt, lg=lg, h32=h32: e.matmul(lg[:], lhsT=h32[:, kt, :], rhs=rw32[:, kt, :], start=(kt == 0), stop=(kt == 7)), reads=[r_h32, r_c], writes=[r_lg])
                            rr_ = [r_rt]
                            v3 = lambda a: a[:].rearrange("p (g k) -> p g k", k=4)
                            b3 = lambda a: a[:].unsqueeze(2).to_broadcast([128, 8, 4])
                            fw.op(A, lambda e, lg=lg: e.activation(out=rt["s"][:], in_=lg[:], func=AF.Sigmoid), reads=[r_lg], writes=rr_)
                            fw.op(V, lambda e: e.tensor_tensor(out=rt["sel"][:], in0=rt["s"][:], in1=rbb[:], op=ALU.add), reads=rr_ + [r_c], writes=rr_)
                            fw.op(V, lambda e: e.tensor_reduce(out=r8["m1"][:], in_=v3(rt["sel"]), axis=AX.X, op=ALU.max), reads=rr_, writes=rr_)
                            fw.op(V, lambda e: e.tensor_tensor(out=v3(rt["eq"]), in0=v3(rt["sel"]), in1=b3(r8["m1"]), op=ALU.is_equal), reads=rr_, writes=rr_)
                            fw.op(V, lambda e: e.scalar_tensor_tensor(out=rt["sel2"][:], in0=rt["eq"][:], scalar=-1.0e9, in1=rt["sel"][:], op0=ALU.mult, op1=ALU.add), reads=rr_, writes=rr_)
                            fw.op(V, lambda e: e.tensor_reduce(out=r8["m2"][:], in_=v3(rt["sel2"]), axis=AX.X, op=ALU.max), reads=rr_, writes=rr_)
                            fw.op(V, lambda e: e.tensor_tensor(out=r8["gs"][:], in0=r8["m1"][:], in1=r8["m2"][:], op=ALU.add), reads=rr_, writes=rr_)
                            fw.op(V, lambda e: e.tensor_reduce(out=r1["gmax"][:], in_=r8["gs"][:], axis=AX.X, op=ALU.max), reads=rr_, writes=rr_)
                            fw.op(V, lambda e: e.tensor_scalar(out=r8["gsel"][:], in0=r8["gs"][:], scalar1=r1["gmax"][:], scalar2=None, op0=ALU.is_equal), reads=rr_, writes=rr_)
                            fw.op(V, lambda e: e.tensor_tensor(out=v3(rt["ge2"]), in0=v3(rt["sel"]), in1=b3(r8["m2"]), op=ALU.is_ge), reads=rr_, writes=rr_)
                            fw.op(V, lambda e: e.tensor_tensor(out=v3(rt["mask"]), in0=v3(rt["ge2"]), in1=b3(r8["gsel"]), op=ALU.mult), reads=rr_, writes=rr_)
                            fw.op(V, lambda e: e.tensor_tensor(out=rt["ws"][:], in0=rt["mask"][:], in1=rt["s"][:], op=ALU.mult), reads=rr_, writes=rr_)
                            fw.op(V, lambda e: e.tensor_reduce(out=r1["den"][:], in_=rt["ws"][:], axis=AX.X, op=ALU.add), reads=rr_, writes=rr_)
                            fw.op(V, lambda e: e.reciprocal(out=r1["den"][:], in_=r1["den"][:]), reads=rr_, writes=rr_)
                            fw.op(V, lambda e, tbh=tbh: e.tensor_scalar(out=Gd[:, tbh, :], in0=rt["ws"][:], scalar1=r1["den"][:], scalar2=None, op0=ALU.mult), reads=rr_, writes=[r_Gd])
                        if "d_G" in dbg_aps and hf == 0 and stop == f"p5_{l}":
                            fw.dma(SY, dbg_aps["d_G"].rearrange("(a p) e -> p a e", p=128)[:, 0:NTBH, :], Gd[:], reads=[r_Gd], writes=[reg("dbg")])
                    with contextlib.ExitStack() as esB:
                        fw.es = esB
                        fw.barrier()
                        ps_h1 = Rot([fw.ps(esB, "p5h1", [128, 512], F32) for i in range(2)])
                        ps_h3 = Rot([fw.ps(esB, "p5h3", [128, 512], F32) for i in range(2)])
                        ps_y = Rot([fw.ps(esB, "p5y", [128, 512], F32) for i in range(4)])
                        st1 = Rot([fw.sb([128, 8, DE], F32, "st1") for _ in range(2)])
                        st3 = Rot([fw.sb([128, 8, DE], F32, "st3") for _ in range(2)])
                        st2 = Rot([fw.sb([128, 2, D], F32, "st2") for _ in range(2)])
                        w1bs = Rot([fw.sb([128, 8, DE], BF16, "w1b") for _ in range(2)])
                        w3bs = Rot([fw.sb([128, 8, DE], BF16, "w3b") for _ in range(2)])
                        w2bs = Rot([fw.sb([128, 2, D], BF16, "w2b") for _ in range(2)])
                        sgs = Rot([fw.sb([128, 512], F32, "sg5") for _ in range(2)])
                        aTs = Rot([fw.sb([128, 2, 512], BF16, "aT") for _ in range(2)])
                        for ei in range(NE + 1):
                            e_id = ei - 1
                            if ei == 0:
                                s1, s3, s2 = shared_w1[l], shared_w3[l], shared_w2[l]
                            else:
                                s1, s3, s2 = exp_w1[l, e_id], exp_w3[l, e_id], exp_w2[l, e_id]
                            (a1, ra1), (a3, ra3), (a2, ra2) = st1.next(), st3.next(), st2.next()
                            fw.dma(SY, a1[:], s1.rearrange("(kt p) n -> p kt n", p=128), writes=[ra1])
                            fw.dma(SY, a3[:], s3.rearrange("(kt p) n -> p kt n", p=128), writes=[ra3])
                            fw.dma(SY, a2[:], s2.rearrange("(kt p) n -> p kt n", p=128), writes=[ra2])
                            (w1b, rw1), (w3b, rw3), (w2b, rw2) = w1bs.next(), w3bs.next(), w2bs.next()
                            fw.op(P, lambda e, w1b=w1b, a1=a1: e.tensor_copy(out=w1b[:], in_=a1[:]), reads=[ra1], writes=[rw1])
                            fw.op(P, lambda e, w3b=w3b, a3=a3: e.tensor_copy(out=w3b[:], in_=a3[:]), reads=[ra3], writes=[rw3])
                            fw.op(A, lambda e, w2b=w2b, a2=a2: e.activation(out=w2b[:], in_=a2[:], func=AF.Identity), reads=[ra2], writes=[rw2])
                            for tg in range(NTGH):
                                aT, r_aT = aTs.next()
                                for m in range(2):
                                    h1, r_h1 = ps_h1.next()
                                    h3, r_h3 = ps_h3.next()
                                    for kt in range(8):
                                        fw.op(T, lambda e, kt=kt, m=m, h1=h1, w1b=w1b, tg=tg: e.matmul(h1[:], lhsT=w1b[:, kt, m * 128:(m + 1) * 128], rhs=h2T[:, kt, tg * 512:(tg + 1) * 512], start=(kt == 0), stop=(kt == 7)),
                                              reads=[rw1, r_h2T], writes=[r_h1])
                                    for kt in range(8):
                                        fw.op(T, lambda e, kt=kt, m=m, h3=h3, w3b=w3b, tg=tg: e.matmul(h3[:], lhsT=w3b[:, kt, m * 128:(m + 1) * 128], rhs=h2T[:, kt, tg * 512:(tg + 1) * 512], start=(kt == 0), stop=(kt == 7)),
                                              reads=[rw3, r_h2T], writes=[r_h3])
                                    sg, r_sg = sgs.next()
                                    fw.op(A, lambda e, sg=sg, h1=h1: e.activation(out=sg[:], in_=h1[:], func=AF.Silu), reads=[r_h1], writes=[r_sg])
                                    fw.op(V, lambda e, sg=sg, h3=h3, aT=aT, m=m: e.tensor_tensor(out=aT[:, m, :], in0=h3[:], in1=sg[:], op=ALU.mult), reads=[r_h3, r_sg], writes=[r_aT])
                                for tb in range(4):
                                    tbh = tg * 4 + tb
                                    for hh in range(2):
                                        y_ps, r_yps = ps_y.next()
                                        for kt in range(2):
                                            fw.op(T, lambda e, kt=kt, hh=hh, tb=tb, y_ps=y_ps, aT=aT, w2b=w2b: e.matmul(y_ps[:], lhsT=aT[:, kt, tb * 128:(tb + 1) * 128], rhs=w2b[:, kt, hh * 512:(hh + 1) * 512], start=(kt == 0), stop=(kt == 1)),
                                                  reads=[rw2, r_aT], writes=[r_yps])
                                        ya = yacc[:, tbh, hh * 512:(hh + 1) * 512]
                                        if ei == 0:
                                            fw.op(V, lambda e, ya=ya, y_ps=y_ps: e.tensor_copy(out=ya, in_=y_ps[:]), reads=[r_yps], writes=[r_yacc[tbh]])
                                        else:
                                            fw.op(V, lambda e, ya=ya, y_ps=y_ps, tbh=tbh, e_id=e_id: e.scalar_tensor_tensor(out=ya, in0=y_ps[:], scalar=Gd[:, tbh, e_id:e_id + 1], in1=ya, op0=ALU.mult, op1=ALU.add),
                                                  reads=[r_yps, r_Gd, r_yacc[tbh]], writes=[r_yacc[tbh]])
                    with contextlib.ExitStack() as esC:
                        fw.es = esC
                        fw.barrier()
                        xts = Rot([fw.sb([128, D], F32, "xt5c") for _ in range(2)])
                        xos = Rot([fw.sb([128, D], F32, "xo5") for _ in range(2)])
                        for tbh in range(NTBH):
                            row0 = tok0 + tbh * 128
                            xt, r_xt = xts.next()
                            fw.dma(SY, xt[:], x1_d[row0:row0 + 128, :], reads=[reg("x1")], writes=[r_xt])
                            ya = yacc[:, tbh, :]
                            r_ya = r_yacc[tbh]
                            fw.op(V, lambda e, ya=ya: e.tensor_tensor(out=ya, in0=ya, in1=g2b[:], op=ALU.mult), reads=[r_ya, r_c], writes=[r_ya])
                            fw.op(V, lambda e, ya=ya, xt=xt: e.scalar_tensor_tensor(out=ya, in0=xt[:], scalar=ALPHA, in1=ya, op0=ALU.mult, op1=ALU.add), reads=[r_xt, r_ya], writes=[r_ya])
                            xo, r_xo = xos.next()
                            final_ln(ya, r_ya, stats, mv, rstd, r_st, epst, r_c, lng, lnb, xo, r_xo)
                            fw.dma(P, x_dst[row0:row0 + 128, :], xo[:], reads=[r_xo], writes=[Reg()])
                    fw.es = es
            fw.es = es0


        import os as _os2
        CAPB = int(_os2.environ.get("KCAPB", "3"))
        CAP = CAPB * 128
        NSB = NE * CAPB
        NOV = 2 * ((S - CAP + 127) // 128) if S > CAP else 0
        NBLK = NSB + NOV
        NROW = NBLK * 128
        RW = 1032

        def idma(out, out_off, in_, in_off, reads=(), writes=(), **kw):
            E = fw.E[P]
            if KSIM:
                ds_ = [fw.new_sem(), 0]
                fw.dsems.append(ds_)
            else:
                ds_ = fw.dsems[fw.dsi]
                fw.dsi = (fw.dsi + 1) % NDS
            toks = fw._deps(reads, writes)
            if ds_[1] > 0:
                toks.append((ds_[0], ds_[1]))
            fw._wait(E, toks)
            if ds_[1] >= SEM_ROT:
                ds_[0] = fw.new_sem()
                ds_[1] = 0
            ds_[1] += 16
            oo = None if out_off is None else bass.IndirectOffsetOnAxis(ap=out_off, axis=0)
            io = None if in_off is None else bass.IndirectOffsetOnAxis(ap=in_off, axis=0)
            E.h.indirect_dma_start(out=out, out_offset=oo, in_=in_, in_offset=io, **kw).then_inc(ds_[0], 16)
            tok = (ds_[0], ds_[1])
            k = id(ds_[0])
            for t in fw._flat(reads):
                t.r[k] = tok
            for t in fw._flat(writes):
                t.w = tok
                t.r = {}

        def p5(l, x_dst, r_xdst):
            with contextlib.ExitStack() as es:
                fw.es = es
                fw.barrier()
                fw.cut_on = True
                scb = fw.sb([128, D], F32, "scb")
                shb = fw.sb([128, D], F32, "shb")
                g2b = fw.sb([128, D], F32, "g2b")
                lng = fw.sb([128, D], F32, "lng5")
                lnb = fw.sb([128, D], F32, "lnb5")
                rbb = fw.sb([128, NE], F32, "rbb")
                rw32 = fw.sb([128, 8, NE], F32, "rw32")
                epst = fw.sb([128, 1], F32, "epst5")
                trisb = fw.sb([128, 128], BF16, "trisb")
                iotp = fw.sb([128, 1], F32, "iotp")
                jv = fw.sb([128, max(NOV, 1)], F32, "jv")
                r_c = Reg()
                fw.dma(SY, shb[:], ada_d[l:l + 1, 3 * D:4 * D].partition_broadcast(128), writes=[r_c])
                fw.dma(SY, scb[:], ada_d[l:l + 1, 4 * D:5 * D].partition_broadcast(128), writes=[r_c])
                fw.dma(SY, g2b[:], ada_d[l:l + 1, 5 * D:6 * D].partition_broadcast(128), writes=[r_c])
                fw.dma(SY, lng[:], ln_g[l, 1:2, :].partition_broadcast(128), writes=[r_c])
                fw.dma(SY, lnb[:], ln_b[l, 1:2, :].partition_broadcast(128), writes=[r_c])
                fw.dma(SY, rbb[:], router_bias[0:1, :].partition_broadcast(128), writes=[r_c])
                fw.dma(SY, rw32[:], router_w.rearrange("(kt p) e -> p kt e", p=128), writes=[r_c])
                fw.dma(SY, trisb[:], tris_bf[:, :], writes=[r_c])
                fw.dma(SY, iotp[:], iota_p[:, :], writes=[r_c])
                fw.dma(SY, jv[:], iota_f[:, 0:max(NOV, 1)], writes=[r_c])
                fw.op(V, lambda e: e.tensor_scalar(out=scb[:], in0=scb[:], scalar1=1.0, scalar2=None, op0=ALU.add), reads=[r_c], writes=[r_c])
                jv0 = fw.sb([128, NE], F32, "jv0")
                fw.dma(SY, jv0[:], iota_f[:, 0:NE], writes=[r_c])
                fw.op(V, lambda e: e.tensor_scalar(out=jv[:], in0=jv[:], scalar1=128.0, scalar2=None, op0=ALU.mult), reads=[r_c], writes=[r_c])
                fw.op(V, lambda e: e.memset(epst[:], LN_EPS), writes=[r_c])
                Gd = fw.sb([128, NTB, NE], F32, "Gd")
                Mk = fw.sb([128, NTB, NE], F32, "Mk")
                Mb = fw.sb([128, NTB, NE], BF16, "Mb")
                r_G = Reg()
                idx2 = fw.sb([128, NTB, 2], I32, "idx2")
                w2t = fw.sb([128, NTB, 2], F32, "w2t")
                idxw = fw.sb([128, max(NOV, 1)], I32, "idxw")
                idxr = fw.sb([128, max(NOV, 1)], I32, "idxr")
                r_idx = Reg()
                stats = fw.sb([128, 2, 6], F32, "stats5")
                mv = fw.sb([128, 2], F32, "mv5")
                rstd = fw.sb([128, 1], F32, "rstd5")
                r_st = Reg()
                with contextlib.ExitStack() as esA:
                    fw.es = esA
                    ps_tp = Rot([fw.ps(esA, "p5tp", [128, 8, 128], F32) for i in range(1)])
                    ps_lg = Rot([fw.ps(esA, "p5lg", [128, NE], F32) for i in range(1)])
                    ps_h = Rot([fw.ps(esA, "p5h", [128, 512], F32) for i in range(2)])
                    ps_y = Rot([fw.ps(esA, "p5y", [128, 512], F32) for i in range(2)])
                    ws1 = fw.sb([128, 8, DE], BF16, "ws1")
                    ws3 = fw.sb([128, 8, DE], BF16, "ws3")
                    ws2 = fw.sb([128, 2, D], BF16, "ws2")
                    r_ws = Reg()
                    import os as _os
                    if "sh" not in _os.environ.get("KSKIP", ""):
                        fw.dma(P, ws1[:], shared_w1[l].rearrange("(kt p) n -> p kt n", p=128), writes=[r_ws])
                        fw.dma(P, ws3[:], shared_w3[l].rearrange("(kt p) n -> p kt n", p=128), writes=[r_ws])
                        fw.dma(P, ws2[:], shared_w2[l].rearrange("(kt p) n -> p kt n", p=128), writes=[r_ws])
                    xts = Rot([fw.sb([128, D], F32, "xt5") for _ in range(3)])
                    h2fs = Rot([fw.sb([128, D], F32, "h2f") for _ in range(3)])
                    rows = Rot([fw.sb([128, RW], BF16, "rowb") for _ in range(2)])
                    h32s = Rot([fw.sb([128, 8, 128], F32, "h32") for _ in range(2)])
                    h2Ts = Rot([fw.sb([128, 8, 512], BF16, "h2Tg") for _ in range(2)])
                    sgs = Rot([fw.sb([128, 512], F32, "sg5") for _ in range(2)])
                    aTs = Rot([fw.sb([128, 2, 512], BF16, "aT5") for _ in range(2)])
                    yshs = Rot([fw.sb([128, D], F32, "ysh") for _ in range(2)])
                    rt = {k: fw.sb([128, NE], F32, "rt" + k) for k in ("s", "sel", "eq", "sel2", "ge2", "ws")}
                    r8 = {k: fw.sb([128, 8], F32, "r8" + k) for k in ("m1", "m2", "gs", "gsel")}
                    r1 = {k: fw.sb([128, 1], F32, "r1" + k) for k in ("gmax", "den")}
                    r_rt = Reg()
                    grp5 = {}
                    blk5 = {}

                    def A_S1(tbg):
                        tg, tb = divmod(tbg, 4)
                        if tb == 0:
                            grp5[tg] = h2Ts.next()
                        h2T, r_h2T = grp5[tg]
                        row0 = tbg * 128
                        xt, r_xt = xts.next()
                        fw.dma(SY, xt[:], x1_d[row0:row0 + 128, :], writes=[r_xt])
                        layer_norm_stats(xt, r_xt, stats, mv, rstd, r_st, epst, r_c)
                        h2f, r_h2f = h2fs.next()
                        fw.op(V, lambda e, h2f=h2f, xt=xt: e.tensor_scalar(out=h2f[:], in0=xt[:], scalar1=mv[:, 0:1], scalar2=rstd[:], op0=ALU.subtract, op1=ALU.mult), reads=[r_xt, r_st], writes=[r_h2f])
                        fw.op(V, lambda e, h2f=h2f: e.tensor_tensor(out=h2f[:], in0=h2f[:], in1=scb[:], op=ALU.mult), reads=[r_h2f, r_c], writes=[r_h2f])
                        fw.op(V, lambda e, h2f=h2f: e.tensor_tensor(out=h2f[:], in0=h2f[:], in1=shb[:], op=ALU.add), reads=[r_h2f, r_c], writes=[r_h2f])
                        rowb, r_rowb = rows.next()
                        fw.op(A, lambda e, rowb=rowb, h2f=h2f: e.activation(out=rowb[:, 0:D], in_=h2f[:], func=AF.Identity), reads=[r_h2f], writes=[r_rowb])
                        if "h2b" not in _os.environ.get("KSKIP", ""):
                            fw.op(P, lambda e, rowb=rowb: e.memset(rowb[:, D:RW], 0.0), writes=[r_rowb])
                            fw.dma(P, h2b_d[row0:row0 + 128, :], rowb[:], reads=[r_rowb], writes=[Reg()])
                        blk5["a", tbg] = (h2f, r_h2f)

                    def A_S1b(tbg):
                        tg, tb = divmod(tbg, 4)
                        h2T, r_h2T = grp5[tg]
                        h2f, r_h2f = blk5.pop(("a", tbg))
                        tp, r_tp = ps_tp.next()
                        for kt in range(8):
                            fw.op(T, lambda e, kt=kt, tp=tp, h2f=h2f: e.transpose(out=tp[:, kt, :], in_=h2f[:, kt * 128:(kt + 1) * 128], identity=identf[:]), reads=[r_h2f, rc_const], writes=[r_tp])
                        h32, r_h32 = h32s.next()
                        for hb in range(2):
                            fw.op(A, lambda e, tp=tp, h32=h32, hb=hb: e.activation(out=h32[:, hb * 4:hb * 4 + 4, :].rearrange("p a b -> p (a b)"), in_=tp[:, hb * 4:hb * 4 + 4, :].rearrange("p a b -> p (a b)"), func=AF.Identity), reads=[r_tp], writes=[r_h32])
                        fw.op(V, lambda e, h32=h32, h2T=h2T, tb=tb: e.tensor_copy(out=h2T[:, :, tb * 128:(tb + 1) * 128], in_=h32[:]), reads=[r_h32], writes=[r_h2T])
                        blk5[tbg] = (h32, r_h32)

                    def A_S2(tbg):
                        h32, r_h32 = blk5.pop(tbg)
                        lg, r_lg = ps_lg.next()
                        for kt in range(8):
                            fw.op(T, lambda e, kt=kt, lg=lg, h32=h32: e.matmul(lg[:], lhsT=h32[:, kt, :], rhs=rw32[:, kt, :], start=(kt == 0), stop=(kt == 7)), reads=[r_h32, r_c], writes=[r_lg])
                        rr_ = [r_rt]
                        v3 = lambda a: a.rearrange("p (g k) -> p g k", k=4)
                        b3 = lambda a: a.unsqueeze(2).to_broadcast([128, 8, 4])
                        fw.op(A, lambda e, lg=lg: e.activation(out=rt["s"][:], in_=lg[:], func=AF.Sigmoid), reads=[r_lg], writes=rr_)
                        fw.op(V, lambda e: e.tensor_tensor(out=rt["sel"][:], in0=rt["s"][:], in1=rbb[:], op=ALU.add), reads=rr_ + [r_c], writes=rr_)
                        fw.op(V, lambda e: e.tensor_reduce(out=r8["m1"][:], in_=v3(rt["sel"][:]), axis=AX.X, op=ALU.max), reads=rr_, writes=rr_)
                        fw.op(V, lambda e: e.tensor_tensor(out=v3(rt["eq"][:]), in0=v3(rt["sel"][:]), in1=b3(r8["m1"][:]), op=ALU.is_equal), reads=rr_, writes=rr_)
                        fw.op(V, lambda e: e.scalar_tensor_tensor(out=rt["sel2"][:], in0=rt["eq"][:], scalar=-1.0e9, in1=rt["sel"][:], op0=ALU.mult, op1=ALU.add), reads=rr_, writes=rr_)
                        fw.op(V, lambda e: e.tensor_reduce(out=r8["m2"][:], in_=v3(rt["sel2"][:]), axis=AX.X, op=ALU.max), reads=rr_, writes=rr_)
                        fw.op(V, lambda e: e.tensor_tensor(out=r8["gs"][:], in0=r8["m1"][:], in1=r8["m2"][:], op=ALU.add), reads=rr_, writes=rr_)
                        fw.op(V, lambda e: e.tensor_reduce(out=r1["gmax"][:], in_=r8["gs"][:], axis=AX.X, op=ALU.max), reads=rr_, writes=rr_)
                        fw.op(V, lambda e: e.tensor_scalar(out=r8["gsel"][:], in0=r8["gs"][:], scalar1=r1["gmax"][:], scalar2=None, op0=ALU.is_equal), reads=rr_, writes=rr_)
                        fw.op(V, lambda e: e.tensor_tensor(out=v3(rt["ge2"][:]), in0=v3(rt["sel"][:]), in1=b3(r8["m2"][:]), op=ALU.is_ge), reads=rr_, writes=rr_)
                        fw.op(V, lambda e, tbg=tbg: e.tensor_tensor(out=v3(Mk[:, tbg, :]), in0=v3(rt["ge2"][:]), in1=b3(r8["gsel"][:]), op=ALU.mult), reads=rr_, writes=rr_ + [r_G])
                        fw.op(V, lambda e, tbg=tbg: e.tensor_tensor(out=rt["ws"][:], in0=Mk[:, tbg, :], in1=rt["s"][:], op=ALU.mult), reads=rr_ + [r_G], writes=rr_)
                        fw.op(V, lambda e: e.tensor_reduce(out=r1["den"][:], in_=rt["ws"][:], axis=AX.X, op=ALU.add), reads=rr_, writes=rr_)
                        fw.op(V, lambda e: e.reciprocal(out=r1["den"][:], in_=r1["den"][:]), reads=rr_, writes=rr_)
                        fw.op(V, lambda e, tbg=tbg: e.tensor_scalar(out=Gd[:, tbg, :], in0=rt["ws"][:], scalar1=r1["den"][:], scalar2=None, op0=ALU.mult), reads=rr_, writes=[r_G])

                    def A_SH(tg):
                        h2T, r_h2T = grp5.pop(tg)
                        aT, r_aT = aTs.next()
                        import os as _os
                        _sk = _os.environ.get("KSKIP", "")
                        for m in (range(2) if "sh" not in _sk else []):
                            h1, r_h1 = ps_h.next()
                            h3, r_h3 = ps_h.next()
                            for kt in range(8):
                                fw.op(T, lambda e, kt=kt, m=m, h1=h1, h2T=h2T: e.matmul(h1[:], lhsT=ws1[:, kt, m * 128:(m + 1) * 128], rhs=h2T[:, kt, :], start=(kt == 0), stop=(kt == 7)), reads=[r_ws, r_h2T], writes=[r_h1])
                            for kt in range(8):
                                fw.op(T, lambda e, kt=kt, m=m, h3=h3, h2T=h2T: e.matmul(h3[:], lhsT=ws3[:, kt, m * 128:(m + 1) * 128], rhs=h2T[:, kt, :], start=(kt == 0), stop=(kt == 7)), reads=[r_ws, r_h2T], writes=[r_h3])
                            sg, r_sg = sgs.next()
                            fw.op(A, lambda e, sg=sg, h1=h1: e.activation(out=sg[:], in_=h1[:], func=AF.Silu), reads=[r_h1], writes=[r_sg])
                            fw.op(V, lambda e, sg=sg, h3=h3, aT=aT, m=m: e.tensor_tensor(out=aT[:, m, :], in0=h3[:], in1=sg[:], op=ALU.mult), reads=[r_h3, r_sg], writes=[r_aT])
                        for tb in (range(4) if "sh" not in _sk else []):
                            row0 = (tg * 4 + tb) * 128
                            ysh, r_ysh = yshs.next()
                            for hh in range(2):
                                y_ps, r_yps = ps_y.next()
                                for kt in range(2):
                                    fw.op(T, lambda e, kt=kt, hh=hh, tb=tb, y_ps=y_ps, aT=aT: e.matmul(y_ps[:], lhsT=aT[:, kt, tb * 128:(tb + 1) * 128], rhs=ws2[:, kt, hh * 512:(hh + 1) * 512], start=(kt == 0), stop=(kt == 1)), reads=[r_ws, r_aT], writes=[r_yps])
                                if hh == 0:
                                    fw.op(A, lambda e, ysh=ysh, y_ps=y_ps: e.activation(out=ysh[:, 0:512], in_=y_ps[:], func=AF.Identity), reads=[r_yps], writes=[r_ysh])
                                else:
                                    fw.op(V, lambda e, ysh=ysh, y_ps=y_ps: e.tensor_copy(out=ysh[:, 512:1024], in_=y_ps[:]), reads=[r_yps], writes=[r_ysh])
                            fw.dma(P, ysh_d[row0:row0 + 128, :], ysh[:], reads=[r_ysh], writes=[Reg()])

                    for i5 in range(NTB + 2):
                        if i5 < NTB:
                            A_S1(i5)
                        if 0 <= i5 - 1 < NTB:
                            A_S1b(i5 - 1)
                        if 0 <= i5 - 2 < NTB:
                            A_S2(i5 - 2)
                            if (i5 - 2) % 4 == 3:
                                A_SH((i5 - 2) // 4)
                    if "d_G" in dbg_aps and (stop or "").startswith("p5") and stop.endswith(f"_{l}"):
                        fw.dma(SY, dbg_aps["d_G"].rearrange("(a p) e -> p a e", p=128), Gd[:], reads=[r_G], writes=[reg("dbg")])
                    import os
                    if "a2" not in os.environ.get("KSKIP", ""):
                        ps_r = ps_lg
                        ps_c = ps_y
                        rank = fw.sb([128, NTB, NE], F32, "rank")
                        md = fw.sb([128, NTB, NE], F32, "md")
                        oh = fw.sb([128, NTB, NE], F32, "oh")
                        before = fw.sb([128, NE], F32, "before")
                        padf = fw.sb([128, NE], F32, "padf")
                        padi = fw.sb([128, NE], I32, "padi")
                        pend = fw.sb([128, NE], F32, "pend")
                        pst1 = fw.sb([128, NE], F32, "pst1")
                        ones32 = fw.sb([128, NE], F32, "ones32")
                        dmax = fw.sb([128, NTB], F32, "dmax")
                        dsum = fw.sb([128, NTB], F32, "dsum")
                        gsum = fw.sb([128, NTB], F32, "gsum")
                        tmpb = fw.sb([128, NTB], F32, "tmpb")
                        cmpt = fw.sb([128, max(NOV, 1), NE], F32, "cmpt")
                        bef = fw.sb([128, max(NOV, 1)], F32, "bef")
                        emp = fw.sb([128, max(NOV, 1)], F32, "emp")
                        e384 = fw.sb([128, NE], F32, "e384")
                        ovb = fw.sb([128, NE], F32, "ovb")
                        Av = fw.sb([128, NTB, NE], F32, "Av")
                        icv = fw.sb([128, NTB, NE], F32, "icv")
                        r_a2 = Reg()
                        a2 = lambda f, eng=V: fw.op(eng, f, reads=[r_a2, r_G, r_c], writes=[r_a2])
                        a2(lambda e: e.tensor_copy(out=Mb[:], in_=Mk[:]))
                        a2(lambda e: e.memset(before[:], 0.0))
                        a2(lambda e: e.memset(ones32[:], 1.0))
                        for b in range(NTB):
                            pr, r_pr = ps_r.next()
                            pc, r_pc = ps_c.next()
                            fw.op(T, lambda e, b=b, pr=pr: e.matmul(pr[:], lhsT=trisb[:], rhs=Mb[:, b, :], start=True, stop=True), reads=[r_a2, r_c], writes=[r_pr])
                            fw.op(T, lambda e, b=b, pc=pc: e.matmul(pc[:, 0:NE], lhsT=onesb[:], rhs=Mb[:, b, :], start=True, stop=True), reads=[r_a2, rc_const], writes=[r_pc])
                            fw.op(V, lambda e, b=b, pr=pr: e.tensor_tensor(out=rank[:, b, :], in0=pr[:], in1=before[:], op=ALU.add), reads=[r_pr, r_a2], writes=[r_a2])
                            fw.op(V, lambda e, pc=pc: e.tensor_tensor(out=before[:], in0=pc[:, 0:NE], in1=before[:], op=ALU.add), reads=[r_pc, r_a2], writes=[r_a2])
                        a2(lambda e: e.tensor_scalar(out=padf[:], in0=before[:], scalar1=-float(CAP), scalar2=0.0, op0=ALU.add, op1=ALU.max))
                        a2(lambda e: e.tensor_scalar(out=padf[:], in0=padf[:], scalar1=127.0, scalar2=None, op0=ALU.add))
                        a2(lambda e: e.tensor_copy(out=padi[:], in_=padf[:]))
                        a2(lambda e: e.tensor_single_scalar(out=padi[:], in_=padi[:], scalar=7, op=ALU.arith_shift_right))
                        a2(lambda e: e.tensor_single_scalar(out=padi[:], in_=padi[:], scalar=7, op=ALU.logical_shift_left))
                        a2(lambda e: e.tensor_copy(out=padf[:], in_=padi[:]))
                        a2(lambda e: e.tensor_tensor_scan(out=pend[:], data0=ones32[:], data1=padf[:], initial=0.0, op0=ALU.mult, op1=ALU.add))
                        a2(lambda e: e.tensor_tensor(out=pst1[:], in0=pend[:], in1=padf[:], op=ALU.subtract))
                        a2(lambda e: e.tensor_scalar(out=ovb[:], in0=pst1[:], scalar1=float(NSB * 128 - CAP + 1), scalar2=None, op0=ALU.add))
                        a2(lambda e: e.tensor_scalar(out=e384[:], in0=jv0[:, 0:NE], scalar1=float(CAP), scalar2=1.0, op0=ALU.mult, op1=ALU.add))
                        bexp = lambda a: a.unsqueeze(1).to_broadcast([128, NTB, NE])
                        a2(lambda e: e.tensor_tensor(out=Av[:], in0=rank[:], in1=bexp(e384[:]), op=ALU.add))
                        a2(lambda e: e.tensor_tensor(out=md[:], in0=rank[:], in1=bexp(ovb[:]), op=ALU.add))
                        a2(lambda e: e.tensor_scalar(out=icv[:], in0=rank[:], scalar1=float(CAP), scalar2=None, op0=ALU.is_lt))
                        a2(lambda e: e.tensor_tensor(out=Av[:], in0=Av[:], in1=md[:], op=ALU.subtract))
                        a2(lambda e: e.tensor_tensor(out=Av[:], in0=Av[:], in1=icv[:], op=ALU.mult))
                        a2(lambda e: e.tensor_tensor(out=md[:], in0=md[:], in1=Av[:], op=ALU.add))
                        a2(lambda e: e.tensor_tensor(out=md[:], in0=md[:], in1=Mk[:], op=ALU.mult))
                        a2(lambda e: e.tensor_reduce(out=dmax[:], in_=md[:], axis=AX.X, op=ALU.max))
                        a2(lambda e: e.tensor_reduce(out=dsum[:], in_=md[:], axis=AX.X, op=ALU.add))
                        a2(lambda e: e.tensor_reduce(out=gsum[:], in_=Gd[:], axis=AX.X, op=ALU.add))
                        a2(lambda e: e.tensor_tensor(out=oh[:], in0=md[:], in1=dmax[:].unsqueeze(2).to_broadcast([128, NTB, NE]), op=ALU.is_equal))
                        a2(lambda e: e.tensor_tensor(out=oh[:], in0=oh[:], in1=Gd[:], op=ALU.mult))
                        a2(lambda e: e.tensor_reduce(out=w2t[:, :, 0], in_=oh[:], axis=AX.X, op=ALU.add))
                        a2(lambda e: e.tensor_tensor(out=w2t[:, :, 1], in0=gsum[:], in1=w2t[:, :, 0], op=ALU.subtract))
                        a2(lambda e: e.tensor_scalar(out=tmpb[:], in0=dmax[:], scalar1=-1.0, scalar2=None, op0=ALU.add))
                        a2(lambda e: e.tensor_copy(out=idx2[:, :, 0], in_=tmpb[:]))
                        a2(lambda e: e.tensor_tensor(out=tmpb[:], in0=dsum[:], in1=dmax[:], op=ALU.subtract))
                        a2(lambda e: e.tensor_scalar(out=tmpb[:], in0=tmpb[:], scalar1=-1.0, scalar2=None, op0=ALU.add))
                        a2(lambda e: e.tensor_copy(out=idx2[:, :, 1], in_=tmpb[:]))
                        NOV1 = max(NOV, 1)
                        a2(lambda e: e.tensor_tensor(out=cmpt[:], in0=pend[:].unsqueeze(1).to_broadcast([128, NOV1, NE]), in1=jv[:].unsqueeze(2).to_broadcast([128, NOV1, NE]), op=ALU.is_le))
                        a2(lambda e: e.tensor_reduce(out=bef[:], in_=cmpt[:], axis=AX.X, op=ALU.add))
                        a2(lambda e: e.tensor_scalar(out=emp[:], in0=jv[:], scalar1=pend[:, NE - 1:NE], scalar2=float(1 << 22), op0=ALU.is_ge, op1=ALU.mult))
                        a2(lambda e: e.tensor_scalar(out=bef[:], in0=bef[:], scalar1=float(NE - 1), scalar2=128.0, op0=ALU.min, op1=ALU.mult))
                        a2(lambda e: e.tensor_scalar(out=bef[:], in0=bef[:], scalar1=iotp[:, 0:1], scalar2=float(l * NE * 128), op0=ALU.add, op1=ALU.add))
                        a2(lambda e: e.tensor_tensor(out=bef[:], in0=bef[:], in1=emp[:], op=ALU.add))
                        fw.op(V, lambda e: e.tensor_copy(out=idxw[:], in_=bef[:]), reads=[r_a2], writes=[r_idx])
                        a2(lambda e: e.tensor_scalar(out=bef[:], in0=jv[:], scalar1=iotp[:, 0:1], scalar2=float(NSB * 128), op0=ALU.add, op1=ALU.add))
                        a2(lambda e: e.tensor_tensor(out=bef[:], in0=bef[:], in1=emp[:], op=ALU.add))
                        fw.op(V, lambda e: e.tensor_copy(out=idxr[:], in_=bef[:]), reads=[r_a2], writes=[r_idx])
                        fw.op(V, lambda e: e.tensor_copy(out=w2t[:, 0:1, 0:1], in_=w2t[:, 0:1, 0:1]), reads=[r_a2], writes=[r_idx])
                        if "d_idx" in dbg_aps and (stop or "").startswith("p5") and stop.endswith(f"_{l}"):
                            fw.dma(SY, dbg_aps["d_idx"].rearrange("(a p) k -> p a k", p=128), idx2[:], reads=[r_idx], writes=[reg("dbg")])
                            fw.dma(SY, dbg_aps["d_idxw"][:, :], idxw[:], reads=[r_idx], writes=[reg("dbg")])
                p5_stages = {"a": 0, "s": 1, "b": 2}.get(stop[2] if (stop or "").startswith("p5") and len(stop) > 3 and stop[2] in "asb" else "", 3)
                with contextlib.ExitStack() as esS:
                  if p5_stages >= 1:
                        fw.es = esS
                        fw.barrier()
                        rbs = Rot([fw.sb([128, RW], BF16, "rbs") for _ in range(4)])
                        for b in range(NTB):
                            for k2 in range(2):
                                rb, r_rb = rbs.next()
                                fw.dma(SY, rb[:], h2b_d[b * 128:(b + 1) * 128, :], writes=[r_rb])
                                fw.op(V, lambda e, rb=rb, b=b, k2=k2: e.tensor_copy(out=rb[:, D:D + 2].bitcast(F32), in_=w2t[:, b, k2:k2 + 1]), reads=[r_rb, r_idx], writes=[r_rb])
                                idma(xs_d[:, :], idx2[:, b, k2:k2 + 1], rb[:, :], None, reads=[r_rb, r_idx], writes=[Reg()])
                with contextlib.ExitStack() as esB:
                  if p5_stages >= 2:
                        fw.es = esB
                        fw.barrier()
                        ps_tp = Rot([fw.ps(esB, "p5btp", [128, 8, 128], BF16) for i in range(2)])
                        ps_h13 = Rot([fw.ps(esB, "p5bh", [128, 512], F32) for i in range(2)])
                        ps_at = Rot([fw.ps(esB, "p5bat", [128, 2, 128], BF16) for i in range(2)])
                        ps_y = Rot([fw.ps(esB, "p5by", [128, 512], F32) for i in range(2)])
                        NW = 4
                        w1bs = Rot([fw.sb([128, 8, DE], BF16, "w1b") for _ in range(NW)])
                        w3bs = Rot([fw.sb([128, 8, DE], BF16, "w3b") for _ in range(NW)])
                        w2bs = Rot([fw.sb([128, 2, D], BF16, "w2b") for _ in range(NW)])
                        xsbs = Rot([fw.sb([128, RW], BF16, "xsb") for _ in range(4)])
                        xsTs = Rot([fw.sb([128, 8, 128], BF16, "xsT") for _ in range(3)])
                        sgs = Rot([fw.sb([128, DE], F32, "sgb") for _ in range(2)])
                        abs_ = Rot([fw.sb([128, DE], BF16, "ab") for _ in range(3)])
                        aTs = Rot([fw.sb([128, 2, 128], BF16, "aTb") for _ in range(3)])
                        ysbs = Rot([fw.sb([128, D], BF16, "ysb") for _ in range(3)])
                        w1v = exp_w1p.rearrange("l e p n -> (l e p) n")
                        w3v = exp_w3p.rearrange("l e p n -> (l e p) n")
                        w2v = exp_w2p.rearrange("l e p n -> (l e p) n")
                        WMAX = fw.E[P].h.alloc_register()
                        fw.E[P].h.reg_mov(WMAX, L * NE * 128 - 1)
                        RMAX = fw.E[P].h.alloc_register()
                        fw.E[P].h.reg_mov(RMAX, NROW - 1)
                        sched = []
                        for ex in range(NE):
                            for k3 in range(CAPB):
                                sched.append(dict(j=ex * CAPB + k3, ex=ex, first=(k3 == 0), ov=None))
                        for jo in range(NOV):
                            sched.append(dict(j=NSB + jo, ex=None, first=True, ov=jo))
                        cur_w = [None]

                        def S1(b):
                            if b["first"]:
                                (w1b, rw1), (w3b, rw3), (w2b, rw2) = w1bs.next(), w3bs.next(), w2bs.next()
                                f1 = lambda t: t[:].rearrange("p a b -> p (a b)")
                                if b["ov"] is None:
                                    ex = b["ex"]
                                    fw.dma(P, f1(w1b), exp_w1p[l, ex], writes=[rw1])
                                    fw.dma(P, f1(w3b), exp_w3p[l, ex], writes=[rw3])
                                    fw.dma(P, f1(w2b), exp_w2p[l, ex], writes=[rw2])
                                else:
                                    jo = b["ov"]
                                    idma(f1(w1b), None, w1v, idxw[:, jo:jo + 1], reads=[r_idx], writes=[rw1], bounds_check=WMAX, oob_is_err=False)
                                    idma(f1(w3b), None, w3v, idxw[:, jo:jo + 1], reads=[r_idx], writes=[rw3], bounds_check=WMAX, oob_is_err=False)
                                    idma(f1(w2b), None, w2v, idxw[:, jo:jo + 1], reads=[r_idx], writes=[rw2], bounds_check=WMAX, oob_is_err=False)
                                cur_w[0] = (w1b, rw1, w3b, rw3, w2b, rw2)
                            b["w"] = cur_w[0]
                            j = b["j"]
                            xsb, r_xsb = xsbs.next()
                            if b["ov"] is None:
                                fw.dma(SY, xsb[:], xs_d[j * 128:(j + 1) * 128, :], writes=[r_xsb])
                            else:
                                idma(xsb[:, :], None, xs_d[:, :], idxr[:, b["ov"]:b["ov"] + 1], reads=[r_idx], writes=[r_xsb], bounds_check=RMAX, oob_is_err=False)
                            tp, r_tp = ps_tp.next()
                            for kt in range(8):
                                fw.op(T, lambda e, kt=kt: e.transpose(out=tp[:, kt, :], in_=xsb[:, kt * 128:(kt + 1) * 128], identity=identb[:]), reads=[r_xsb, rc_const], writes=[r_tp])
                            xsT, r_xsT = xsTs.next()
                            fw.op(A, lambda e: e.activation(out=xsT[:, 0:4, :], in_=tp[:, 0:4, :], func=AF.Identity), reads=[r_tp], writes=[r_xsT])
                            fw.op(V, lambda e: e.tensor_copy(out=xsT[:, 4:8, :], in_=tp[:, 4:8, :]), reads=[r_tp], writes=[r_xsT])
                            b.update(xsb=xsb, r_xsb=r_xsb, xsT=xsT, r_xsT=r_xsT)

                        def S2(b):
                            w1b, rw1, w3b, rw3, w2b, rw2 = b["w"]
                            xsT, r_xsT, xsb, r_xsb = b["xsT"], b["r_xsT"], b["xsb"], b["r_xsb"]
                            h13, r_h13 = ps_h13.next()
                            for kt in range(8):
                                fw.op(T, lambda e, kt=kt: e.matmul(h13[:, 0:DE], lhsT=xsT[:, kt, :], rhs=w1b[:, kt, :], start=(kt == 0), stop=(kt == 7), skip_group_check=True), reads=[r_xsT, rw1], writes=[r_h13])
                            for kt in range(8):
                                fw.op(T, lambda e, kt=kt: e.matmul(h13[:, DE:2 * DE], lhsT=xsT[:, kt, :], rhs=w3b[:, kt, :], start=False, stop=(kt == 7), skip_group_check=True), reads=[r_xsT, rw3], writes=[r_h13])
                            sg, r_sg = sgs.next()
                            fw.op(A, lambda e: e.activation(out=sg[:], in_=h13[:, 0:DE], func=AF.Silu), reads=[r_h13], writes=[r_sg])
                            ab, r_ab = abs_.next()
                            fw.op(V, lambda e: e.scalar_tensor_tensor(out=ab[:], in0=h13[:, DE:2 * DE], scalar=xsb[:, D:D + 2].bitcast(F32), in1=sg[:], op0=ALU.mult, op1=ALU.mult), reads=[r_h13, r_xsb, r_sg], writes=[r_ab])
                            b.update(ab=ab, r_ab=r_ab)

                        def S3(b):
                            ab, r_ab = b["ab"], b["r_ab"]
                            at, r_at = ps_at.next()
                            for kt in range(2):
                                fw.op(T, lambda e, kt=kt: e.transpose(out=at[:, kt, :], in_=ab[:, kt * 128:(kt + 1) * 128], identity=identb[:]), reads=[r_ab, rc_const], writes=[r_at])
                            aT, r_aT = aTs.next()
                            fw.op(V, lambda e: e.tensor_copy(out=aT[:], in_=at[:]), reads=[r_at], writes=[r_aT])
                            b.update(aT=aT, r_aT=r_aT)

                        def S4(b):
                            w1b, rw1, w3b, rw3, w2b, rw2 = b["w"]
                            aT, r_aT = b["aT"], b["r_aT"]
                            j = b["j"]
                            ysb, r_ysb = ysbs.next()
                            for hh in range(2):
                                y_ps, r_yps = ps_y.next()
                                for kt in range(2):
                                    fw.op(T, lambda e, kt=kt, hh=hh, y_ps=y_ps: e.matmul(y_ps[:], lhsT=aT[:, kt, :], rhs=w2b[:, kt, hh * 512:(hh + 1) * 512], start=(kt == 0), stop=(kt == 1)), reads=[r_aT, rw2], writes=[r_yps])
                                if hh == 0:
                                    fw.op(A, lambda e, y_ps=y_ps: e.activation(out=ysb[:, 0:512], in_=y_ps[:], func=AF.Identity), reads=[r_yps], writes=[r_ysb])
                                else:
                                    fw.op(V, lambda e, y_ps=y_ps: e.tensor_copy(out=ysb[:, 512:1024], in_=y_ps[:]), reads=[r_yps], writes=[r_ysb])
                            if b["ov"] is None:
                                fw.dma(A, ys_d[j * 128:(j + 1) * 128, :], ysb[:], reads=[r_ysb], writes=[Reg()])
                            else:
                                idma(ys_d[:, :], idxr[:, b["ov"]:b["ov"] + 1], ysb[:, :], None, reads=[r_ysb, r_idx], writes=[Reg()], bounds_check=RMAX, oob_is_err=False)
                            b.clear()

                        nb = len(sched)
                        for i in range(nb + 3):
                            if i < nb:
                                S1(sched[i])
                            if 0 <= i - 1 < nb:
                                S2(sched[i - 1])
                            if 0 <= i - 2 < nb:
                                S3(sched[i - 2])
                            if 0 <= i - 3 < nb:
                                S4(sched[i - 3])
                        fw.E[P].h.free_register(WMAX)
                        fw.E[P].h.free_register(RMAX)
                with contextlib.ExitStack() as esC:
                  if p5_stages >= 3:
                        fw.es = esC
                        fw.barrier()
                        xts = Rot([fw.sb([128, D], F32, "xt5c") for _ in range(2)])
                        g1s = Rot([fw.sb([128, D], BF16, "g1c") for _ in range(3)])
                        g2s = Rot([fw.sb([128, D], BF16, "g2c") for _ in range(3)])
                        accs = Rot([fw.sb([128, D], F32, "acc5c") for _ in range(2)])
                        yss = Rot([fw.sb([128, D], F32, "ysc") for _ in range(2)])
                        xos = Rot([fw.sb([128, D], F32, "xo5") for _ in range(2)])
                        for b in range(NTB):
                            row0 = b * 128
                            xt, r_xt = xts.next()
                            ga, r_ga = g1s.next()
                            gb_, r_gb = g2s.next()
                            ys_, r_ys = yss.next()
                            fw.dma(SY, xt[:], x1_d[row0:row0 + 128, :], writes=[r_xt])
                            fw.dma(SY, ys_[:], ysh_d[row0:row0 + 128, :], writes=[r_ys])
                            idma(ga[:, :], None, ys_d[:, :], idx2[:, b, 0:1], reads=[r_idx], writes=[r_ga])
                            idma(gb_[:, :], None, ys_d[:, :], idx2[:, b, 1:2], reads=[r_idx], writes=[r_gb])
                            acc, r_acc = accs.next()
                            fw.op(V, lambda e, acc=acc, ga=ga, ys_=ys_: e.tensor_tensor(out=acc[:], in0=ga[:], in1=ys_[:], op=ALU.add), reads=[r_ga, r_ys], writes=[r_acc])
                            fw.op(V, lambda e, acc=acc, gb_=gb_: e.tensor_tensor(out=acc[:], in0=acc[:], in1=gb_[:], op=ALU.add), reads=[r_acc, r_gb], writes=[r_acc])
                            fw.op(V, lambda e, acc=acc: e.tensor_tensor(out=acc[:], in0=acc[:], in1=g2b[:], op=ALU.mult), reads=[r_acc, r_c], writes=[r_acc])
                            fw.op(V, lambda e, acc=acc, xt=xt: e.scalar_tensor_tensor(out=acc[:], in0=xt[:], scalar=ALPHA, in1=acc[:], op0=ALU.mult, op1=ALU.add), reads=[r_xt, r_acc], writes=[r_acc])
                            xo, r_xo = xos.next()
                            final_ln(acc, r_acc, stats, mv, rstd, r_st, epst, r_c, lng, lnb, xo, r_xo, eng2=V)
                            fw.dma(A, x_dst[row0:row0 + 128, :], xo[:], reads=[r_xo], writes=[Reg()])
                fw.es = es
            fw.es = es0

        r_xin = Reg()
        for l in range(L):
            x_src, r_xsrc = (x_in, r_xin) if l == 0 else (x2_d, reg("x2"))
            p1(l, x_src, r_xsrc)
            if stop == f"p1_{l}":
                fw.barrier()
                for nm, src in (("hT", hT_d), ("zc", zc_d), ("u", u_d), ("cq", cq_d), ("ckv", ckv_d), ("kr", kr_d)):
                    if "d_" + nm in dbg_aps:
                        fw.dma(SY, dbg_aps["d_" + nm][:, :], src[:, :], reads=[reg(nm)], writes=[reg("dbg")])
                fw.finish(list(R.values()))
                return nc

            p2(l)
            if stop == f"p2_{l}":
                fw.barrier()
                if "d_zs" in dbg_aps:
                    fw.dma(SY, dbg_aps["d_zs"][:, :], zs_d[:, :], reads=[reg("zs")], writes=[reg("dbg")])
                fw.finish(list(R.values()))
                return nc
            p3(l)
            if stop == f"p3_{l}":
                fw.barrier()
                if "d_oT" in dbg_aps:
                    fw.dma(SY, dbg_aps["d_oT"][:, :], oT_d[:, :], reads=[reg("oT")], writes=[reg("dbg")])
                fw.finish(list(R.values()))
                return nc
            p4(l, x_src, r_xsrc)
            if stop == f"p4_{l}":
                fw.barrier()
                if "d_x1" in dbg_aps:
                    fw.dma(SY, dbg_aps["d_x1"][:, :], x1_d[:, :], reads=[reg("x1")], writes=[reg("dbg")])
                fw.finish(list(R.values()))
                return nc
            last = (l == L - 1)
            p5(l, out_d if last else x2_d, reg("out") if last else reg("x2"))
            fw.cut_on = False
            if stop in (f"p5_{l}", f"p5a_{l}", f"p5s_{l}", f"p5b_{l}"):
                fw.barrier()
                if "d_x2" in dbg_aps:
                    fw.dma(SY, dbg_aps["d_x2"][:, :], (out_d if last else x2_d)[:, :], reads=[reg("out") if last else reg("x2")], writes=[reg("dbg")])
                fw.finish(list(R.values()))
                return nc
        fw.finish(list(R.values()))
    return nc


def host_consts():
    tri = np.triu(np.ones((128, 128), np.float32))
    inv_freq = (10000.0 ** (-np.arange(0, ROPE, 2, dtype=np.float32) / ROPE)).astype(np.float32)
    ropec = np.zeros((32, 2), np.float32)
    ropec[:, 0] = np.concatenate([inv_freq, inv_freq])
    ropec[:16, 1] = -1.0
    ropec[16:, 1] = 1.0
    return {
        "ident_bf": np.eye(128, dtype=np.float32).astype(ml_dtypes.bfloat16),
        "ident_f": np.eye(128, dtype=np.float32),
        "ones_bf": np.ones((128, 128), np.float32).astype(ml_dtypes.bfloat16),
        "tri_bf": tri.astype(ml_dtypes.bfloat16),
        "ropec": ropec,
        "iota_f": np.tile(np.arange(512, dtype=np.float32)[None, :], (128, 1)),
        "iota_p": np.arange(128, dtype=np.float32).reshape(128, 1),
        "tris_bf": np.triu(np.ones((128, 128), np.float32), 1).astype(ml_dtypes.bfloat16),
    }


def prep_inputs(inp, S):
    f = lambda a: np.ascontiguousarray(np.asarray(a))
    w_in = f(inp["w_in"])
    kr0 = IN_MIX - ROPE
    w_in_a = np.concatenate([w_in[:, :, :IN_MIX], w_in[:, :, kr0 + 16:kr0 + 32], w_in[:, :, kr0:kr0 + 16]], axis=2)
    w_in_g = w_in[:, :, IN_MIX:]
    w_uq = f(inp["w_uq"])
    Lw = w_uq.shape[0]
    wq4 = w_uq.reshape(Lw, QL, H, DK)
    wsw = np.zeros_like(wq4)
    wsw[..., NOPE:NOPE + 16] = wq4[..., NOPE + 16:NOPE + 32]
    wsw[..., NOPE + 16:NOPE + 32] = wq4[..., NOPE:NOPE + 16]
    shared = {
        "w_in_a": f(w_in_a), "w_in_g": f(w_in_g), "w_uq_sw": f(wsw.reshape(Lw, QL, H * DK)),
        "router_bias": f(inp["router_bias"]).reshape(1, NE),
    }
    for k in ("w_ada", "b_ada", "conv_w", "ssm_a_re", "ssm_a_im", "ssm_b_re", "ssm_b_im", "ssm_c_re", "ssm_c_im", "ssm_d",
              "ssm_log_dt", "ssm_w_glu", "q_norm", "w_uq", "kv_norm", "w_uk", "w_uv", "w_up_conv", "w_up_ssm", "w_up_attn",
              "w_o", "ln_g", "ln_b", "router_w", "shared_w1", "shared_w3", "shared_w2"):
        shared[k] = f(inp[k])
    Le = np.asarray(inp["exp_w1"]).shape[0]
    shared["exp_w1p"] = f(np.asarray(inp["exp_w1"]).reshape(Le, NE, 8, 128, DE).transpose(0, 1, 3, 2, 4).reshape(Le, NE, 128, 8 * DE))
    shared["exp_w3p"] = f(np.asarray(inp["exp_w3"]).reshape(Le, NE, 8, 128, DE).transpose(0, 1, 3, 2, 4).reshape(Le, NE, 128, 8 * DE))
    shared["exp_w2p"] = f(np.asarray(inp["exp_w2"]).reshape(Le, NE, 2, 128, D).transpose(0, 1, 3, 2, 4).reshape(Le, NE, 128, 2 * D))
    shared.update(host_consts())
    x = f(inp["x"])
    c = f(inp["c"])
    pos = f(inp["positions"]).astype(np.int32)
    B = x.shape[0]
    maps = []
    for b in range(B):
        m = dict(shared)
        m["x"] = f(x[b, :S])
        m["c"] = f(c[b:b + 1])
        m["positions"] = f(pos[b:b + 1, :S])
        maps.append(m)
    return maps


def kernel(**inputs):
    S = inputs["x"].shape[1]
    nc = build_program(S)
    maps = prep_inputs(inputs, S)
    res = run_bass_kernel_spmd(nc, maps, core_ids=list(range(len(maps))))
    return np.stack([np.asarray(r["out"]) for r in res.results], axis=0).astype(np.float32)
```

```python
import contextlib
import math
import numpy as np
import ml_dtypes
import concourse.bass as bass
import concourse.mybir as mybir
from concourse.bass_utils import run_bass_kernel_spmd

F32 = mybir.dt.float32
BF16 = mybir.dt.bfloat16
I32 = mybir.dt.int32
AF = mybir.ActivationFunctionType
ALU = mybir.AluOpType
AX = mybir.AxisListType

D = 1024
DEPTH = 2
DC = 512
G = 32
NST = 64
GD = 16
H = 8
NOPE = 64
ROPE = 32
DK = 96
DV = 64
QL = 256
KVL = 128
IN_MIX = 3 * DC + DC + QL + KVL + ROPE
NA = IN_MIX + ROPE
NE = 32
DE = 256
ALPHA = (2 * DEPTH) ** 0.25
LN_EPS = 1e-5
RMS_EPS = 1e-6
TWO_PI = 2.0 * math.pi

SEM_ROT = 12000
NDS = 40
SAME_ENG_SYNC = True
KSIM = False


class Reg:
    __slots__ = ("w", "r")

    def __init__(self):
        self.w = None
        self.r = {}


class Eng:
    def __init__(self, fw, name, h):
        self.name = name
        self.h = h
        self.sem = fw.new_sem()
        self.own = {id(self.sem)}
        self.cnt = 0
        self.seen = {}


class FW:
    def __init__(self, nc, es):
        self.nc = nc
        self.es = es
        self.nsem = 0
        self.E = {n: Eng(self, n, getattr(nc, n)) for n in ("tensor", "vector", "scalar", "gpsimd", "sync")}
        self.dsems = [[self.new_sem(), 0] for _ in range(NDS)]
        self.dsi = 0
        self.ntile = 0

    def new_sem(self):
        s = self.es.enter_context(self.nc.semaphore(f"sm{self.nsem}"))
        self.nsem += 1
        return s

    def sb(self, shape, dt, name=None):
        self.ntile += 1
        return self.es.enter_context(self.nc.sbuf_tensor(f"{name or 't'}{self.ntile}", list(shape), dt))

    def ps(self, es, name, shape, dt):
        self.ntile += 1
        return es.enter_context(self.nc.psum_tensor(f"{name}_{self.ntile}", list(shape), dt))

    @staticmethod
    def _flat(regs):
        out = []
        for t in regs:
            if isinstance(t, RegList):
                out.extend(t)
            else:
                out.append(t)
        return out

    def _deps(self, reads, writes):
        toks = []
        reads = self._flat(reads)
        writes = self._flat(writes)
        for t in reads:
            if t.w is not None:
                toks.append(t.w)
        for t in writes:
            if t.w is not None:
                toks.append(t.w)
            toks.extend(t.r.values())
        return toks

    def _wait(self, E, toks):
        need = {}
        for (s, v) in toks:
            k = id(s)
            if k not in need or need[k][1] < v:
                need[k] = (s, v)
        for k, (s, v) in need.items():
            if k in E.own and (E.name == "tensor" or not SAME_ENG_SYNC):
                continue
            if E.seen.get(k, 0) < v:
                E.h.wait_ge(s, v)
                E.seen[k] = v

    def op(self, eng, build, reads=(), writes=()):
        if self.cut():
            return None
        E = self.E[eng]
        self._wait(E, self._deps(reads, writes))
        if E.cnt >= SEM_ROT:
            E.sem = self.new_sem()
            E.own.add(id(E.sem))
            E.cnt = 0
        inst = build(E.h)
        E.cnt += 1
        inst.then_inc(E.sem, 1)
        tok = (E.sem, E.cnt)
        k = id(E.sem)
        for t in self._flat(reads):
            t.r[k] = tok
        for t in self._flat(writes):
            t.w = tok
            t.r = {}
        return inst

    def cut(self):
        import os
        kc = os.environ.get("KCUT")
        if kc is None or not getattr(self, "cut_on", False):
            return False
        self.cut_n = getattr(self, "cut_n", 0) + 1
        return self.cut_n > int(kc)

    def dma(self, q, out, in_, reads=(), writes=(), **kw):
        if self.cut():
            return None
        E = self.E[q]
        if KSIM and q == "gpsimd":
            ds = [self.new_sem(), 0]
            self.dsems.append(ds)
        else:
            ds = self.dsems[self.dsi]
            self.dsi = (self.dsi + 1) % NDS
        toks = self._deps(reads, writes)
        if ds[1] > 0:
            toks.append((ds[0], ds[1]))
        self._wait(E, toks)
        if ds[1] >= SEM_ROT:
            ds[0] = self.new_sem()
            ds[1] = 0
        ds[1] += 16
        E.h.dma_start(out=out, in_=in_, **kw).then_inc(ds[0], 16)
        tok = (ds[0], ds[1])
        k = id(ds[0])
        for t in self._flat(reads):
            t.r[k] = tok
        for t in self._flat(writes):
            t.w = tok
            t.r = {}

    def barrier(self):
        toks = [(E.sem, E.cnt) for E in self.E.values() if E.cnt > 0]
        toks += [(d[0], d[1]) for d in self.dsems if d[1] > 0]
        for E in self.E.values():
            need = [(s_, v) for (s_, v) in toks if id(s_) not in E.own]
            self._wait(E, need)

    def finish(self, regs=None):
        E = self.E["sync"]
        toks = [(e.sem, e.cnt) for e in self.E.values() if e.cnt > 0 and e is not E]
        toks += [(d[0], d[1]) for d in self.dsems if d[1] > 0]
        self._wait(E, toks)


class RegList(list):
    pass


class Rot:
    def __init__(self, tiles):
        self.t = [(a, Reg()) for a in tiles]
        self.i = 0

    def next(self):
        r = self.t[self.i]
        self.i = (self.i + 1) % len(self.t)
        return r


def build_program(S, L=DEPTH, stop=None, dbg=None):
    nc = bass.Bass("TRN2", target_bir_lowering=False)
    NTB = S // 128
    NTG = S // 512
    dbg = dbg or {}

    def din(name, shape, dt=F32):
        return nc.dram_tensor(name, list(shape), dt, kind="ExternalInput").ap()

    def dscr(name, shape, dt):
        return nc.dram_tensor(name, list(shape), dt, kind="Internal").ap()

    x_in = din("x", [S, D])
    c_in = din("c", [1, D])
    pos_in = din("positions", [1, S], I32)
    w_ada = din("w_ada", [L, D, 6 * D])
    b_ada = din("b_ada", [L, 6 * D])
    w_in_a = din("w_in_a", [L, D, NA])
    w_in_g = din("w_in_g", [L, D, 3 * D])
    conv_w = din("conv_w", [L, 3, DC])
    ssm_a_re = din("ssm_a_re", [L, G, NST])
    ssm_a_im = din("ssm_a_im", [L, G, NST])
    ssm_b_re = din("ssm_b_re", [L, G, NST, GD])
    ssm_b_im = din("ssm_b_im", [L, G, NST, GD])
    ssm_c_re = din("ssm_c_re", [L, G, GD, NST])
    ssm_c_im = din("ssm_c_im", [L, G, GD, NST])
    ssm_d = din("ssm_d", [L, DC])
    ssm_log_dt = din("ssm_log_dt", [L, G])
    ssm_w_glu = din("ssm_w_glu", [L, DC, 2 * DC])
    q_norm = din("q_norm", [L, QL])
    w_uq = din("w_uq", [L, QL, H * DK])
    w_uq_sw = din("w_uq_sw", [L, QL, H * DK])
    kv_norm = din("kv_norm", [L, KVL])
    w_uk = din("w_uk", [L, KVL, H * NOPE])
    w_uv = din("w_uv", [L, KVL, H * DV])
    w_up_conv = din("w_up_conv", [L, DC, D])
    w_up_ssm = din("w_up_ssm", [L, DC, D])
    w_up_attn = din("w_up_attn", [L, DC, D])
    w_o = din("w_o", [L, D, D])
    ln_g = din("ln_g", [L, 2, D])
    ln_b = din("ln_b", [L, 2, D])
    router_w = din("router_w", [D, NE])
    router_bias = din("router_bias", [1, NE])
    exp_w1p = din("exp_w1p", [L, NE, 128, 8 * DE])
    exp_w3p = din("exp_w3p", [L, NE, 128, 8 * DE])
    exp_w2p = din("exp_w2p", [L, NE, 128, 2 * D])
    shared_w1 = din("shared_w1", [L, D, DE])
    shared_w3 = din("shared_w3", [L, D, DE])
    shared_w2 = din("shared_w2", [L, DE, D])
    ident_bf = din("ident_bf", [128, 128], BF16)
    ident_f = din("ident_f", [128, 128])
    ones_bf = din("ones_bf", [128, 128], BF16)
    tri_bf = din("tri_bf", [128, 128], BF16)
    iota_f = din("iota_f", [128, 512])
    iota_p = din("iota_p", [128, 1])
    tris_bf = din("tris_bf", [128, 128], BF16)
    ropec = din("ropec", [32, 2])

    out_d = nc.dram_tensor("out", [S, D], F32, kind="ExternalOutput").ap()
    dbg_aps = {k: nc.dram_tensor(k, list(v[0]), v[1], kind="ExternalOutput").ap() for k, v in dbg.items()}

    ada_d = dscr("ada_d", [L, 6 * D], F32)
    cos_d = dscr("cos_d", [32, S], F32)
    sin_d = dscr("sin_d", [32, S], F32)
    hT_d = dscr("hT_d", [D, S], BF16)
    zc_d = dscr("zc_d", [DC, S], BF16)
    u_d = dscr("u_d", [DC, S], BF16)
    cq_d = dscr("cq_d", [QL, S], BF16)
    ckv_d = dscr("ckv_d", [KVL, S], BF16)
    kr_d = dscr("kr_d", [32, S], BF16)
    zs_d = dscr("zs_d", [DC, S], BF16)
    oT_d = dscr("oT_d", [DC, S], BF16)
    x1_d = dscr("x1_d", [S, D], F32)
    x2_d = dscr("x2_d", [S, D], F32)
    import os as _os3
    _cb = int(_os3.environ.get("KCAPB", "3"))
    NBLK_ = NE * _cb + (2 * ((S - _cb * 128 + 127) // 128) if S > _cb * 128 else 0)
    h2b_d = dscr("h2b_d", [S, 1032], BF16)
    xs_d = dscr("xs_d", [NBLK_ * 128, 1032], BF16)
    ys_d = dscr("ys_d", [NBLK_ * 128, D], BF16)
    ysh_d = dscr("ysh_d", [S, D], F32)

    R = {}

    def reg(name, i=None):
        key = name if i is None else (name, i)
        if key not in R:
            R[key] = Reg()
        return R[key]

    def regs_all(name):
        return [v for k, v in R.items() if (isinstance(k, tuple) and k[0] == name) or k == name]

    with contextlib.ExitStack() as es0:
        fw = FW(nc, es0)
        V, A, P, T, SY = "vector", "scalar", "gpsimd", "tensor", "sync"

        identb = fw.sb([128, 128], BF16, "identb")
        identf = fw.sb([128, 128], F32, "identf")
        onesb = fw.sb([128, 128], BF16, "onesb")
        trib = fw.sb([128, 128], BF16, "trib")
        rc_const = Reg()
        fw.dma(SY, identb[:], ident_bf[:, :], writes=[rc_const])
        fw.dma(SY, identf[:], ident_f[:, :], writes=[rc_const])
        fw.dma(SY, onesb[:], ones_bf[:, :], writes=[rc_const])
        fw.dma(SY, trib[:], tri_bf[:, :], writes=[rc_const])

        with contextlib.ExitStack() as es:
            fw.es = es
            ps_row = fw.ps(es, "ps_row", [1, 512], F32)
            r_ps_row = Reg()
            ccol = fw.sb([128, 8], F32, "ccol")
            cond = fw.sb([128, 8], F32, "cond")
            r_c = Reg()
            fw.dma(SY, ccol[:], c_in.rearrange("o (kt p) -> p (o kt)", p=128), writes=[r_c], allow_slow_non_contiguous=True)
            fw.op(A, lambda e: e.activation(out=cond[:], in_=ccol[:], func=AF.Silu), reads=[r_c], writes=[r_c])
            wa = Rot([fw.sb([128, 8, 512], F32, "wa") for _ in range(2)])
            arow = fw.sb([1, 6 * D], F32, "arow")
            brow = fw.sb([1, 6 * D], F32, "brow")
            r_arow = Reg()
            r_brow = Reg()
            for l in range(L):
                fw.dma(SY, brow[:], b_ada[l:l + 1, :], writes=[r_brow])
                for n in range(12):
                    wt, rw = wa.next()
                    fw.dma(SY, wt[:], w_ada[l, :, n * 512:(n + 1) * 512].rearrange("(kt p) n -> p kt n", p=128), writes=[rw])
                    for kt in range(8):
                        fw.op(T, lambda e, kt=kt, wt=wt: e.matmul(ps_row[:], lhsT=cond[:, kt:kt + 1], rhs=wt[:, kt, :], start=(kt == 0), stop=(kt == 7)),
                              reads=[r_c, rw], writes=[r_ps_row])
                    fw.op(V, lambda e, n=n: e.tensor_tensor(out=arow[:, n * 512:(n + 1) * 512], in0=ps_row[:], in1=brow[:, n * 512:(n + 1) * 512], op=ALU.add),
                          reads=[r_ps_row, r_brow], writes=[r_arow])
                fw.dma(SY, ada_d[l:l + 1, :], arow[:], reads=[r_arow], writes=[reg("ada")])

            posi = fw.sb([32, S], I32, "posi")
            ang = fw.sb([32, S], F32, "ang")
            kf = fw.sb([32, S], F32, "kf")
            ki = fw.sb([32, S], I32, "ki")
            red = fw.sb([32, S], F32, "red")
            tab = fw.sb([32, S], F32, "tab")
            rcs = fw.sb([32, 2], F32, "rcs")
            r_rcs = Reg()
            r_pos = Reg()
            r_ang = Reg()
            r_kf = Reg()
            r_ki = Reg()
            r_red = Reg()
            r_tab = Reg()
            fw.dma(SY, rcs[:], ropec[:, :], writes=[r_rcs])
            fw.dma(SY, posi[:], pos_in.partition_broadcast(32), writes=[r_pos])
            fw.op(V, lambda e: e.tensor_copy(out=ang[:], in_=posi[:]), reads=[r_pos], writes=[r_ang])
            fw.op(V, lambda e: e.tensor_scalar(out=ang[:], in0=ang[:], scalar1=rcs[:, 0:1], scalar2=None, op0=ALU.mult), reads=[r_ang, r_rcs], writes=[r_ang])
            fw.op(V, lambda e: e.tensor_scalar(out=kf[:], in0=ang[:], scalar1=1.0 / TWO_PI, scalar2=None, op0=ALU.mult), reads=[r_ang], writes=[r_kf])
            fw.op(V, lambda e: e.tensor_copy(out=ki[:], in_=kf[:]), reads=[r_kf], writes=[r_ki])
            fw.op(V, lambda e: e.tensor_copy(out=kf[:], in_=ki[:]), reads=[r_ki], writes=[r_kf])
            c1 = float(np.float32(6.28125))
            c2 = float(np.float32(TWO_PI - 6.28125))
            c3 = float(TWO_PI - 6.28125 - float(np.float32(TWO_PI - 6.28125)))
            for cc in (c1, c2, c3):
                fw.op(V, lambda e, cc=cc: e.scalar_tensor_tensor(out=ang[:], in0=kf[:], scalar=-cc, in1=ang[:], op0=ALU.mult, op1=ALU.add), reads=[r_ang, r_kf], writes=[r_ang])

            def wrap_pi(src, shift):
                fw.op(V, lambda e: e.tensor_scalar(out=tab[:], in0=src[:], scalar1=shift, scalar2=None, op0=ALU.add), reads=[r_ang, r_tab], writes=[r_tab])
                for _ in range(2):
                    fw.op(V, lambda e: e.tensor_scalar(out=red[:], in0=tab[:], scalar1=math.pi, scalar2=None, op0=ALU.is_gt), reads=[r_tab], writes=[r_red])
                    fw.op(V, lambda e: e.scalar_tensor_tensor(out=tab[:], in0=red[:], scalar=-TWO_PI, in1=tab[:], op0=ALU.mult, op1=ALU.add), reads=[r_red, r_tab], writes=[r_tab])
                    fw.op(V, lambda e: e.tensor_scalar(out=red[:], in0=tab[:], scalar1=-math.pi, scalar2=None, op0=ALU.is_lt), reads=[r_tab], writes=[r_red])
                    fw.op(V, lambda e: e.scalar_tensor_tensor(out=tab[:], in0=red[:], scalar=TWO_PI, in1=tab[:], op0=ALU.mult, op1=ALU.add), reads=[r_red, r_tab], writes=[r_tab])

            for which, shift, dst in (("sin", 0.0, sin_d), ("cos", math.pi / 2, cos_d)):
                wrap_pi(ang, shift)
                fw.op(A, lambda e: e.activation(out=tab[:], in_=tab[:], func=AF.Sin), reads=[r_tab], writes=[r_tab])
                if which == "sin":
                    fw.op(V, lambda e: e.tensor_scalar(out=tab[:], in0=tab[:], scalar1=rcs[:, 1:2], scalar2=None, op0=ALU.mult), reads=[r_tab, r_rcs], writes=[r_tab])
                fw.dma(SY, dst[:, :], tab[:], reads=[r_tab], writes=[reg("rope")])
            if "d_cos" in dbg_aps:
                pass
        fw.es = es0

        if stop == "p0":
            fw.barrier()
            if "d_ada" in dbg_aps:
                fw.dma(SY, dbg_aps["d_ada"][:, :], ada_d[:, :], reads=[reg("ada")], writes=[reg("dbg")])
            if "d_cos" in dbg_aps:
                fw.dma(SY, dbg_aps["d_cos"][:, :], cos_d[:, :], reads=[reg("rope")], writes=[reg("dbg")])
                fw.dma(SY, dbg_aps["d_sin"][:, :], sin_d[:, :], reads=[reg("rope")], writes=[reg("dbg")])
            fw.finish(list(R.values()))
            return nc


        def mm8(ps_ap, wtile, c0, c1, rhs_tile, regs_r, reg_w, nk=8):
            for kt in range(nk):
                fw.op(T, lambda e, kt=kt: e.matmul(ps_ap, lhsT=wtile[:, kt, c0:c1], rhs=rhs_tile[:, kt, :], start=(kt == 0), stop=(kt == nk - 1)),
                      reads=regs_r, writes=[reg_w])

        def load_cast(dst_tile, r_dst, src_rows_fn, nk, ncols, stg):
            for kt in range(nk):
                rg = Reg()
                r_dst.append(rg)
                fw.dma(P, dst_tile[:, kt, :], src_rows_fn(kt), writes=[rg])

        def layer_norm_stats(xt, r_xt, stats, mv, rstd, r_st, epst, r_eps):
            for hh in range(2):
                fw.op(V, lambda e, hh=hh: e.bn_stats(out=stats[:, hh, :], in_=xt[:, hh * 512:(hh + 1) * 512]), reads=[r_xt], writes=[r_st])
            fw.op(V, lambda e: e.bn_aggr(out=mv[:], in_=stats[:].rearrange("p a b -> p (a b)")), reads=[r_st], writes=[r_st])
            fw.op(A, lambda e: e.activation(out=rstd[:], in_=mv[:, 1:2], func=AF.Sqrt, bias=epst[:], scale=1.0), reads=[r_st, r_eps], writes=[r_st])
            fw.op(V, lambda e: e.reciprocal(out=rstd[:], in_=rstd[:]), reads=[r_st], writes=[r_st])

        def p1(l, x_src, r_xsrc):
            with contextlib.ExitStack() as es:
                fw.es = es
                fw.barrier()
                pst = Rot([fw.ps(es, "p1tp", [128, 8, 128], BF16) for i in range(2)])
                psm = Rot([fw.ps(es, "p1mm", [128, 512], F32) for i in range(5)])
                wA = fw.sb([128, 8, NA], BF16, "wA")
                r_wA = RegList()
                stg = None
                load_cast(wA, r_wA, lambda kt: w_in_a[l, kt * 128:(kt + 1) * 128, :], 8, NA, stg)
                sc = fw.sb([128, 8], F32, "sc")
                sh = fw.sb([128, 8], F32, "sh")
                cw = fw.sb([128, 4, 3], F32, "cw")
                gq = fw.sb([128, 2], F32, "gq")
                gkv = fw.sb([128, 1], F32, "gkv")
                epst = fw.sb([128, 1], F32, "epst")
                epsq = fw.sb([128, 1], F32, "epsq")
                r_small = Reg()
                fw.dma(SY, sh[:], ada_d[l, 0:D].rearrange("(kt p) -> p kt", p=128), reads=[reg("ada")], writes=[r_small], allow_slow_non_contiguous=True)
                fw.dma(SY, sc[:], ada_d[l, D:2 * D].rearrange("(kt p) -> p kt", p=128), reads=[reg("ada")], writes=[r_small], allow_slow_non_contiguous=True)
                for k3 in range(3):
                    fw.dma(SY, cw[:, :, k3], conv_w[l, k3].rearrange("(j p) -> p j", p=128), writes=[r_small], allow_slow_non_contiguous=True)
                fw.dma(SY, gq[:], q_norm[l].rearrange("(j p) -> p j", p=128), writes=[r_small], allow_slow_non_contiguous=True)
                fw.dma(SY, gkv[:], kv_norm[l].rearrange("(j p) -> p j", p=128), writes=[r_small], allow_slow_non_contiguous=True)
                fw.op(V, lambda e: e.tensor_scalar(out=sc[:], in0=sc[:], scalar1=1.0, scalar2=None, op0=ALU.add), reads=[r_small], writes=[r_small])
                fw.op(V, lambda e: e.memset(epst[:], LN_EPS), writes=[r_small])
                fw.op(V, lambda e: e.memset(epsq[:], RMS_EPS), writes=[r_small])
                cosT = fw.sb([32, S], F32, "cosT")
                sinT = fw.sb([32, S], F32, "sinT")
                r_tabs = Reg()
                fw.dma(SY, cosT[:], cos_d[:, :], reads=[reg("rope")], writes=[r_tabs])
                fw.dma(SY, sinT[:], sin_d[:, :], reads=[reg("rope")], writes=[r_tabs])
                ucv = [fw.sb([128, 514], F32, "ucv") for _ in range(4)]
                r_ucv = [Reg() for _ in range(4)]
                for j in range(4):
                    fw.op(V, lambda e, j=j: e.memset(ucv[j][:, 0:2], 0.0), writes=[r_ucv[j]])
                xts = Rot([fw.sb([128, D], F32, "xt") for _ in range(3)])
                xns = Rot([fw.sb([128, D], BF16, "xn") for _ in range(2)])
                hTs = Rot([fw.sb([128, 8, 512], BF16, "hT") for _ in range(2)])
                stats = fw.sb([128, 2, 6], F32, "stats")
                mv = fw.sb([128, 2], F32, "mv")
                rstd = fw.sb([128, 1], F32, "rstd")
                r_st = Reg()
                gcs = Rot([fw.sb([128, 512], F32, "gcs") for _ in range(2)])
                t1s = Rot([fw.sb([128, 512], F32, "t1s") for _ in range(2)])
                zcs = Rot([fw.sb([128, 4, 512], BF16, "zcs") for _ in range(2)])
                ugs = Rot([fw.sb([128, 4, 512], BF16, "ugs") for _ in range(2)])
                sqs = Rot([fw.sb([128, 512], BF16, "sqs") for _ in range(3)])
                rsts = Rot([fw.sb([128, 512], F32, "rsts") for _ in range(2)])
                cqs = Rot([fw.sb([128, 2, 512], BF16, "cqs") for _ in range(2)])
                ckvs = Rot([fw.sb([128, 512], BF16, "ckvs") for _ in range(2)])
                krs = Rot([fw.sb([32, 512], BF16, "krs") for _ in range(2)])
                ta = fw.sb([32, 512], F32, "ta")
                tb_ = fw.sb([32, 512], F32, "tb")
                r_ta = Reg()
                r_tb = Reg()
                hts1 = {}

                def P1_A(tg):
                    t0 = tg * 512
                    hT, r_hT = hTs.next()
                    for tb in range(4):
                        xt, r_xt = xts.next()
                        row0 = t0 + tb * 128
                        fw.dma(SY, xt[:], x_src[row0:row0 + 128, :], reads=[r_xsrc], writes=[r_xt])
                        layer_norm_stats(xt, r_xt, stats, mv, rstd, r_st, epst, r_small)
                        xn, r_xn = xns.next()
                        fw.op(V, lambda e, xn=xn, xt=xt: e.tensor_scalar(out=xn[:], in0=xt[:], scalar1=mv[:, 0:1], scalar2=rstd[:], op0=ALU.subtract, op1=ALU.mult),
                              reads=[r_xt, r_st], writes=[r_xn])
                        tp, r_tp = pst.next()
                        for kt in range(8):
                            fw.op(T, lambda e, kt=kt, tp=tp, xn=xn: e.transpose(out=tp[:, kt, :], in_=xn[:, kt * 128:(kt + 1) * 128], identity=identb[:]),
                                  reads=[r_xn, rc_const], writes=[r_tp])
                        for kt in range(8):
                            fw.op(A, lambda e, kt=kt, tp=tp, hT=hT, tb=tb: e.activation(out=hT[:, kt, tb * 128:(tb + 1) * 128], in_=tp[:, kt, :], func=AF.Identity,
                                                                                  scale=sc[:, kt:kt + 1], bias=sh[:, kt:kt + 1]),
                                  reads=[r_tp, r_small], writes=[r_hT])
                    fw.dma(P, hT_d.rearrange("(kt p) s -> p kt s", p=128)[:, :, t0:t0 + 512], hT[:], reads=[r_hT], writes=[Reg()])
                    hts1[tg] = (hT, r_hT)

                def P1_B(tg):
                    t0 = tg * 512
                    hT, r_hT = hts1.pop(tg)
                    rd = [r_wA, r_hT]
                    zc, r_zc = zcs.next()
                    for j in range(4):
                        xc_ps, r_xc = psm.next()
                        gc_ps, r_gc = psm.next()
                        gb_ps, r_gb = psm.next()
                        mm8(xc_ps[:], wA, j * 128, (j + 1) * 128, hT, rd, r_xc)
                        mm8(gc_ps[:], wA, DC + j * 128, DC + (j + 1) * 128, hT, rd, r_gc)
                        mm8(gb_ps[:], wA, 2 * DC + j * 128, 2 * DC + (j + 1) * 128, hT, rd, r_gb)
                        gc, r_gcs = gcs.next()
                        fw.op(A, lambda e, gc=gc, gc_ps=gc_ps: e.activation(out=gc[:], in_=gc_ps[:], func=AF.Identity), reads=[r_gc], writes=[r_gcs])
                        fw.op(V, lambda e, j=j, gc=gc, xc_ps=xc_ps: e.tensor_tensor(out=ucv[j][:, 2:514], in0=xc_ps[:], in1=gc[:], op=ALU.mult),
                              reads=[r_xc, r_gcs], writes=[r_ucv[j]])
                        t1, r_t1 = t1s.next()
                        fw.op(V, lambda e, j=j, t1=t1: e.tensor_scalar(out=t1[:], in0=ucv[j][:, 0:512], scalar1=cw[:, j, 0:1], scalar2=None, op0=ALU.mult),
                              reads=[r_ucv[j], r_small], writes=[r_t1])
                        fw.op(V, lambda e, j=j, t1=t1: e.scalar_tensor_tensor(out=t1[:], in0=ucv[j][:, 1:513], scalar=cw[:, j, 1:2], in1=t1[:], op0=ALU.mult, op1=ALU.add),
                              reads=[r_ucv[j], r_small, r_t1], writes=[r_t1])
                        fw.op(V, lambda e, j=j, t1=t1: e.scalar_tensor_tensor(out=t1[:], in0=ucv[j][:, 2:514], scalar=cw[:, j, 2:3], in1=t1[:], op0=ALU.mult, op1=ALU.add),
                              reads=[r_ucv[j], r_small, r_t1], writes=[r_t1])
                        fw.op(V, lambda e, j=j, t1=t1, zc=zc, gb_ps=gb_ps: e.tensor_tensor(out=zc[:, j, :], in0=gb_ps[:], in1=t1[:], op=ALU.mult),
                              reads=[r_gb, r_t1], writes=[r_zc])
                        fw.op(A, lambda e, j=j: e.activation(out=ucv[j][:, 0:2], in_=ucv[j][:, 512:514], func=AF.Identity), reads=[r_ucv[j]], writes=[r_ucv[j]])
                    fw.dma(P, zc_d.rearrange("(j p) s -> p j s", p=128)[:, :, t0:t0 + 512], zc[:], reads=[r_zc], writes=[Reg()])
                    ug, r_ug = ugs.next()
                    for j in range(4):
                        u_ps, r_u = psm.next()
                        mm8(u_ps[:], wA, 3 * DC + j * 128, 3 * DC + (j + 1) * 128, hT, rd, r_u)
                        fw.op(A, lambda e, j=j, ug=ug, u_ps=u_ps: e.activation(out=ug[:, j, :], in_=u_ps[:], func=AF.Identity), reads=[r_u], writes=[r_ug])
                    fw.dma(P, u_d.rearrange("(j p) s -> p j s", p=128)[:, :, t0:t0 + 512], ug[:], reads=[r_ug], writes=[Reg()])
                    for (nj, c0, gvec, dst_rot, dst_d, dname) in ((2, 4 * DC, gq, cqs, cq_d, "cq"), (1, 4 * DC + QL, gkv, ckvs, ckv_d, "ckv")):
                        dst, r_dst = dst_rot.next()
                        pss = []
                        ssq_ps, r_ssq = psm.next()
                        for j in range(nj):
                            c_ps, r_cps = psm.next()
                            mm8(c_ps[:], wA, c0 + j * 128, c0 + (j + 1) * 128, hT, rd, r_cps)
                            sq, r_sq = sqs.next()
                            fw.op(A, lambda e, sq=sq, c_ps=c_ps: e.activation(out=sq[:], in_=c_ps[:], func=AF.Square), reads=[r_cps], writes=[r_sq])
                            fw.op(T, lambda e, sq=sq, j=j, ssq_ps=ssq_ps, nj=nj: e.matmul(ssq_ps[:], lhsT=onesb[:], rhs=sq[:], start=(j == 0), stop=(j == nj - 1)),
                                  reads=[r_sq, rc_const], writes=[r_ssq])
                            pss.append((c_ps, r_cps))
                        rst, r_rst = rsts.next()
                        fw.op(A, lambda e, rst=rst, ssq_ps=ssq_ps, nj=nj: e.activation(out=rst[:], in_=ssq_ps[:], func=AF.Sqrt, bias=epsq[:], scale=1.0 / (128 * nj)),
                              reads=[r_ssq, r_small], writes=[r_rst])
                        fw.op(V, lambda e, rst=rst: e.reciprocal(out=rst[:], in_=rst[:]), reads=[r_rst], writes=[r_rst])
                        for j in range(nj):
                            c_ps, r_cps = pss[j]
                            o_ap = dst[:, j, :] if nj == 2 else dst[:]
                            fw.op(V, lambda e, o_ap=o_ap, c_ps=c_ps, j=j, rst=rst, gvec=gvec: e.scalar_tensor_tensor(out=o_ap, in0=c_ps[:], scalar=gvec[:, j:j + 1], in1=rst[:], op0=ALU.mult, op1=ALU.mult),
                                  reads=[r_cps, r_rst, r_small], writes=[r_dst])
                        if nj == 2:
                            fw.dma(P, dst_d.rearrange("(j p) s -> p j s", p=128)[:, :, t0:t0 + 512], dst[:], reads=[r_dst], writes=[Reg()])
                        else:
                            fw.dma(P, dst_d[:, t0:t0 + 512], dst[:], reads=[r_dst], writes=[Reg()])
                    kr_ps, r_kr = psm.next()
                    ksw_ps, r_ksw = psm.next()
                    mm8(kr_ps[0:32, :], wA, IN_MIX - ROPE, IN_MIX, hT, rd, r_kr)
                    mm8(ksw_ps[0:32, :], wA, IN_MIX, IN_MIX + ROPE, hT, rd, r_ksw)
                    kr, r_krs = krs.next()
                    fw.op(V, lambda e, kr_ps=kr_ps: e.tensor_tensor(out=ta[:], in0=kr_ps[0:32, :], in1=cosT[:, t0:t0 + 512], op=ALU.mult), reads=[r_kr, r_tabs], writes=[r_ta])
                    fw.op(V, lambda e, ksw_ps=ksw_ps: e.tensor_tensor(out=tb_[:], in0=ksw_ps[0:32, :], in1=sinT[:, t0:t0 + 512], op=ALU.mult), reads=[r_ksw, r_tabs], writes=[r_tb])
                    fw.op(V, lambda e, kr=kr: e.tensor_tensor(out=kr[:], in0=ta[:], in1=tb_[:], op=ALU.add), reads=[r_ta, r_tb], writes=[r_krs])
                    fw.dma(P, kr_d[:, t0:t0 + 512], kr[:], reads=[r_krs], writes=[Reg()])

                P1_A(0)
                for tg in range(NTG):
                    if tg + 1 < NTG:
                        P1_A(tg + 1)
                    P1_B(tg)
            fw.es = es0


        def p3(l):
            with contextlib.ExitStack() as es:
                fw.es = es
                fw.barrier()
                ps_s = Rot([fw.ps(es, "p3s", [128, 512], F32) for i in range(3)])
                ps_o = Rot([fw.ps(es, "p3o", [128, 4, DV + 1], F32) for i in range(2)])
                ps_q = Rot([fw.ps(es, "p3q", [128, 512], F32) for i in range(2)])
                ps_t = Rot([fw.ps(es, "p3t", [128, 4, 128], BF16) for i in range(1)])
                stg = None
                wq = fw.sb([128, 2, H * DK], BF16, "wq")
                wqs = fw.sb([128, 2, H * DK], BF16, "wqs")
                wk = fw.sb([128, 1, H * NOPE], BF16, "wk")
                wv = fw.sb([128, 1, H * DV], BF16, "wv")
                r_w = RegList()
                load_cast(wq, r_w, lambda kt: w_uq[l, kt * 128:(kt + 1) * 128, :], 2, H * DK, stg)
                load_cast(wqs, r_w, lambda kt: w_uq_sw[l, kt * 128:(kt + 1) * 128, :], 2, H * DK, stg)
                load_cast(wk, r_w, lambda kt: w_uk[l, kt * 128:(kt + 1) * 128, :], 1, H * NOPE, stg)
                load_cast(wv, r_w, lambda kt: w_uv[l, kt * 128:(kt + 1) * 128, :], 1, H * DV, stg)
                ckv = fw.sb([128, S], BF16, "ckv")
                r_ckv = Reg()
                fw.dma(SY, ckv[:], ckv_d[:, :], reads=[reg("ckv")], writes=[r_ckv])
                KT = fw.sb([DK, H, S], BF16, "KT")
                r_KT = Reg()
                Vt = fw.sb([128, NTB, H, DV + 1], BF16, "Vt")
                r_V = Reg()
                for h in range(H):
                    fw.dma(SY, KT[NOPE:DK, h, :], kr_d[:, :], reads=[reg("kr")], writes=[r_KT])
                fw.op(P, lambda e: e.memset(Vt[:, :, :, DV:DV + 1], 1.0), writes=[r_V])
                cnt = 0
                for tg in range(NTG):
                    t0 = tg * 512
                    for h in range(H):
                        kp, r_kp = ps_q.next()
                        fw.op(T, lambda e, kp=kp, h=h, t0=t0: e.matmul(kp[0:NOPE, :], lhsT=wk[:, 0, h * NOPE:(h + 1) * NOPE], rhs=ckv[:, t0:t0 + 512], start=True, stop=True),
                              reads=[r_w, r_ckv], writes=[r_kp])
                        if cnt % 2 == 0:
                            fw.op(A, lambda e, kp=kp, h=h, t0=t0: e.activation(out=KT[0:NOPE, h, t0:t0 + 512], in_=kp[0:NOPE, :], func=AF.Identity), reads=[r_kp], writes=[r_KT])
                        else:
                            fw.op(V, lambda e, kp=kp, h=h, t0=t0: e.tensor_copy(out=KT[0:NOPE, h, t0:t0 + 512], in_=kp[0:NOPE, :]), reads=[r_kp], writes=[r_KT])
                        cnt += 1
                    for tb in range(4):
                        tbg = tg * 4 + tb
                        vp, r_vp = ps_q.next()
                        fw.op(T, lambda e, vp=vp, tbg=tbg: e.matmul(vp[:], lhsT=ckv[:, tbg * 128:(tbg + 1) * 128], rhs=wv[:, 0, :], start=True, stop=True),
                              reads=[r_w, r_ckv], writes=[r_vp])
                        if cnt % 2 == 0:
                            fw.op(A, lambda e, vp=vp, tbg=tbg: e.activation(out=Vt[:, tbg, :, 0:DV], in_=vp[:].rearrange("p (h d) -> p h d", h=H), func=AF.Identity), reads=[r_vp], writes=[r_V])
                        else:
                            fw.op(V, lambda e, vp=vp, tbg=tbg: e.tensor_copy(out=Vt[:, tbg, :, 0:DV], in_=vp[:].rearrange("p (h d) -> p h d", h=H)), reads=[r_vp], writes=[r_V])
                        cnt += 1
                cqgs = Rot([fw.sb([128, 2, 512], BF16, "cqg") for _ in range(2)])
                css = Rot([fw.sb([DK, 512], F32, "cs") for _ in range(2)])
                sns = Rot([fw.sb([DK, 512], F32, "sn") for _ in range(2)])
                qTs = Rot([fw.sb([DK, 512], BF16, "qT") for _ in range(3)])
                pTs = Rot([fw.sb([128, 512], BF16, "pT") for _ in range(4)])
                tas = Rot([fw.sb([DK, 512], F32, "ta3") for _ in range(2)])
                tbs = Rot([fw.sb([DK, 512], F32, "tb3") for _ in range(2)])
                otoks = Rot([fw.sb([128, 4, DC], BF16, "otok") for _ in range(2)])
                oTgs = Rot([fw.sb([128, 4, 512], BF16, "oTg") for _ in range(2)])
                rinvs = Rot([fw.sb([128, 4], F32, "rinv") for _ in range(2)])
                sm_scale = float(DK) ** -0.5
                for qg in range(NTG):
                    t0 = qg * 512
                    cqg, r_cqg = cqgs.next()
                    cs, r_cs = css.next()
                    sn, r_sn = sns.next()
                    fw.dma(SY, cqg[:], cq_d.rearrange("(j p) s -> p j s", p=128)[:, :, t0:t0 + 512], reads=[reg("cq")], writes=[r_cqg])
                    fw.dma(SY, cs[NOPE:DK, :], cos_d[:, t0:t0 + 512], reads=[reg("rope")], writes=[r_cs])
                    fw.dma(SY, sn[NOPE:DK, :], sin_d[:, t0:t0 + 512], reads=[reg("rope")], writes=[r_sn])
                    otok, r_otok = otoks.next()
                    qst = {}

                    def Q3(h):
                        q_ps, r_qps = ps_q.next()
                        qs_ps, r_qsps = ps_q.next()
                        for kt in range(2):
                            fw.op(T, lambda e, kt=kt, q_ps=q_ps, h=h, cqg=cqg: e.matmul(q_ps[0:DK, :], lhsT=wq[:, kt, h * DK:(h + 1) * DK], rhs=cqg[:, kt, :], start=(kt == 0), stop=(kt == 1)),
                                  reads=[r_w, r_cqg], writes=[r_qps])
                        for kt in range(2):
                            fw.op(T, lambda e, kt=kt, qs_ps=qs_ps, h=h, cqg=cqg: e.matmul(qs_ps[0:DK, :], lhsT=wqs[:, kt, h * DK:(h + 1) * DK], rhs=cqg[:, kt, :], start=(kt == 0), stop=(kt == 1)),
                                  reads=[r_w, r_cqg], writes=[r_qsps])
                        qT, r_qT = qTs.next()
                        ta3, r_ta3 = tas.next()
                        tb3, r_tb3 = tbs.next()
                        fw.op(A, lambda e, qT=qT, q_ps=q_ps: e.activation(out=qT[0:NOPE, :], in_=q_ps[0:NOPE, :], func=AF.Identity), reads=[r_qps], writes=[r_qT])
                        fw.op(V, lambda e, ta3=ta3, q_ps=q_ps, cs=cs: e.tensor_tensor(out=ta3[NOPE:DK, :], in0=q_ps[NOPE:DK, :], in1=cs[NOPE:DK, :], op=ALU.mult), reads=[r_qps, r_cs], writes=[r_ta3])
                        fw.op(V, lambda e, tb3=tb3, qs_ps=qs_ps, sn=sn: e.tensor_tensor(out=tb3[NOPE:DK, :], in0=qs_ps[NOPE:DK, :], in1=sn[NOPE:DK, :], op=ALU.mult), reads=[r_qsps, r_sn], writes=[r_tb3])
                        fw.op(V, lambda e, qT=qT, ta3=ta3, tb3=tb3: e.tensor_tensor(out=qT[NOPE:DK, :], in0=ta3[NOPE:DK, :], in1=tb3[NOPE:DK, :], op=ALU.add), reads=[r_ta3, r_tb3], writes=[r_qT])
                        qst[h] = (qT, r_qT)

                    def KB3(h):
                        qT, r_qT = qst.pop(h)
                        o_ps, r_ops = ps_o.next()
                        nkb = 4 * qg + 4

                        def emit_s(kb):
                            m = kb - 4 * qg
                            c0 = max(0, m) * 128
                            s_ps, r_sps = ps_s.next()
                            fw.op(T, lambda e: e.matmul(s_ps[:, c0:512], lhsT=KT[0:DK, h, kb * 128:(kb + 1) * 128], rhs=qT[0:DK, c0:512], start=True, stop=True),
                                  reads=[r_KT, r_qT], writes=[r_sps])
                            return (s_ps, r_sps, m, c0)

                        pend = [emit_s(0)]
                        if nkb > 1:
                            pend.append(emit_s(1))
                        for kb in range(nkb):
                            s_ps, r_sps, m, c0 = pend.pop(0)
                            if kb + 2 < nkb:
                                pend.append(emit_s(kb + 2))
                            pT, r_pT = pTs.next()
                            fw.op(A, lambda e, pT=pT, s_ps=s_ps, c0=c0: e.activation(out=pT[:, c0:512], in_=s_ps[:, c0:512], func=AF.Exp, scale=sm_scale), reads=[r_sps], writes=[r_pT])
                            if m >= 0:
                                fw.op(V, lambda e, pT=pT, c0=c0: e.tensor_tensor(out=pT[:, c0:c0 + 128], in0=pT[:, c0:c0 + 128], in1=trib[:], op=ALU.mult), reads=[r_pT, rc_const], writes=[r_pT])
                            for qb in range(max(0, m), 4):
                                fw.op(T, lambda e, qb=qb, pT=pT, kb=kb: e.matmul(o_ps[:, qb, :], lhsT=pT[:, qb * 128:(qb + 1) * 128], rhs=Vt[:, kb, h, :], start=(kb == 0 and qb == 0), stop=(kb == 4 * qg + qb), skip_group_check=True),
                                      reads=[r_pT, r_V], writes=[r_ops])
                        rinv, r_rinv = rinvs.next()
                        fw.op(V, lambda e, rinv=rinv, o_ps=o_ps: e.reciprocal(out=rinv[:], in_=o_ps[:, :, DV]), reads=[r_ops], writes=[r_rinv])
                        for qb in range(4):
                            fw.op(V, lambda e, qb=qb, rinv=rinv, o_ps=o_ps, otok=otok, h=h: e.tensor_scalar(out=otok[:, qb, h * DV:(h + 1) * DV], in0=o_ps[:, qb, 0:DV], scalar1=rinv[:, qb:qb + 1], scalar2=None, op0=ALU.mult),
                                  reads=[r_ops, r_rinv], writes=[r_otok])

                    Q3(0)
                    for h in range(H):
                        if h + 1 < H:
                            Q3(h + 1)
                        KB3(h)
                    oTg, r_oTg = oTgs.next()
                    for qb in range(4):
                        tp, r_tp = ps_t.next()
                        for j in range(4):
                            fw.op(T, lambda e, j=j, qb=qb, tp=tp, otok=otok: e.transpose(out=tp[:, j, :], in_=otok[:, qb, j * 128:(j + 1) * 128], identity=identb[:]), reads=[r_otok, rc_const], writes=[r_tp])
                        fw.op(A, lambda e, qb=qb, tp=tp, oTg=oTg: e.activation(out=oTg[:, :, qb * 128:(qb + 1) * 128], in_=tp[:], func=AF.Identity), reads=[r_tp], writes=[r_oTg])
                    fw.dma(P, oT_d.rearrange("(j p) s -> p j s", p=128)[:, :, t0:t0 + 512], oTg[:], reads=[r_oTg], writes=[Reg()])
                if "d_KT" in dbg_aps:
                    fw.dma(SY, dbg_aps["d_KT"][:, :], KT[:].rearrange("p h s -> p (h s)"), reads=[r_KT], writes=[reg("dbg")])
                    fw.dma(SY, dbg_aps["d_V"][:, :], Vt[:].rearrange("p a h d -> p (a h d)"), reads=[r_V], writes=[reg("dbg")])
                    fw.dma(SY, dbg_aps["d_qT"][:, :], qT[:], reads=[r_qT], writes=[reg("dbg")])
                    fw.dma(SY, dbg_aps["d_pT"][:, :], pT[:], reads=[r_pT], writes=[reg("dbg")])
                    fw.dma(SY, dbg_aps["d_otok"][:, :], otok[:].rearrange("p a d -> p (a d)"), reads=[r_otok], writes=[reg("dbg")])
            fw.es = es0


        C1 = float(np.float32(6.28125))
        C2 = float(np.float32(TWO_PI - 6.28125))
        C3 = float(TWO_PI - 6.28125 - float(np.float32(TWO_PI - 6.28125)))
        GELU_K = 2.0 * math.sqrt(2.0 / math.pi)

        def p2(l):
            TC = 512
            with contextlib.ExitStack() as es:
                fw.es = es
                fw.barrier()
                ps_b = Rot([fw.ps(es, "p2b", [128, 512], F32) for i in range(4)])
                ps_y = Rot([fw.ps(es, "p2y", [128, 512], F32) for i in range(2)])
                ps_g = Rot([fw.ps(es, "p2g", [128, 512], F32) for i in range(2)])
                r_pre = Reg()
                sh16 = [128, 16]
                are = fw.sb(sh16, F32, "are")
                aim = fw.sb(sh16, F32, "aim")
                dtl = fw.sb(sh16, F32, "dtl")
                rr = fw.sb(sh16, F32, "rr")
                th = fw.sb(sh16, F32, "th")
                thT = fw.sb(sh16, F32, "thT")
                lre = fw.sb(sh16, F32, "lre")
                lim = fw.sb(sh16, F32, "lim")
                cTc = fw.sb(sh16, F32, "cTc")
                sTc = fw.sb(sh16, F32, "sTc")
                nsTc = fw.sb(sh16, F32, "nsTc")
                fre = fw.sb(sh16, F32, "fre")
                fim = fw.sb(sh16, F32, "fim")
                nfim = fw.sb(sh16, F32, "nfim")
                den = fw.sb(sh16, F32, "den")
                t16a = fw.sb(sh16, F32, "t16a")
                t16b = fw.sb(sh16, F32, "t16b")
                t16i = fw.sb(sh16, I32, "t16i")
                dsk = fw.sb([128, 4], F32, "dsk")
                iot = fw.sb([128, TC], F32, "iot")
                fw.dma(SY, are[:], ssm_a_re[l].rearrange("(gp g2) n -> (g2 n) gp", g2=2), writes=[r_pre], allow_slow_non_contiguous=True)
                fw.dma(SY, aim[:], ssm_a_im[l].rearrange("(gp g2) n -> (g2 n) gp", g2=2), writes=[r_pre], allow_slow_non_contiguous=True)
                ldt2 = ssm_log_dt[l].rearrange("(gp g2) -> g2 gp", g2=2)
                for g2 in range(2):
                    fw.dma(SY, dtl[g2 * 64:(g2 + 1) * 64, :], ldt2[g2:g2 + 1, :].partition_broadcast(64), writes=[r_pre], allow_slow_non_contiguous=True)
                fw.dma(SY, dsk[:], ssm_d[l].rearrange("(ct p) -> p ct", p=128), writes=[r_pre], allow_slow_non_contiguous=True)
                fw.dma(SY, iot[:], iota_f[:, 0:TC], writes=[r_pre])

                def vop(f, reads=(r_pre,), writes=(r_pre,), eng=V):
                    fw.op(eng, f, reads=list(reads), writes=list(writes))

                def sincos(src, sin_dst, cos_dst, shape, tf, ti, tm):
                    vop(lambda e: e.tensor_scalar(out=tf, in0=src, scalar1=1.0 / TWO_PI, scalar2=None, op0=ALU.mult))
                    vop(lambda e: e.tensor_copy(out=ti, in_=tf))
                    vop(lambda e: e.tensor_copy(out=tf, in_=ti))
                    vop(lambda e: e.scalar_tensor_tensor(out=tm, in0=tf, scalar=-C1, in1=src, op0=ALU.mult, op1=ALU.add))
                    vop(lambda e: e.scalar_tensor_tensor(out=tm, in0=tf, scalar=-C2, in1=tm, op0=ALU.mult, op1=ALU.add))
                    vop(lambda e: e.scalar_tensor_tensor(out=tm, in0=tf, scalar=-C3, in1=tm, op0=ALU.mult, op1=ALU.add))
                    for dst, shift in ((sin_dst, 0.0), (cos_dst, math.pi / 2)):
                        vop(lambda e, dst=dst, shift=shift: e.tensor_scalar(out=dst, in0=tm, scalar1=shift, scalar2=None, op0=ALU.add))
                        vop(lambda e, dst=dst: e.tensor_scalar(out=tf, in0=dst, scalar1=math.pi, scalar2=None, op0=ALU.is_gt))
                        vop(lambda e, dst=dst: e.scalar_tensor_tensor(out=dst, in0=tf, scalar=-TWO_PI, in1=dst, op0=ALU.mult, op1=ALU.add))
                        vop(lambda e, dst=dst: e.tensor_scalar(out=tf, in0=dst, scalar1=-math.pi, scalar2=None, op0=ALU.is_lt))
                        vop(lambda e, dst=dst: e.scalar_tensor_tensor(out=dst, in0=tf, scalar=TWO_PI, in1=dst, op0=ALU.mult, op1=ALU.add))
                        vop(lambda e, dst=dst: e.tensor_scalar(out=dst, in0=dst, scalar1=math.pi, scalar2=-math.pi, op0=ALU.min, op1=ALU.max))
                        vop(lambda e, dst=dst: e.activation(out=dst, in_=dst, func=AF.Sin), eng=A)

                vop(lambda e: e.activation(out=dtl[:], in_=dtl[:], func=AF.Exp), eng=A)
                vop(lambda e: e.tensor_tensor(out=rr[:], in0=are[:], in1=dtl[:], op=ALU.mult))
                vop(lambda e: e.activation(out=rr[:], in_=rr[:], func=AF.Exp), eng=A)
                vop(lambda e: e.tensor_tensor(out=th[:], in0=aim[:], in1=dtl[:], op=ALU.mult))
                sincos(th[:], lim[:], lre[:], sh16, t16a[:], t16i[:], t16b[:])
                vop(lambda e: e.tensor_tensor(out=lre[:], in0=lre[:], in1=rr[:], op=ALU.mult))
                vop(lambda e: e.tensor_tensor(out=lim[:], in0=lim[:], in1=rr[:], op=ALU.mult))
                vop(lambda e: e.tensor_scalar(out=thT[:], in0=th[:], scalar1=float(TC), scalar2=None, op0=ALU.mult))
                sincos(thT[:], sTc[:], cTc[:], sh16, t16a[:], t16i[:], t16b[:])
                vop(lambda e: e.tensor_scalar(out=nsTc[:], in0=sTc[:], scalar1=-1.0, scalar2=None, op0=ALU.mult))
                vop(lambda e: e.tensor_tensor(out=den[:], in0=are[:], in1=are[:], op=ALU.mult))
                vop(lambda e: e.tensor_tensor(out=t16a[:], in0=aim[:], in1=aim[:], op=ALU.mult))
                vop(lambda e: e.tensor_tensor(out=den[:], in0=den[:], in1=t16a[:], op=ALU.add))
                vop(lambda e: e.reciprocal(out=den[:], in_=den[:]))
                vop(lambda e: e.tensor_scalar(out=t16a[:], in0=lre[:], scalar1=-1.0, scalar2=None, op0=ALU.add))
                vop(lambda e: e.tensor_tensor(out=fre[:], in0=t16a[:], in1=are[:], op=ALU.mult))
                vop(lambda e: e.tensor_tensor(out=t16b[:], in0=lim[:], in1=aim[:], op=ALU.mult))
                vop(lambda e: e.tensor_tensor(out=fre[:], in0=fre[:], in1=t16b[:], op=ALU.add))
                vop(lambda e: e.tensor_tensor(out=fre[:], in0=fre[:], in1=den[:], op=ALU.mult))
                vop(lambda e: e.tensor_tensor(out=fim[:], in0=lim[:], in1=are[:], op=ALU.mult))
                vop(lambda e: e.tensor_tensor(out=t16b[:], in0=t16a[:], in1=aim[:], op=ALU.mult))
                vop(lambda e: e.tensor_tensor(out=fim[:], in0=fim[:], in1=t16b[:], op=ALU.subtract))
                vop(lambda e: e.tensor_tensor(out=fim[:], in0=fim[:], in1=den[:], op=ALU.mult))
                vop(lambda e: e.tensor_scalar(out=nfim[:], in0=fim[:], scalar1=-1.0, scalar2=None, op0=ALU.mult))

                BT = [fw.sb([128, 16, 128], BF16, "BTre"), fw.sb([128, 16, 128], BF16, "BTim")]
                CT = [fw.sb([128, 16, 128], BF16, "CTre"), fw.sb([128, 16, 128], BF16, "CTim"), fw.sb([128, 16, 128], BF16, "CTnre")]
                r_BT = Reg()
                r_CT = Reg()
                with contextlib.ExitStack() as es_pre:
                    fw.es = es_pre
                    BD = [fw.sb([128, 16, 128], F32, "BDre"), fw.sb([128, 16, 128], F32, "BDim")]
                    BB = [fw.sb([128, 16, 128], F32, "BBre"), fw.sb([128, 16, 128], F32, "BBim")]
                    CBD = [fw.sb([32, 16, 128], F32, "CBDre"), fw.sb([32, 16, 128], F32, "CBDim")]
                    r_BD = Reg()
                    r_BB = Reg()
                    r_CBD = Reg()
                    for ri, src in enumerate((ssm_b_re, ssm_b_im)):
                        fw.op(P, lambda e, ri=ri: e.memset(BD[ri][:], 0.0), writes=[r_BD])
                        v = src[l].rearrange("(ct j g2) n q -> j g2 n ct q", j=4, g2=2)
                        for j in range(4):
                            for g2 in range(2):
                                dstv = BD[ri][g2 * 64:(g2 + 1) * 64, :, j * 32 + g2 * 16:j * 32 + g2 * 16 + 16].rearrange("p (ct jj) q -> p ct jj q", jj=4)[:, :, j, :]
                                fw.dma(SY, dstv, v[j, g2], writes=[r_BD])
                    for ri, src in enumerate((ssm_c_re, ssm_c_im)):
                        fw.op(P, lambda e, ri=ri: e.memset(CBD[ri][:], 0.0), writes=[r_CBD])
                        fw.op(P, lambda e, ri=ri: e.memset(CT[ri][:], 0.0), writes=[r_CT])
                        if ri == 0:
                            fw.op(P, lambda e: e.memset(CT[2][:], 0.0), writes=[r_CT])
                        v = src[l].rearrange("(gp g2) p n -> g2 p gp n", g2=2)
                        for g2 in range(2):
                            fw.dma(SY, CBD[ri][g2 * 16:(g2 + 1) * 16, :, g2 * 64:(g2 + 1) * 64], v[g2], writes=[r_CBD])
                    bc16 = lambda t: t[:].unsqueeze(2).to_broadcast([128, 16, 128])
                    BBt = fw.sb([128, 16, 128], F32, "BBt")
                    fw.op(V, lambda e: e.tensor_tensor(out=BB[0][:], in0=BD[0][:], in1=bc16(fre), op=ALU.mult), reads=[r_BD, r_pre], writes=[r_BB])
                    fw.op(V, lambda e: e.tensor_tensor(out=BBt[:], in0=BD[1][:], in1=bc16(nfim), op=ALU.mult), reads=[r_BD, r_pre], writes=[r_BB])
                    fw.op(V, lambda e: e.tensor_tensor(out=BB[0][:], in0=BB[0][:], in1=BBt[:], op=ALU.add), reads=[r_BB], writes=[r_BB])
                    fw.op(V, lambda e: e.tensor_tensor(out=BB[1][:], in0=BD[1][:], in1=bc16(fre), op=ALU.mult), reads=[r_BD, r_pre], writes=[r_BB])
                    fw.op(V, lambda e: e.tensor_tensor(out=BBt[:], in0=BD[0][:], in1=bc16(fim), op=ALU.mult), reads=[r_BD, r_pre], writes=[r_BB])
                    fw.op(V, lambda e: e.tensor_tensor(out=BB[1][:], in0=BB[1][:], in1=BBt[:], op=ALU.add), reads=[r_BB], writes=[r_BB])
                    for gp in range(16):
                        for ri in range(2):
                            tp, r_tp = ps_b.next()
                            fw.op(T, lambda e, gp=gp, ri=ri, tp=tp: e.transpose(out=tp[:, 0:128], in_=BB[ri][:, gp, :], identity=identf[:]), reads=[r_BB, rc_const], writes=[r_tp])
                            fw.op(A, lambda e, gp=gp, ri=ri, tp=tp: e.activation(out=BT[ri][:, gp, :], in_=tp[:, 0:128], func=AF.Identity), reads=[r_tp], writes=[r_BT])
                            tp2, r_tp2 = ps_b.next()
                            fw.op(T, lambda e, gp=gp, ri=ri, tp2=tp2: e.transpose(out=tp2[:, 0:32], in_=CBD[ri][:, gp, :], identity=identf[0:32, 0:32]), reads=[r_CBD, rc_const], writes=[r_tp2])
                            jj = gp % 4
                            fw.op(A, lambda e, gp=gp, ri=ri, tp2=tp2, jj=jj: e.activation(out=CT[ri][:, gp, jj * 32:(jj + 1) * 32], in_=tp2[:, 0:32], func=AF.Identity, scale=(1.0 if ri == 0 else -1.0)),
                                  reads=[r_tp2], writes=[r_CT])
                            if ri == 0:
                                fw.op(A, lambda e, gp=gp, tp2=tp2, jj=jj: e.activation(out=CT[2][:, gp, jj * 32:(jj + 1) * 32], in_=tp2[:, 0:32], func=AF.Identity, scale=-1.0),
                                      reads=[r_tp2], writes=[r_CT])
                fw.es = es
                fw.barrier()
                cosT = fw.sb([128, 16, TC], F32, "cosT2")
                sinT = fw.sb([128, 16, TC], F32, "sinT2")
                with contextlib.ExitStack() as es_t:
                    fw.es = es_t
                    NT = 8 * TC
                    angt = fw.sb([128, NT], F32, "angt")
                    tft = fw.sb([128, NT], F32, "tft")
                    tit = fw.sb([128, NT], I32, "tit")
                    tmt = fw.sb([128, NT], F32, "tmt")
                    g3 = lambda t: t[:].rearrange("p (g t) -> p g t", g=8)
                    for hf in range(2):
                        gs_ = slice(hf * 8, hf * 8 + 8)
                        vop(lambda e, gs_=gs_: e.tensor_tensor(out=g3(angt), in0=iot[:].unsqueeze(1).to_broadcast([128, 8, TC]), in1=th[:, gs_].unsqueeze(2).to_broadcast([128, 8, TC]), op=ALU.mult))
                        sincos(angt[:], sinT[:, gs_, :].rearrange("p g t -> p (g t)"), cosT[:, gs_, :].rearrange("p g t -> p (g t)"), [128, NT], tft[:], tit[:], tmt[:])
                fw.es = es
                fw.barrier()
                wg = fw.sb([128, 4, 2 * DC], BF16, "wg")
                r_wg = RegList()
                stg = None
                load_cast(wg, r_wg, lambda kt: ssm_w_glu[l, kt * 128:(kt + 1) * 128, :], 4, 2 * DC, stg)
                car = [fw.sb([128, 16], F32, "car_re"), fw.sb([128, 16], F32, "car_im")]
                r_car = [Reg() for _ in range(16)]
                fw.op(V, lambda e: e.memset(car[0][:], 0.0), writes=r_car)
                fw.op(V, lambda e: e.memset(car[1][:], 0.0), writes=r_car)
                uts = Rot([fw.sb([128, 4, TC], BF16, "uT2") for _ in range(2)])
                f32t = lambda nm, n: Rot([fw.sb([128, TC], F32, nm) for _ in range(n)])
                t1s, t2s, t3s, t4s = f32t("s1", 2), f32t("s2", 2), f32t("s3", 2), f32t("s4", 2)
                wres, wims = f32t("wre", 3), f32t("wim", 3)
                vres, vims = f32t("vre", 3), f32t("vim", 3)
                bft = lambda nm, n: Rot([fw.sb([128, TC], BF16, nm) for _ in range(n)])
                p1s, p2s, p3s, p4s = bft("q1", 3), bft("q2", 3), bft("q3", 3), bft("q4", 3)
                sres = Rot([fw.sb([128, TC], BF16, "sre") for _ in range(3)])
                sims = Rot([fw.sb([128, TC], BF16, "sim") for _ in range(3)])
                ygs = Rot([fw.sb([128, 4, TC], BF16, "yg") for _ in range(2)])
                yfs = f32t("yf", 2)
                ysq = f32t("ysq", 2)
                sgs = f32t("sg2", 2)
                zss = Rot([fw.sb([128, 4, TC], BF16, "zs") for _ in range(2)])
                ctmp = fw.sb([128, 2], F32, "ctmp")
                r_ctmp = Reg()
                for c in range(S // TC):
                    t0 = c * TC
                    ut, r_ut = uts.next()
                    fw.dma(SY, ut[:], u_d.rearrange("(j p) s -> p j s", p=128)[:, :, t0:t0 + TC], reads=[reg("u")], writes=[r_ut])
                    yg, r_yg = ygs.next()
                    st2 = {}

                    def P2A(it):
                        ct, j = divmod(it, 4)
                        gp = it
                        cs_ = cosT[:, gp, :]
                        sn_ = sinT[:, gp, :]
                        bre, r_bre = ps_b.next()
                        bim, r_bim = ps_b.next()
                        fw.op(T, lambda e, gp=gp, bre=bre, ct=ct, ut=ut: e.matmul(bre[:, 0:TC], lhsT=BT[0][:, gp, :], rhs=ut[:, ct, :], start=True, stop=True), reads=[r_BT, r_ut], writes=[r_bre])
                        fw.op(T, lambda e, gp=gp, bim=bim, ct=ct, ut=ut: e.matmul(bim[:, 0:TC], lhsT=BT[1][:, gp, :], rhs=ut[:, ct, :], start=True, stop=True), reads=[r_BT, r_ut], writes=[r_bim])
                        cs_ = cosT[:, gp, :]
                        sn_ = sinT[:, gp, :]
                        (t1, r1), (t2, r2), (t3, r3), (t4, r4) = t1s.next(), t2s.next(), t3s.next(), t4s.next()
                        fw.op(V, lambda e, t1=t1, bre=bre, cs_=cs_: e.tensor_tensor(out=t1[:], in0=bre[:, 0:TC], in1=cs_, op=ALU.mult), reads=[r_bre, r_pre], writes=[r1])
                        fw.op(V, lambda e, t2=t2, bim=bim, sn_=sn_: e.tensor_tensor(out=t2[:], in0=bim[:, 0:TC], in1=sn_, op=ALU.mult), reads=[r_bim, r_pre], writes=[r2])
                        fw.op(V, lambda e, t3=t3, bim=bim, cs_=cs_: e.tensor_tensor(out=t3[:], in0=bim[:, 0:TC], in1=cs_, op=ALU.mult), reads=[r_bim, r_pre], writes=[r3])
                        fw.op(V, lambda e, t4=t4, bre=bre, sn_=sn_: e.tensor_tensor(out=t4[:], in0=bre[:, 0:TC], in1=sn_, op=ALU.mult), reads=[r_bre, r_pre], writes=[r4])
                        (wre, r_wre), (wim, r_wim) = wres.next(), wims.next()
                        fw.op(P, lambda e, wre=wre, t1=t1, t2=t2: e.tensor_tensor(out=wre[:], in0=t1[:], in1=t2[:], op=ALU.add), reads=[r1, r2], writes=[r_wre])
                        fw.op(P, lambda e, wim=wim, t3=t3, t4=t4: e.tensor_tensor(out=wim[:], in0=t3[:], in1=t4[:], op=ALU.subtract), reads=[r3, r4], writes=[r_wim])
                        st2[it] = dict(wre=wre, r_wre=r_wre, wim=wim, r_wim=r_wim)

                    def P2B(it):
                        ct, j = divmod(it, 4)
                        gp = it
                        cs_ = cosT[:, gp, :]
                        sn_ = sinT[:, gp, :]
                        d_ = st2[it]
                        wre, r_wre, wim, r_wim = d_['wre'], d_['r_wre'], d_['wim'], d_['r_wim']
                        (vre, r_vre), (vim, r_vim) = vres.next(), vims.next()
                        fw.op(V, lambda e, vre=vre, wre=wre, gp=gp: e.tensor_tensor_scan(out=vre[:], data0=rr[:, gp:gp + 1].to_broadcast([128, TC]), data1=wre[:], initial=car[0][:, gp:gp + 1], op0=ALU.mult, op1=ALU.add),
                              reads=[r_wre, r_pre, r_car[gp]], writes=[r_vre])
                        fw.op(V, lambda e, vim=vim, wim=wim, gp=gp: e.tensor_tensor_scan(out=vim[:], data0=rr[:, gp:gp + 1].to_broadcast([128, TC]), data1=wim[:], initial=car[1][:, gp:gp + 1], op0=ALU.mult, op1=ALU.add),
                              reads=[r_wim, r_pre, r_car[gp]], writes=[r_vim])
                        fw.op(A, lambda e, vre=vre, gp=gp: e.activation(out=ctmp[:, 0:1], in_=vre[:, TC - 1:TC], func=AF.Identity, scale=cTc[:, gp:gp + 1]), reads=[r_vre, r_pre], writes=[r_ctmp])
                        fw.op(A, lambda e, vre=vre, gp=gp: e.activation(out=ctmp[:, 1:2], in_=vre[:, TC - 1:TC], func=AF.Identity, scale=sTc[:, gp:gp + 1]), reads=[r_vre, r_pre], writes=[r_ctmp])
                        fw.op(A, lambda e, vim=vim, gp=gp: e.activation(out=car[0][:, gp:gp + 1], in_=vim[:, TC - 1:TC], func=AF.Identity, scale=nsTc[:, gp:gp + 1], bias=ctmp[:, 0:1]),
                              reads=[r_vim, r_pre, r_ctmp], writes=[r_car[gp]])
                        fw.op(A, lambda e, vim=vim, gp=gp: e.activation(out=car[1][:, gp:gp + 1], in_=vim[:, TC - 1:TC], func=AF.Identity, scale=cTc[:, gp:gp + 1], bias=ctmp[:, 1:2]),
                              reads=[r_vim, r_pre, r_ctmp], writes=[r_car[gp]])
                        (q1, rq1), (q2, rq2), (q3, rq3), (q4, rq4) = p1s.next(), p2s.next(), p3s.next(), p4s.next()
                        fw.op(P, lambda e, q1=q1, vre=vre, cs_=cs_: e.tensor_tensor(out=q1[:], in0=vre[:], in1=cs_, op=ALU.mult), reads=[r_vre, r_pre], writes=[rq1])
                        fw.op(P, lambda e, q2=q2, vim=vim, sn_=sn_: e.tensor_tensor(out=q2[:], in0=vim[:], in1=sn_, op=ALU.mult), reads=[r_vim, r_pre], writes=[rq2])
                        fw.op(P, lambda e, q3=q3, vre=vre, sn_=sn_: e.tensor_tensor(out=q3[:], in0=vre[:], in1=sn_, op=ALU.mult), reads=[r_vre, r_pre], writes=[rq3])
                        fw.op(V, lambda e, q4=q4, vim=vim, cs_=cs_: e.tensor_tensor(out=q4[:], in0=vim[:], in1=cs_, op=ALU.mult), reads=[r_vim, r_pre], writes=[rq4])
                        st2[it] = dict(q1=q1, rq1=rq1, q2=q2, rq2=rq2, q3=q3, rq3=rq3, q4=q4, rq4=rq4)

                    def P2C(it):
                        ct, j = divmod(it, 4)
                        gp = it
                        d_ = st2.pop(it)
                        q1, rq1, q2, rq2, q3, rq3, q4, rq4 = d_['q1'], d_['rq1'], d_['q2'], d_['rq2'], d_['q3'], d_['rq3'], d_['q4'], d_['rq4']
                        if j == 0:
                            st2['y', ct] = ps_y.next()
                        y_ps, r_yps = st2['y', ct]
                        fw.op(T, lambda e, gp=gp, q1=q1, j=j, y_ps=y_ps: e.matmul(y_ps[:, 0:TC], lhsT=CT[0][:, gp, :], rhs=q1[:], start=(j == 0), stop=False), reads=[r_CT, rq1], writes=[r_yps])
                        fw.op(T, lambda e, gp=gp, q2=q2, y_ps=y_ps: e.matmul(y_ps[:, 0:TC], lhsT=CT[2][:, gp, :], rhs=q2[:], start=False, stop=False), reads=[r_CT, rq2], writes=[r_yps])
                        fw.op(T, lambda e, gp=gp, q3=q3, y_ps=y_ps: e.matmul(y_ps[:, 0:TC], lhsT=CT[1][:, gp, :], rhs=q3[:], start=False, stop=False), reads=[r_CT, rq3], writes=[r_yps])
                        fw.op(T, lambda e, gp=gp, q4=q4, j=j, y_ps=y_ps: e.matmul(y_ps[:, 0:TC], lhsT=CT[1][:, gp, :], rhs=q4[:], start=False, stop=(j == 3)), reads=[r_CT, rq4], writes=[r_yps])
                        if j == 3:
                            (yf, r_yf), (yq, r_yq), (sg, r_sg) = yfs.next(), ysq.next(), sgs.next()
                            fw.op(V, lambda e, yf=yf, ut=ut, ct=ct, y_ps=y_ps: e.scalar_tensor_tensor(out=yf[:], in0=ut[:, ct, :], scalar=dsk[:, ct:ct + 1], in1=y_ps[:, 0:TC], op0=ALU.mult, op1=ALU.add),
                                  reads=[r_ut, r_pre, r_yps], writes=[r_yf])
                            fw.op(A, lambda e, yq=yq, yf=yf: e.activation(out=yq[:], in_=yf[:], func=AF.Square), reads=[r_yf], writes=[r_yq])
                            fw.op(V, lambda e, yq=yq: e.tensor_scalar(out=yq[:], in0=yq[:], scalar1=0.044715, scalar2=1.0, op0=ALU.mult, op1=ALU.add), reads=[r_yq], writes=[r_yq])
                            fw.op(V, lambda e, yq=yq, yf=yf: e.tensor_tensor(out=yq[:], in0=yq[:], in1=yf[:], op=ALU.mult), reads=[r_yq, r_yf], writes=[r_yq])
                            fw.op(A, lambda e, sg=sg, yq=yq: e.activation(out=sg[:], in_=yq[:], func=AF.Sigmoid, scale=GELU_K), reads=[r_yq], writes=[r_sg])
                            fw.op(P, lambda e, yg=yg, ct=ct, yf=yf, sg=sg: e.tensor_tensor(out=yg[:, ct, :], in0=yf[:], in1=sg[:], op=ALU.mult), reads=[r_yf, r_sg], writes=[r_yg])

                    for it2 in range(16 + 2):
                        if it2 < 16:
                            P2A(it2)
                        if 0 <= it2 - 1 < 16:
                            P2B(it2 - 1)
                        if 0 <= it2 - 2 < 16:
                            P2C(it2 - 2)
                    zs, r_zs = zss.next()
                    for m in range(4):
                        v_ps, r_vps = ps_g.next()
                        g_ps, r_gps = ps_g.next()
                        for kt in range(4):
                            fw.op(T, lambda e, kt=kt, m=m, v_ps=v_ps, yg=yg: e.matmul(v_ps[:, 0:TC], lhsT=wg[:, kt, m * 128:(m + 1) * 128], rhs=yg[:, kt, :], start=(kt == 0), stop=(kt == 3)), reads=[r_wg, r_yg], writes=[r_vps])
                        for kt in range(4):
                            fw.op(T, lambda e, kt=kt, m=m, g_ps=g_ps, yg=yg: e.matmul(g_ps[:, 0:TC], lhsT=wg[:, kt, DC + m * 128:DC + (m + 1) * 128], rhs=yg[:, kt, :], start=(kt == 0), stop=(kt == 3)), reads=[r_wg, r_yg], writes=[r_gps])
                        sg, r_sg = sgs.next()
                        fw.op(A, lambda e, sg=sg, g_ps=g_ps: e.activation(out=sg[:], in_=g_ps[:, 0:TC], func=AF.Sigmoid), reads=[r_gps], writes=[r_sg])
                        fw.op(V, lambda e, zs=zs, m=m, v_ps=v_ps, sg=sg: e.tensor_tensor(out=zs[:, m, :], in0=v_ps[:, 0:TC], in1=sg[:], op=ALU.mult), reads=[r_vps, r_sg], writes=[r_zs])
                    fw.dma(P, zs_d.rearrange("(j p) s -> p j s", p=128)[:, :, t0:t0 + TC], zs[:], reads=[r_zs], writes=[Reg()])
            fw.es = es0


        def load_cast2(dst_fn, r_dst, src_fn, nk, ncols, stg):
            for kt in range(nk):
                rg = Reg()
                r_dst.append(rg)
                fw.dma(P, dst_fn(kt), src_fn(kt), writes=[rg])

        def final_ln(tt, r_tt, stats, mv, rstd, r_st, epst, r_c, lng, lnb, xo, r_xo, eng2=P):
            layer_norm_stats(tt, r_tt, stats, mv, rstd, r_st, epst, r_c)
            fw.op(V, lambda e: e.tensor_scalar(out=tt[:], in0=tt[:], scalar1=mv[:, 0:1], scalar2=rstd[:], op0=ALU.subtract, op1=ALU.mult), reads=[r_tt, r_st], writes=[r_tt])
            fw.op(eng2, lambda e: e.tensor_tensor(out=tt[:], in0=tt[:], in1=lng[:], op=ALU.mult), reads=[r_tt, r_c], writes=[r_tt])
            fw.op(eng2, lambda e: e.tensor_tensor(out=xo[:], in0=tt[:], in1=lnb[:], op=ALU.add), reads=[r_tt, r_c], writes=[r_xo])

        def p4(l, x_src, r_xsrc):
            with contextlib.ExitStack() as es:
                fw.es = es
                fw.barrier()
                ps_yb = [Rot([fw.ps(es, "p4y", [128, 512], F32)]) for i in range(3)]
                ps_g = Rot([fw.ps(es, "p4g", [128, 512], F32) for i in range(3)])
                ps_o = Rot([fw.ps(es, "p4o", [128, 512], F32) for i in range(2)])
                stg = None
                wgate = fw.sb([128, 8, 3 * D], BF16, "wgate")
                wup = [fw.sb([128, 4, D], BF16, f"wup{i}") for i in range(3)]
                wo = fw.sb([128, 8, D], BF16, "wo")
                r_w = RegList()
                for i, src in enumerate((w_up_conv, w_up_ssm, w_up_attn)):
                    load_cast2(lambda kt, i=i: wup[i][:, kt, :], r_w, lambda kt, src=src: src[l, kt * 128:(kt + 1) * 128, :], 4, D, stg)
                for cch in range(3):
                    load_cast2(lambda kt, cch=cch: wgate[:, kt, cch * D:(cch + 1) * D], r_w, lambda kt, cch=cch: w_in_g[l, kt * 128:(kt + 1) * 128, cch * D:(cch + 1) * D], 8, D, stg)
                load_cast2(lambda kt: wo[:, kt, :], r_w, lambda kt: w_o[l, kt * 128:(kt + 1) * 128, :], 8, D, stg)
                g1b = fw.sb([128, D], F32, "g1b")
                lng = fw.sb([128, D], F32, "lng")
                lnb = fw.sb([128, D], F32, "lnb")
                epst = fw.sb([128, 1], F32, "epst4")
                r_c = Reg()
                fw.dma(SY, g1b[:], ada_d[l:l + 1, 2 * D:3 * D].partition_broadcast(128), reads=[reg("ada")], writes=[r_c])
                fw.dma(SY, lng[:], ln_g[l, 0:1, :].partition_broadcast(128), writes=[r_c])
                fw.dma(SY, lnb[:], ln_b[l, 0:1, :].partition_broadcast(128), writes=[r_c])
                fw.op(V, lambda e: e.memset(epst[:], LN_EPS), writes=[r_c])
                hTs = Rot([fw.sb([128, 8, 512], BF16, "hT4") for _ in range(2)])
                zts = [Rot([fw.sb([128, 4, 512], BF16, f"z4{i}") for _ in range(2)]) for i in range(3)]
                mgs = Rot([fw.sb([128, 8, 512], BF16, "mg") for _ in range(2)])
                sgs = Rot([fw.sb([128, 512], F32, "sg4") for _ in range(4)])
                accs = Rot([fw.sb([128, 512], F32, "acc4") for _ in range(2)])
                tts = Rot([fw.sb([128, 512], F32, "tt4") for _ in range(3)])
                xts = Rot([fw.sb([128, D], F32, "xt4") for _ in range(2)])
                ybs = Rot([fw.sb([128, D], F32, "yb4") for _ in range(2)])
                xos = Rot([fw.sb([128, D], F32, "xo4") for _ in range(2)])
                stats = fw.sb([128, 2, 6], F32, "stats4")
                mv = fw.sb([128, 2], F32, "mv4")
                rstd = fw.sb([128, 1], F32, "rstd4")
                r_st = Reg()
                zsrc = ((zc_d, "zc"), (zs_d, "zs"), (oT_d, "oT"))
                for tg in range(NTG):
                    t0 = tg * 512
                    hT, r_hT = hTs.next()
                    fw.dma(SY, hT[:], hT_d.rearrange("(kt p) s -> p kt s", p=128)[:, :, t0:t0 + 512], reads=[reg("hT")], writes=[r_hT])
                    zt = []
                    for i in range(3):
                        z, r_z = zts[i].next()
                        fw.dma(SY, z[:], zsrc[i][0].rearrange("(j p) s -> p j s", p=128)[:, :, t0:t0 + 512], reads=[reg(zsrc[i][1])], writes=[r_z])
                        zt.append((z, r_z))
                    mg, r_mg = mgs.next()
                    for m in range(8):
                        yps = []
                        for i in range(3):
                            y_ps, r_yps = ps_yb[i].next()
                            z, r_z = zt[i]
                            for kt in range(4):
                                fw.op(T, lambda e, kt=kt, i=i, m=m, y_ps=y_ps, z=z: e.matmul(y_ps[:], lhsT=wup[i][:, kt, m * 128:(m + 1) * 128], rhs=z[:, kt, :], start=(kt == 0), stop=(kt == 3)),
                                      reads=[r_w, r_z], writes=[r_yps])
                            yps.append((y_ps, r_yps))
                        sg_l = []
                        for i in range(3):
                            g_ps, r_gps = ps_g.next()
                            for kt in range(8):
                                fw.op(T, lambda e, kt=kt, i=i, m=m, g_ps=g_ps, hT=hT: e.matmul(g_ps[:], lhsT=wgate[:, kt, i * D + m * 128:i * D + (m + 1) * 128], rhs=hT[:, kt, :], start=(kt == 0), stop=(kt == 7)),
                                      reads=[r_w, r_hT], writes=[r_gps])
                            sg, r_sg = sgs.next()
                            fw.op(A, lambda e, sg=sg, g_ps=g_ps: e.activation(out=sg[:], in_=g_ps[:], func=AF.Sigmoid), reads=[r_gps], writes=[r_sg])
                            sg_l.append((sg, r_sg))
                        acc, r_acc = accs.next()
                        ta_, r_ta_ = tts.next()
                        tb2, r_tb2 = tts.next()
                        fw.op(V, lambda e, acc=acc: e.tensor_tensor(out=acc[:], in0=yps[0][0][:], in1=sg_l[0][0][:], op=ALU.mult), reads=[yps[0][1], sg_l[0][1]], writes=[r_acc])
                        fw.op(V, lambda e, ta_=ta_: e.tensor_tensor(out=ta_[:], in0=yps[1][0][:], in1=sg_l[1][0][:], op=ALU.mult), reads=[yps[1][1], sg_l[1][1]], writes=[r_ta_])
                        fw.op(V, lambda e, tb2=tb2: e.tensor_tensor(out=tb2[:], in0=yps[2][0][:], in1=sg_l[2][0][:], op=ALU.mult), reads=[yps[2][1], sg_l[2][1]], writes=[r_tb2])
                        fw.op(P, lambda e, acc=acc, ta_=ta_: e.tensor_tensor(out=acc[:], in0=acc[:], in1=ta_[:], op=ALU.add), reads=[r_acc, r_ta_], writes=[r_acc])
                        fw.op(P, lambda e, acc=acc, tb2=tb2, mg=mg, m=m: e.tensor_tensor(out=mg[:, m, :], in0=acc[:], in1=tb2[:], op=ALU.add), reads=[r_acc, r_tb2], writes=[r_mg])
                    for tb in range(4):
                        row0 = t0 + tb * 128
                        xt, r_xt = xts.next()
                        fw.dma(SY, xt[:], x_src[row0:row0 + 128, :], reads=[r_xsrc], writes=[r_xt])
                        yb, r_yb = ybs.next()
                        for hh in range(2):
                            o_ps, r_ops = ps_o.next()
                            for kt in range(8):
                                fw.op(T, lambda e, kt=kt, hh=hh, tb=tb, o_ps=o_ps, mg=mg: e.matmul(o_ps[:], lhsT=mg[:, kt, tb * 128:(tb + 1) * 128], rhs=wo[:, kt, hh * 512:(hh + 1) * 512], start=(kt == 0), stop=(kt == 7)),
                                      reads=[r_w, r_mg], writes=[r_ops])
                            fw.op(V, lambda e, hh=hh, o_ps=o_ps, yb=yb: e.tensor_tensor(out=yb[:, hh * 512:(hh + 1) * 512], in0=o_ps[:], in1=g1b[:, hh * 512:(hh + 1) * 512], op=ALU.mult), reads=[r_ops, r_c], writes=[r_yb])
                        fw.op(V, lambda e, xt=xt, yb=yb: e.scalar_tensor_tensor(out=yb[:], in0=xt[:], scalar=ALPHA, in1=yb[:], op0=ALU.mult, op1=ALU.add), reads=[r_xt, r_yb], writes=[r_yb])
                        xo, r_xo = xos.next()
                        final_ln(yb, r_yb, stats, mv, rstd, r_st, epst, r_c, lng, lnb, xo, r_xo)
                        fw.dma(P, x1_d[row0:row0 + 128, :], xo[:], reads=[r_xo], writes=[Reg()])
            fw.es = es0

        def p5_dense(l, x_dst, r_xdst):
            NH = max(1, S // 2048)
            HALF = S // NH
            NTBH = HALF // 128
            NTGH = HALF // 512
            with contextlib.ExitStack() as es:
                fw.es = es
                fw.barrier()
                sc = fw.sb([128, 8], F32, "sc5")
                sh = fw.sb([128, 8], F32, "sh5")
                g2b = fw.sb([128, D], F32, "g2b")
                lng = fw.sb([128, D], F32, "lng5")
                lnb = fw.sb([128, D], F32, "lnb5")
                rbb = fw.sb([128, NE], F32, "rbb")
                rw32 = fw.sb([128, 8, NE], F32, "rw32")
                epst = fw.sb([128, 1], F32, "epst5")
                r_c = Reg()
                fw.dma(SY, sh[:], ada_d[l, 3 * D:4 * D].rearrange("(kt p) -> p kt", p=128), reads=[reg("ada")], writes=[r_c], allow_slow_non_contiguous=True)
                fw.dma(SY, sc[:], ada_d[l, 4 * D:5 * D].rearrange("(kt p) -> p kt", p=128), reads=[reg("ada")], writes=[r_c], allow_slow_non_contiguous=True)
                fw.dma(SY, g2b[:], ada_d[l:l + 1, 5 * D:6 * D].partition_broadcast(128), reads=[reg("ada")], writes=[r_c])
                fw.dma(SY, lng[:], ln_g[l, 1:2, :].partition_broadcast(128), writes=[r_c])
                fw.dma(SY, lnb[:], ln_b[l, 1:2, :].partition_broadcast(128), writes=[r_c])
                fw.dma(SY, rbb[:], router_bias[0:1, :].partition_broadcast(128), writes=[r_c])
                fw.dma(SY, rw32[:], router_w.rearrange("(kt p) e -> p kt e", p=128), writes=[r_c])
                fw.op(V, lambda e: e.tensor_scalar(out=sc[:], in0=sc[:], scalar1=1.0, scalar2=None, op0=ALU.add), reads=[r_c], writes=[r_c])
                fw.op(V, lambda e: e.memset(epst[:], LN_EPS), writes=[r_c])
                h2T = fw.sb([128, 8, HALF], BF16, "h2T")
                r_h2T = Reg()
                Gd = fw.sb([128, NTBH, NE], F32, "Gd")
                r_Gd = Reg()
                yacc = fw.sb([128, NTBH, D], F32, "yacc")
                r_yacc = [Reg() for _ in range(NTBH)]
                stats = fw.sb([128, 2, 6], F32, "stats5")
                mv = fw.sb([128, 2], F32, "mv5")
                rstd = fw.sb([128, 1], F32, "rstd5")
                r_st = Reg()
                for hf in range(NH):
                    tok0 = hf * HALF
                    with contextlib.ExitStack() as esA:
                        fw.es = esA
                        fw.barrier()
                        ps_tp = Rot([fw.ps(esA, "p5tp", [128, 8, 128], F32) for i in range(2)])
                        ps_lg = Rot([fw.ps(esA, "p5lg", [128, NE], F32) for i in range(2)])
                        xts = Rot([fw.sb([128, D], F32, "xt5") for _ in range(3)])
                        xns = Rot([fw.sb([128, D], F32, "xn5") for _ in range(2)])
                        h32s = Rot([fw.sb([128, 8, 128], F32, "h32") for _ in range(2)])
                        rt = {k: fw.sb([128, NE], F32, "rt" + k) for k in ("s", "sel", "eq", "sel2", "ge2", "mask", "ws")}
                        r8 = {k: fw.sb([128, 8], F32, "r8" + k) for k in ("m1", "m2", "gs", "gsel")}
                        r1 = {k: fw.sb([128, 1], F32, "r1" + k) for k in ("gmax", "den")}
                        r_rt = Reg()
                        for tbh in range(NTBH):
                            row0 = tok0 + tbh * 128
                            xt, r_xt = xts.next()
                            fw.dma(SY, xt[:], x1_d[row0:row0 + 128, :], reads=[reg("x1")], writes=[r_xt])
                            layer_norm_stats(xt, r_xt, stats, mv, rstd, r_st, epst, r_c)
                            xn, r_xn = xns.next()
                            fw.op(V, lambda e, xn=xn, xt=xt: e.tensor_scalar(out=xn[:], in0=xt[:], scalar1=mv[:, 0:1], scalar2=rstd[:], op0=ALU.subtract, op1=ALU.mult), reads=[r_xt, r_st], writes=[r_xn])
                            tp, r_tp = ps_tp.next()
                            for kt in range(8):
                                fw.op(T, lambda e, kt=kt, tp=tp, xn=xn: e.transpose(out=tp[:, kt, :], in_=xn[:, kt * 128:(kt + 1) * 128], identity=identf[:]), reads=[r_xn, rc_const], writes=[r_tp])
                            h32, r_h32 = h32s.next()
                            for kt in range(8):
                                fw.op(A, lambda e, kt=kt, tp=tp, h32=h32: e.activation(out=h32[:, kt, :], in_=tp[:, kt, :], func=AF.Identity, scale=sc[:, kt:kt + 1], bias=sh[:, kt:kt + 1]), reads=[r_tp, r_c], writes=[r_h32])
                            fw.op(P, lambda e, h32=h32, tbh=tbh: e.tensor_copy(out=h2T[:, :, tbh * 128:(tbh + 1) * 128], in_=h32[:]), reads=[r_h32], writes=[r_h2T])
                            lg, r_lg = ps_lg.next()
                            for kt in range(8):
                                fw.op(T, lambda e, kt=kt, lg=lg, h32=h32: e.matmul(lg[:], lhsT=h32[:, kt, :], rhs=rw32[:, kt, :], start=(kt == 0), stop=(kt == 7)), reads=[r_h32, r_c], writes=[r_lg])
                            rr_ = [r_rt]
                            v3 = lambda a: a[:].rearrange("p (g k) -> p g k", k=4)
                            b3 = lambda a: a[:].unsqueeze(2).to_broadcast([128, 8, 4])
                            fw.op(A, lambda e, lg=lg: e.activation(out=rt["s"][:], in_=lg[:], func=AF.Sigmoid), reads=[r_lg], writes=rr_)
                            fw.op(V, lambda e: e.tensor_tensor(out=rt["sel"][:], in0=rt["s"][:], in1=rbb[:], op=ALU.add), reads=rr_ + [r_c], writes=rr_)
                            fw.op(V, lambda e: e.tensor_reduce(out=r8["m1"][:], in_=v3(rt["sel"]), axis=AX.X, op=ALU.max), reads=rr_, writes=rr_)
                            fw.op(V, lambda e: e.tensor_tensor(out=v3(rt["eq"]), in0=v3(rt["sel"]), in1=b3(r8["m1"]), op=ALU.is_equal), reads=rr_, writes=rr_)
                            fw.op(V, lambda e: e.scalar_tensor_tensor(out=rt["sel2"][:], in0=rt["eq"][:], scalar=-1.0e9, in1=rt["sel"][:], op0=ALU.mult, op1=ALU.add), reads=rr_, writes=rr_)
                            fw.op(V, lambda e: e.tensor_reduce(out=r8["m2"][:], in_=v3(rt["sel2"]), axis=AX.X, op=ALU.max), reads=rr_, writes=rr_)
                            fw.op(V, lambda e: e.tensor_tensor(out=r8["gs"][:], in0=r8["m1"][:], in1=r8["m2"][:], op=ALU.add), reads=rr_, writes=rr_)
                            fw.op(V, lambda e: e.tensor_reduce(out=r1["gmax"][:], in_=r8["gs"][:], axis=AX.X, op=ALU.max), reads=rr_, writes=rr_)
                            fw.op(V, lambda e: e.tensor_scalar(out=r8["gsel"][:], in0=r8["gs"][:], scalar1=r1["gmax"][:], scalar2=None, op0=ALU.is_equal), reads=rr_, writes=rr_)
                            fw.op(V, lambda e: e.tensor_tensor(out=v3(rt["ge2"]), in0=v3(rt["sel"]), in1=b3(r8["m2"]), op=ALU.is_ge), reads=rr_, writes=rr_)
                            fw.op(V, lambda e: e.tensor_tensor(out=v3(rt["mask"]), in0=v3(rt["ge2"]), in1=b3(r8["gsel"]), op=ALU.mult), reads=rr_, writes=rr_)
                            fw.op(V, lambda e: e.tensor_tensor(out=rt["ws"][:], in0=rt["mask"][:], in1=rt["s"][:], op=ALU.mult), reads=rr_, writes=rr_)
                            fw.op(V, lambda e: e.tensor_reduce(out=r1["den"][:], in_=rt["ws"][:], axis=AX.X, op=ALU.add), reads=rr_, writes=rr_)
                            fw.op(V, lambda e: e.reciprocal(out=r1["den"][:], in_=r1["den"][:]), reads=rr_, writes=rr_)
                            fw.op(V, lambda e, tbh=tbh: e.tensor_scalar(out=Gd[:, tbh, :], in0=rt["ws"][:], scalar1=r1["den"][:], scalar2=None, op0=ALU.mult), reads=rr_, writes=[r_Gd])
                        if "d_G" in dbg_aps and hf == 0 and stop == f"p5_{l}":
                            fw.dma(SY, dbg_aps["d_G"].rearrange("(a p) e -> p a e", p=128)[:, 0:NTBH, :], Gd[:], reads=[r_Gd], writes=[reg("dbg")])
                    with contextlib.ExitStack() as esB:
                        fw.es = esB
                        fw.barrier()
                        ps_h1 = Rot([fw.ps(esB, "p5h1", [128, 512], F32) for i in range(2)])
                        ps_h3 = Rot([fw.ps(esB, "p5h3", [128, 512], F32) for i in range(2)])
                        ps_y = Rot([fw.ps(esB, "p5y", [128, 512], F32) for i in range(4)])
                        st1 = Rot([fw.sb([128, 8, DE], F32, "st1") for _ in range(2)])
                        st3 = Rot([fw.sb([128, 8, DE], F32, "st3") for _ in range(2)])
                        st2 = Rot([fw.sb([128, 2, D], F32, "st2") for _ in range(2)])
                        w1bs = Rot([fw.sb([128, 8, DE], BF16, "w1b") for _ in range(2)])
                        w3bs = Rot([fw.sb([128, 8, DE], BF16, "w3b") for _ in range(2)])
                        w2bs = Rot([fw.sb([128, 2, D], BF16, "w2b") for _ in range(2)])
                        sgs = Rot([fw.sb([128, 512], F32, "sg5") for _ in range(2)])
                        aTs = Rot([fw.sb([128, 2, 512], BF16, "aT") for _ in range(2)])
                        for ei in range(NE + 1):
                            e_id = ei - 1
                            if ei == 0:
                                s1, s3, s2 = shared_w1[l], shared_w3[l], shared_w2[l]
                            else:
                                s1, s3, s2 = exp_w1[l, e_id], exp_w3[l, e_id], exp_w2[l, e_id]
                            (a1, ra1), (a3, ra3), (a2, ra2) = st1.next(), st3.next(), st2.next()
                            fw.dma(SY, a1[:], s1.rearrange("(kt p) n -> p kt n", p=128), writes=[ra1])
                            fw.dma(SY, a3[:], s3.rearrange("(kt p) n -> p kt n", p=128), writes=[ra3])
                            fw.dma(SY, a2[:], s2.rearrange("(kt p) n -> p kt n", p=128), writes=[ra2])
                            (w1b, rw1), (w3b, rw3), (w2b, rw2) = w1bs.next(), w3bs.next(), w2bs.next()
                            fw.op(P, lambda e, w1b=w1b, a1=a1: e.tensor_copy(out=w1b[:], in_=a1[:]), reads=[ra1], writes=[rw1])
                            fw.op(P, lambda e, w3b=w3b, a3=a3: e.tensor_copy(out=w3b[:], in_=a3[:]), reads=[ra3], writes=[rw3])
                            fw.op(A, lambda e, w2b=w2b, a2=a2: e.activation(out=w2b[:], in_=a2[:], func=AF.Identity), reads=[ra2], writes=[rw2])
                            for tg in range(NTGH):
                                aT, r_aT = aTs.next()
                                for m in range(2):
                                    h1, r_h1 = ps_h1.next()
                                    h3, r_h3 = ps_h3.next()
                                    for kt in range(8):
                                        fw.op(T, lambda e, kt=kt, m=m, h1=h1, w1b=w1b, tg=tg: e.matmul(h1[:], lhsT=w1b[:, kt, m * 128:(m + 1) * 128], rhs=h2T[:, kt, tg * 512:(tg + 1) * 512], start=(kt == 0), stop=(kt == 7)),
                                              reads=[rw1, r_h2T], writes=[r_h1])
                                    for kt in range(8):
                                        fw.op(T, lambda e, kt=kt, m=m, h3=h3, w3b=w3b, tg=tg: e.matmul(h3[:], lhsT=w3b[:, kt, m * 128:(m + 1) * 128], rhs=h2T[:, kt, tg * 512:(tg + 1) * 512], start=(kt == 0), stop=(kt == 7)),
                                              reads=[rw3, r_h2T], writes=[r_h3])
                                    sg, r_sg = sgs.next()
                                    fw.op(A, lambda e, sg=sg, h1=h1: e.activation(out=sg[:], in_=h1[:], func=AF.Silu), reads=[r_h1], writes=[r_sg])
                                    fw.op(V, lambda e, sg=sg, h3=h3, aT=aT, m=m: e.tensor_tensor(out=aT[:, m, :], in0=h3[:], in1=sg[:], op=ALU.mult), reads=[r_h3, r_sg], writes=[r_aT])
                                for tb in range(4):
                                    tbh = tg * 4 + tb
                                    for hh in range(2):
                                        y_ps, r_yps = ps_y.next()
                                        for kt in range(2):
                                            fw.op(T, lambda e, kt=kt, hh=hh, tb=tb, y_ps=y_ps, aT=aT, w2b=w2b: e.matmul(y_ps[:], lhsT=aT[:, kt, tb * 128:(tb + 1) * 128], rhs=w2b[:, kt, hh * 512:(hh + 1) * 512], start=(kt == 0), stop=(kt == 1)),
                                                  reads=[rw2, r_aT], writes=[r_yps])
                                        ya = yacc[:, tbh, hh * 512:(hh + 1) * 512]
                                        if ei == 0:
                                            fw.op(V, lambda e, ya=ya, y_ps=y_ps: e.tensor_copy(out=ya, in_=y_ps[:]), reads=[r_yps], writes=[r_yacc[tbh]])
                                        else:
                                            fw.op(V, lambda e, ya=ya, y_ps=y_ps, tbh=tbh, e_id=e_id: e.scalar_tensor_tensor(out=ya, in0=y_ps[:], scalar=Gd[:, tbh, e_id:e_id + 1], in1=ya, op0=ALU.mult, op1=ALU.add),
                                                  reads=[r_yps, r_Gd, r_yacc[tbh]], writes=[r_yacc[tbh]])
                    with contextlib.ExitStack() as esC:
                        fw.es = esC
                        fw.barrier()
                        xts = Rot([fw.sb([128, D], F32, "xt5c") for _ in range(2)])
                        xos = Rot([fw.sb([128, D], F32, "xo5") for _ in range(2)])
                        for tbh in range(NTBH):
                            row0 = tok0 + tbh * 128
                            xt, r_xt = xts.next()
                            fw.dma(SY, xt[:], x1_d[row0:row0 + 128, :], reads=[reg("x1")], writes=[r_xt])
                            ya = yacc[:, tbh, :]
                            r_ya = r_yacc[tbh]
                            fw.op(V, lambda e, ya=ya: e.tensor_tensor(out=ya, in0=ya, in1=g2b[:], op=ALU.mult), reads=[r_ya, r_c], writes=[r_ya])
                            fw.op(V, lambda e, ya=ya, xt=xt: e.scalar_tensor_tensor(out=ya, in0=xt[:], scalar=ALPHA, in1=ya, op0=ALU.mult, op1=ALU.add), reads=[r_xt, r_ya], writes=[r_ya])
                            xo, r_xo = xos.next()
                            final_ln(ya, r_ya, stats, mv, rstd, r_st, epst, r_c, lng, lnb, xo, r_xo)
                            fw.dma(P, x_dst[row0:row0 + 128, :], xo[:], reads=[r_xo], writes=[Reg()])
                    fw.es = es
            fw.es = es0


        import os as _os2
        CAPB = int(_os2.environ.get("KCAPB", "3"))
        CAP = CAPB * 128
        NSB = NE * CAPB
        NOV = 2 * ((S - CAP + 127) // 128) if S > CAP else 0
        NBLK = NSB + NOV
        NROW = NBLK * 128
        RW = 1032

        def idma(out, out_off, in_, in_off, reads=(), writes=(), **kw):
            E = fw.E[P]
            if KSIM:
                ds_ = [fw.new_sem(), 0]
                fw.dsems.append(ds_)
            else:
                ds_ = fw.dsems[fw.dsi]
                fw.dsi = (fw.dsi + 1) % NDS
            toks = fw._deps(reads, writes)
            if ds_[1] > 0:
                toks.append((ds_[0], ds_[1]))
            fw._wait(E, toks)
            if ds_[1] >= SEM_ROT:
                ds_[0] = fw.new_sem()
                ds_[1] = 0
            ds_[1] += 16
            oo = None if out_off is None else bass.IndirectOffsetOnAxis(ap=out_off, axis=0)
            io = None if in_off is None else bass.IndirectOffsetOnAxis(ap=in_off, axis=0)
            E.h.indirect_dma_start(out=out, out_offset=oo, in_=in_, in_offset=io, **kw).then_inc(ds_[0], 16)
            tok = (ds_[0], ds_[1])
            k = id(ds_[0])
            for t in fw._flat(reads):
                t.r[k] = tok
            for t in fw._flat(writes):
                t.w = tok
                t.r = {}

        def p5(l, x_dst, r_xdst):
            with contextlib.ExitStack() as es:
                fw.es = es
                fw.barrier()
                fw.cut_on = True
                scb = fw.sb([128, D], F32, "scb")
                shb = fw.sb([128, D], F32, "shb")
                g2b = fw.sb([128, D], F32, "g2b")
                lng = fw.sb([128, D], F32, "lng5")
                lnb = fw.sb([128, D], F32, "lnb5")
                rbb = fw.sb([128, NE], F32, "rbb")
                rw32 = fw.sb([128, 8, NE], F32, "rw32")
                epst = fw.sb([128, 1], F32, "epst5")
                trisb = fw.sb([128, 128], BF16, "trisb")
                iotp = fw.sb([128, 1], F32, "iotp")
                jv = fw.sb([128, max(NOV, 1)], F32, "jv")
                r_c = Reg()
                fw.dma(SY, shb[:], ada_d[l:l + 1, 3 * D:4 * D].partition_broadcast(128), writes=[r_c])
                fw.dma(SY, scb[:], ada_d[l:l + 1, 4 * D:5 * D].partition_broadcast(128), writes=[r_c])
                fw.dma(SY, g2b[:], ada_d[l:l + 1, 5 * D:6 * D].partition_broadcast(128), writes=[r_c])
                fw.dma(SY, lng[:], ln_g[l, 1:2, :].partition_broadcast(128), writes=[r_c])
                fw.dma(SY, lnb[:], ln_b[l, 1:2, :].partition_broadcast(128), writes=[r_c])
                fw.dma(SY, rbb[:], router_bias[0:1, :].partition_broadcast(128), writes=[r_c])
                fw.dma(SY, rw32[:], router_w.rearrange("(kt p) e -> p kt e", p=128), writes=[r_c])
                fw.dma(SY, trisb[:], tris_bf[:, :], writes=[r_c])
                fw.dma(SY, iotp[:], iota_p[:, :], writes=[r_c])
                fw.dma(SY, jv[:], iota_f[:, 0:max(NOV, 1)], writes=[r_c])
                fw.op(V, lambda e: e.tensor_scalar(out=scb[:], in0=scb[:], scalar1=1.0, scalar2=None, op0=ALU.add), reads=[r_c], writes=[r_c])
                jv0 = fw.sb([128, NE], F32, "jv0")
                fw.dma(SY, jv0[:], iota_f[:, 0:NE], writes=[r_c])
                fw.op(V, lambda e: e.tensor_scalar(out=jv[:], in0=jv[:], scalar1=128.0, scalar2=None, op0=ALU.mult), reads=[r_c], writes=[r_c])
                fw.op(V, lambda e: e.memset(epst[:], LN_EPS), writes=[r_c])
                Gd = fw.sb([128, NTB, NE], F32, "Gd")
                Mk = fw.sb([128, NTB, NE], F32, "Mk")
                Mb = fw.sb([128, NTB, NE], BF16, "Mb")
                r_G = Reg()
                idx2 = fw.sb([128, NTB, 2], I32, "idx2")
                w2t = fw.sb([128, NTB, 2], F32, "w2t")
                idxw = fw.sb([128, max(NOV, 1)], I32, "idxw")
                r_idx = Reg()
                stats = fw.sb([128, 2, 6], F32, "stats5")
                mv = fw.sb([128, 2], F32, "mv5")
                rstd = fw.sb([128, 1], F32, "rstd5")
                r_st = Reg()
                with contextlib.ExitStack() as esA:
                    fw.es = esA
                    ps_tp = Rot([fw.ps(esA, "p5tp", [128, 8, 128], F32) for i in range(1)])
                    ps_lg = Rot([fw.ps(esA, "p5lg", [128, 4, NE], F32) for i in range(2)])
                    ps_h = Rot([fw.ps(esA, "p5h", [128, 512], F32) for i in range(2)])
                    ps_y = Rot([fw.ps(esA, "p5y", [128, 512], F32) for i in range(2)])
                    ws1 = fw.sb([128, 8, DE], BF16, "ws1")
                    ws3 = fw.sb([128, 8, DE], BF16, "ws3")
                    ws2 = fw.sb([128, 2, D], BF16, "ws2")
                    r_ws = Reg()
                    import os as _os
                    if "sh" not in _os.environ.get("KSKIP", ""):
                        fw.dma(P, ws1[:], shared_w1[l].rearrange("(kt p) n -> p kt n", p=128), writes=[r_ws])
                        fw.dma(P, ws3[:], shared_w3[l].rearrange("(kt p) n -> p kt n", p=128), writes=[r_ws])
                        fw.dma(P, ws2[:], shared_w2[l].rearrange("(kt p) n -> p kt n", p=128), writes=[r_ws])
                    xts = Rot([fw.sb([128, D], F32, "xt5") for _ in range(3)])
                    h2fs = Rot([fw.sb([128, D], F32, "h2f") for _ in range(3)])
                    rows = Rot([fw.sb([128, RW], BF16, "rowb") for _ in range(2)])
                    h32s = Rot([fw.sb([128, 8, 128], F32, "h32") for _ in range(2)])
                    h2Ts = Rot([fw.sb([128, 8, 512], BF16, "h2Tg") for _ in range(2)])
                    sgs = Rot([fw.sb([128, 512], F32, "sg5") for _ in range(2)])
                    aTs = Rot([fw.sb([128, 2, 512], BF16, "aT5") for _ in range(2)])
                    yshs = Rot([fw.sb([128, D], F32, "ysh") for _ in range(2)])
                    rt = {k: fw.sb([128, 4 * NE], F32, "rt" + k) for k in ("s", "sel", "eq", "sel2", "ge2", "ws")}
                    r8 = {k: fw.sb([128, 32], F32, "r8" + k) for k in ("m1", "m2", "gs", "gsel")}
                    r1 = {k: fw.sb([128, 4], F32, "r1" + k) for k in ("gmax", "den")}
                    lgs5 = {}
                    r_rt = Reg()
                    grp5 = {}
                    blk5 = {}

                    def A_S1(tbg):
                        tg, tb = divmod(tbg, 4)
                        if tb == 0:
                            grp5[tg] = h2Ts.next()
                        h2T, r_h2T = grp5[tg]
                        row0 = tbg * 128
                        xt, r_xt = xts.next()
                        fw.dma(SY, xt[:], x1_d[row0:row0 + 128, :], writes=[r_xt])
                        layer_norm_stats(xt, r_xt, stats, mv, rstd, r_st, epst, r_c)
                        h2f, r_h2f = h2fs.next()
                        fw.op(V, lambda e, h2f=h2f, xt=xt: e.tensor_scalar(out=h2f[:], in0=xt[:], scalar1=mv[:, 0:1], scalar2=rstd[:], op0=ALU.subtract, op1=ALU.mult), reads=[r_xt, r_st], writes=[r_h2f])
                        fw.op(V, lambda e, h2f=h2f: e.tensor_tensor(out=h2f[:], in0=h2f[:], in1=scb[:], op=ALU.mult), reads=[r_h2f, r_c], writes=[r_h2f])
                        fw.op(V, lambda e, h2f=h2f: e.tensor_tensor(out=h2f[:], in0=h2f[:], in1=shb[:], op=ALU.add), reads=[r_h2f, r_c], writes=[r_h2f])
                        rowb, r_rowb = rows.next()
                        fw.op(A, lambda e, rowb=rowb, h2f=h2f: e.activation(out=rowb[:, 0:D], in_=h2f[:], func=AF.Identity), reads=[r_h2f], writes=[r_rowb])
                        if "h2b" not in _os.environ.get("KSKIP", ""):
                            fw.op(P, lambda e, rowb=rowb: e.memset(rowb[:, D:RW], 0.0), writes=[r_rowb])
                            fw.dma(P, h2b_d[row0:row0 + 128, :], rowb[:], reads=[r_rowb], writes=[Reg()])
                        blk5["a", tbg] = (h2f, r_h2f)

                    def A_S1b(tbg):
                        tg, tb = divmod(tbg, 4)
                        h2T, r_h2T = grp5[tg]
                        h2f, r_h2f = blk5.pop(("a", tbg))
                        tp, r_tp = ps_tp.next()
                        for kt in range(8):
                            fw.op(T, lambda e, kt=kt, tp=tp, h2f=h2f: e.transpose(out=tp[:, kt, :], in_=h2f[:, kt * 128:(kt + 1) * 128], identity=identf[:]), reads=[r_h2f, rc_const], writes=[r_tp])
                        h32, r_h32 = h32s.next()
                        for hb in range(2):
                            fw.op(A, lambda e, tp=tp, h32=h32, hb=hb: e.activation(out=h32[:, hb * 4:hb * 4 + 4, :].rearrange("p a b -> p (a b)"), in_=tp[:, hb * 4:hb * 4 + 4, :].rearrange("p a b -> p (a b)"), func=AF.Identity), reads=[r_tp], writes=[r_h32])
                        fw.op(V, lambda e, h32=h32, h2T=h2T, tb=tb: e.tensor_copy(out=h2T[:, :, tb * 128:(tb + 1) * 128], in_=h32[:]), reads=[r_h32], writes=[r_h2T])
                        blk5[tbg] = (h32, r_h32)

                    def A_S2(tbg):
                        h32, r_h32 = blk5.pop(tbg)
                        tg, tb = divmod(tbg, 4)
                        if tb == 0:
                            lgs5[tg] = ps_lg.next()
                        lg, r_lg = lgs5[tg]
                        for kt in range(8):
                            fw.op(T, lambda e, kt=kt: e.matmul(lg[:, tb, :], lhsT=h32[:, kt, :], rhs=rw32[:, kt, :], start=(tb == 0 and kt == 0), stop=(kt == 7), skip_group_check=True), reads=[r_h32, r_c], writes=[r_lg])

                    def A_RT(tg):
                        lg, r_lg = lgs5.pop(tg)
                        rr_ = [r_rt]
                        v3 = lambda a: a.rearrange("p (g k) -> p g k", k=4)
                        b3 = lambda a: a.unsqueeze(2).to_broadcast([128, 32, 4])
                        t3 = lambda a: a.rearrange("p (t e) -> p t e", t=4)
                        g3_ = lambda a: a.rearrange("p (t g) -> p t g", t=4)
                        Mk4 = Mk[:, tg * 4:(tg + 1) * 4, :]
                        Gd4 = Gd[:, tg * 4:(tg + 1) * 4, :]
                        Mk2 = Mk4.rearrange("p t e -> p (t e)")
                        fw.op(A, lambda e: e.activation(out=rt["s"][:], in_=lg[:].rearrange("p t e -> p (t e)"), func=AF.Sigmoid), reads=[r_lg], writes=rr_)
                        fw.op(V, lambda e: e.tensor_tensor(out=t3(rt["sel"][:]), in0=t3(rt["s"][:]), in1=rbb[:].unsqueeze(1).to_broadcast([128, 4, NE]), op=ALU.add), reads=rr_ + [r_c], writes=rr_)
                        fw.op(V, lambda e: e.tensor_reduce(out=r8["m1"][:], in_=v3(rt["sel"][:]), axis=AX.X, op=ALU.max), reads=rr_, writes=rr_)
                        fw.op(V, lambda e: e.tensor_tensor(out=v3(rt["eq"][:]), in0=v3(rt["sel"][:]), in1=b3(r8["m1"][:]), op=ALU.is_equal), reads=rr_, writes=rr_)
                        fw.op(V, lambda e: e.scalar_tensor_tensor(out=rt["sel2"][:], in0=rt["eq"][:], scalar=-1.0e9, in1=rt["sel"][:], op0=ALU.mult, op1=ALU.add), reads=rr_, writes=rr_)
                        fw.op(V, lambda e: e.tensor_reduce(out=r8["m2"][:], in_=v3(rt["sel2"][:]), axis=AX.X, op=ALU.max), reads=rr_, writes=rr_)
                        fw.op(V, lambda e: e.tensor_tensor(out=r8["gs"][:], in0=r8["m1"][:], in1=r8["m2"][:], op=ALU.add), reads=rr_, writes=rr_)
                        fw.op(V, lambda e: e.tensor_reduce(out=r1["gmax"][:], in_=g3_(r8["gs"][:]), axis=AX.X, op=ALU.max), reads=rr_, writes=rr_)
                        fw.op(V, lambda e: e.tensor_tensor(out=g3_(r8["gsel"][:]), in0=g3_(r8["gs"][:]), in1=r1["gmax"][:].unsqueeze(2).to_broadcast([128, 4, 8]), op=ALU.is_equal), reads=rr_, writes=rr_)
                        fw.op(V, lambda e: e.tensor_tensor(out=v3(rt["ge2"][:]), in0=v3(rt["sel"][:]), in1=b3(r8["m2"][:]), op=ALU.is_ge), reads=rr_, writes=rr_)
                        fw.op(V, lambda e: e.tensor_tensor(out=v3(Mk2), in0=v3(rt["ge2"][:]), in1=b3(r8["gsel"][:]), op=ALU.mult), reads=rr_, writes=rr_ + [r_G])
                        fw.op(V, lambda e: e.tensor_tensor(out=rt["ws"][:], in0=Mk2, in1=rt["s"][:], op=ALU.mult), reads=rr_ + [r_G], writes=rr_)
                        fw.op(V, lambda e: e.tensor_reduce(out=r1["den"][:], in_=t3(rt["ws"][:]), axis=AX.X, op=ALU.add), reads=rr_, writes=rr_)
                        fw.op(V, lambda e: e.reciprocal(out=r1["den"][:], in_=r1["den"][:]), reads=rr_, writes=rr_)
                        fw.op(V, lambda e: e.tensor_tensor(out=Gd4, in0=t3(rt["ws"][:]), in1=r1["den"][:].unsqueeze(2).to_broadcast([128, 4, NE]), op=ALU.mult), reads=rr_, writes=[r_G])

                    def A_SH(tg):
                        h2T, r_h2T = grp5.pop(tg)
                        aT, r_aT = aTs.next()
                        import os as _os
                        _sk = _os.environ.get("KSKIP", "")
                        for m in (range(2) if "sh" not in _sk else []):
                            h1, r_h1 = ps_h.next()
                            h3, r_h3 = ps_h.next()
                            for kt in range(8):
                                fw.op(T, lambda e, kt=kt, m=m, h1=h1, h2T=h2T: e.matmul(h1[:], lhsT=ws1[:, kt, m * 128:(m + 1) * 128], rhs=h2T[:, kt, :], start=(kt == 0), stop=(kt == 7)), reads=[r_ws, r_h2T], writes=[r_h1])
                            for kt in range(8):
                                fw.op(T, lambda e, kt=kt, m=m, h3=h3, h2T=h2T: e.matmul(h3[:], lhsT=ws3[:, kt, m * 128:(m + 1) * 128], rhs=h2T[:, kt, :], start=(kt == 0), stop=(kt == 7)), reads=[r_ws, r_h2T], writes=[r_h3])
                            sg, r_sg = sgs.next()
                            fw.op(A, lambda e, sg=sg, h1=h1: e.activation(out=sg[:], in_=h1[:], func=AF.Silu), reads=[r_h1], writes=[r_sg])
                            fw.op(V, lambda e, sg=sg, h3=h3, aT=aT, m=m: e.tensor_tensor(out=aT[:, m, :], in0=h3[:], in1=sg[:], op=ALU.mult), reads=[r_h3, r_sg], writes=[r_aT])
                        for tb in (range(4) if "sh" not in _sk else []):
                            row0 = (tg * 4 + tb) * 128
                            ysh, r_ysh = yshs.next()
                            for hh in range(2):
                                y_ps, r_yps = ps_y.next()
                                for kt in range(2):
                                    fw.op(T, lambda e, kt=kt, hh=hh, tb=tb, y_ps=y_ps, aT=aT: e.matmul(y_ps[:], lhsT=aT[:, kt, tb * 128:(tb + 1) * 128], rhs=ws2[:, kt, hh * 512:(hh + 1) * 512], start=(kt == 0), stop=(kt == 1)), reads=[r_ws, r_aT], writes=[r_yps])
                                if hh == 0:
                                    fw.op(A, lambda e, ysh=ysh, y_ps=y_ps: e.activation(out=ysh[:, 0:512], in_=y_ps[:], func=AF.Identity), reads=[r_yps], writes=[r_ysh])
                                else:
                                    fw.op(V, lambda e, ysh=ysh, y_ps=y_ps: e.tensor_copy(out=ysh[:, 512:1024], in_=y_ps[:]), reads=[r_yps], writes=[r_ysh])
                            fw.dma(P, ysh_d[row0:row0 + 128, :], ysh[:], reads=[r_ysh], writes=[Reg()])

                    for i5 in range(NTB + 2):
                        if i5 < NTB:
                            A_S1(i5)
                        if 0 <= i5 - 1 < NTB:
                            A_S1b(i5 - 1)
                        if 0 <= i5 - 2 < NTB:
                            A_S2(i5 - 2)
                            if (i5 - 2) % 4 == 3:
                                A_RT((i5 - 2) // 4)
                                A_SH((i5 - 2) // 4)
                    if "d_G" in dbg_aps and (stop or "").startswith("p5") and stop.endswith(f"_{l}"):
                        fw.dma(SY, dbg_aps["d_G"].rearrange("(a p) e -> p a e", p=128), Gd[:], reads=[r_G], writes=[reg("dbg")])
                    import os
                    if "a2" not in os.environ.get("KSKIP", ""):
                        ps_r = ps_lg
                        ps_c = ps_y
                        rank = fw.sb([128, NTB, NE], F32, "rank")
                        md = fw.sb([128, NTB, NE], F32, "md")
                        oh = fw.sb([128, NTB, NE], F32, "oh")
                        before = fw.sb([128, NE], F32, "before")
                        padf = fw.sb([128, NE], F32, "padf")
                        padi = fw.sb([128, NE], I32, "padi")
                        pend = fw.sb([128, NE], F32, "pend")
                        pst1 = fw.sb([128, NE], F32, "pst1")
                        ones32 = fw.sb([128, NE], F32, "ones32")
                        dmax = fw.sb([128, NTB], F32, "dmax")
                        dsum = fw.sb([128, NTB], F32, "dsum")
                        gsum = fw.sb([128, NTB], F32, "gsum")
                        tmpb = fw.sb([128, NTB], F32, "tmpb")
                        cmpt = fw.sb([128, max(NOV, 1), NE], F32, "cmpt")
                        bef = fw.sb([128, max(NOV, 1)], F32, "bef")
                        emp = fw.sb([128, max(NOV, 1)], F32, "emp")
                        e384 = fw.sb([128, NE], F32, "e384")
                        ovb = fw.sb([128, NE], F32, "ovb")
                        Av = fw.sb([128, NTB, NE], F32, "Av")
                        icv = fw.sb([128, NTB, NE], F32, "icv")
                        r_a2 = Reg()
                        a2 = lambda f, eng=V: fw.op(eng, f, reads=[r_a2, r_G, r_c], writes=[r_a2])
                        a2(lambda e: e.tensor_copy(out=Mb[:], in_=Mk[:]))
                        a2(lambda e: e.memset(before[:], 0.0))
                        a2(lambda e: e.memset(ones32[:], 1.0))
                        for b in range(NTB):
                            pr, r_pr = ps_r.next()
                            pc, r_pc = ps_c.next()
                            fw.op(T, lambda e, b=b, pr=pr: e.matmul(pr[:, 0, :], lhsT=trisb[:], rhs=Mb[:, b, :], start=True, stop=True), reads=[r_a2, r_c], writes=[r_pr])
                            fw.op(T, lambda e, b=b, pc=pc: e.matmul(pc[:, 0:NE], lhsT=onesb[:], rhs=Mb[:, b, :], start=True, stop=True), reads=[r_a2, rc_const], writes=[r_pc])
                            fw.op(V, lambda e, b=b, pr=pr: e.tensor_tensor(out=rank[:, b, :], in0=pr[:, 0, :], in1=before[:], op=ALU.add), reads=[r_pr, r_a2], writes=[r_a2])
                            fw.op(V, lambda e, pc=pc: e.tensor_tensor(out=before[:], in0=pc[:, 0:NE], in1=before[:], op=ALU.add), reads=[r_pc, r_a2], writes=[r_a2])
                        a2(lambda e: e.tensor_scalar(out=padf[:], in0=before[:], scalar1=-float(CAP), scalar2=0.0, op0=ALU.add, op1=ALU.max))
                        a2(lambda e: e.tensor_scalar(out=padf[:], in0=padf[:], scalar1=127.0, scalar2=None, op0=ALU.add))
                        a2(lambda e: e.tensor_copy(out=padi[:], in_=padf[:]))
                        a2(lambda e: e.tensor_single_scalar(out=padi[:], in_=padi[:], scalar=7, op=ALU.arith_shift_right))
                        a2(lambda e: e.tensor_single_scalar(out=padi[:], in_=padi[:], scalar=7, op=ALU.logical_shift_left))
                        a2(lambda e: e.tensor_copy(out=padf[:], in_=padi[:]))
                        a2(lambda e: e.tensor_tensor_scan(out=pend[:], data0=ones32[:], data1=padf[:], initial=0.0, op0=ALU.mult, op1=ALU.add))
                        a2(lambda e: e.tensor_tensor(out=pst1[:], in0=pend[:], in1=padf[:], op=ALU.subtract))
                        a2(lambda e: e.tensor_scalar(out=ovb[:], in0=pst1[:], scalar1=float(NSB * 128 - CAP + 1), scalar2=None, op0=ALU.add))
                        a2(lambda e: e.tensor_scalar(out=e384[:], in0=jv0[:, 0:NE], scalar1=float(CAP), scalar2=1.0, op0=ALU.mult, op1=ALU.add))
                        bexp = lambda a: a.unsqueeze(1).to_broadcast([128, NTB, NE])
                        a2(lambda e: e.tensor_tensor(out=Av[:], in0=rank[:], in1=bexp(e384[:]), op=ALU.add))
                        a2(lambda e: e.tensor_tensor(out=md[:], in0=rank[:], in1=bexp(ovb[:]), op=ALU.add))
                        a2(lambda e: e.tensor_scalar(out=icv[:], in0=rank[:], scalar1=float(CAP), scalar2=None, op0=ALU.is_lt))
                        a2(lambda e: e.tensor_tensor(out=Av[:], in0=Av[:], in1=md[:], op=ALU.subtract))
                        a2(lambda e: e.tensor_tensor(out=Av[:], in0=Av[:], in1=icv[:], op=ALU.mult))
                        a2(lambda e: e.tensor_tensor(out=md[:], in0=md[:], in1=Av[:], op=ALU.add))
                        a2(lambda e: e.tensor_tensor(out=md[:], in0=md[:], in1=Mk[:], op=ALU.mult))
                        a2(lambda e: e.tensor_reduce(out=dmax[:], in_=md[:], axis=AX.X, op=ALU.max))
                        a2(lambda e: e.tensor_reduce(out=dsum[:], in_=md[:], axis=AX.X, op=ALU.add))
                        a2(lambda e: e.tensor_reduce(out=gsum[:], in_=Gd[:], axis=AX.X, op=ALU.add))
                        a2(lambda e: e.tensor_tensor(out=oh[:], in0=md[:], in1=dmax[:].unsqueeze(2).to_broadcast([128, NTB, NE]), op=ALU.is_equal))
                        a2(lambda e: e.tensor_tensor(out=oh[:], in0=oh[:], in1=Gd[:], op=ALU.mult))
                        a2(lambda e: e.tensor_reduce(out=w2t[:, :, 0], in_=oh[:], axis=AX.X, op=ALU.add))
                        a2(lambda e: e.tensor_tensor(out=w2t[:, :, 1], in0=gsum[:], in1=w2t[:, :, 0], op=ALU.subtract))
                        a2(lambda e: e.tensor_scalar(out=tmpb[:], in0=dmax[:], scalar1=-1.0, scalar2=None, op0=ALU.add))
                        a2(lambda e: e.tensor_copy(out=idx2[:, :, 0], in_=tmpb[:]))
                        a2(lambda e: e.tensor_tensor(out=tmpb[:], in0=dsum[:], in1=dmax[:], op=ALU.subtract))
                        a2(lambda e: e.tensor_scalar(out=tmpb[:], in0=tmpb[:], scalar1=-1.0, scalar2=None, op0=ALU.add))
                        a2(lambda e: e.tensor_copy(out=idx2[:, :, 1], in_=tmpb[:]))
                        NOV1 = max(NOV, 1)
                        a2(lambda e: e.tensor_tensor(out=cmpt[:], in0=pend[:].unsqueeze(1).to_broadcast([128, NOV1, NE]), in1=jv[:].unsqueeze(2).to_broadcast([128, NOV1, NE]), op=ALU.is_le))
                        a2(lambda e: e.tensor_reduce(out=bef[:], in_=cmpt[:], axis=AX.X, op=ALU.add))
                        a2(lambda e: e.tensor_scalar(out=emp[:], in0=jv[:], scalar1=pend[:, NE - 1:NE], scalar2=float(1 << 22), op0=ALU.is_ge, op1=ALU.mult))
                        a2(lambda e: e.tensor_scalar(out=bef[:], in0=bef[:], scalar1=float(NE - 1), scalar2=128.0, op0=ALU.min, op1=ALU.mult))
                        a2(lambda e: e.tensor_scalar(out=bef[:], in0=bef[:], scalar1=iotp[:, 0:1], scalar2=float(l * NE * 128), op0=ALU.add, op1=ALU.add))
                        a2(lambda e: e.tensor_tensor(out=bef[:], in0=bef[:], in1=emp[:], op=ALU.add))
                        fw.op(V, lambda e: e.tensor_copy(out=idxw[:], in_=bef[:]), reads=[r_a2], writes=[r_idx])
                        fw.op(V, lambda e: e.tensor_copy(out=w2t[:, 0:1, 0:1], in_=w2t[:, 0:1, 0:1]), reads=[r_a2], writes=[r_idx])
                        if "d_idx" in dbg_aps and (stop or "").startswith("p5") and stop.endswith(f"_{l}"):
                            fw.dma(SY, dbg_aps["d_idx"].rearrange("(a p) k -> p a k", p=128), idx2[:], reads=[r_idx], writes=[reg("dbg")])
                            fw.dma(SY, dbg_aps["d_idxw"][:, :], idxw[:], reads=[r_idx], writes=[reg("dbg")])
                p5_stages = {"a": 0, "s": 1, "b": 2}.get(stop[2] if (stop or "").startswith("p5") and len(stop) > 3 and stop[2] in "asb" else "", 3)
                with contextlib.ExitStack() as esS:
                  if p5_stages >= 1:
                        fw.es = esS
                        fw.barrier()
                        rbs = Rot([fw.sb([128, RW], BF16, "rbs") for _ in range(4)])
                        for b in range(NTB):
                            for k2 in range(2):
                                rb, r_rb = rbs.next()
                                fw.dma(SY, rb[:], h2b_d[b * 128:(b + 1) * 128, :], writes=[r_rb])
                                fw.op(V, lambda e, rb=rb, b=b, k2=k2: e.tensor_copy(out=rb[:, D:D + 2].bitcast(F32), in_=w2t[:, b, k2:k2 + 1]), reads=[r_rb, r_idx], writes=[r_rb])
                                idma(xs_d[:, :], idx2[:, b, k2:k2 + 1], rb[:, :], None, reads=[r_rb, r_idx], writes=[Reg()])
                with contextlib.ExitStack() as esB:
                  if p5_stages >= 2:
                        fw.es = esB
                        fw.barrier()
                        ps_tp = Rot([fw.ps(esB, "p5btp", [128, 8, 128], BF16) for i in range(2)])
                        ps_h13 = Rot([fw.ps(esB, "p5bh", [128, 512], F32) for i in range(2)])
                        ps_at = Rot([fw.ps(esB, "p5bat", [128, 2, 128], BF16) for i in range(2)])
                        ps_y = Rot([fw.ps(esB, "p5by", [128, 512], F32) for i in range(2)])
                        NW = 4
                        w1bs = Rot([fw.sb([128, 8, DE], BF16, "w1b") for _ in range(NW)])
                        w3bs = Rot([fw.sb([128, 8, DE], BF16, "w3b") for _ in range(NW)])
                        w2bs = Rot([fw.sb([128, 2, D], BF16, "w2b") for _ in range(NW)])
                        xsbs = Rot([fw.sb([128, RW], BF16, "xsb") for _ in range(4)])
                        xsTs = Rot([fw.sb([128, 8, 128], BF16, "xsT") for _ in range(3)])
                        sgs = Rot([fw.sb([128, DE], F32, "sgb") for _ in range(2)])
                        abs_ = Rot([fw.sb([128, DE], BF16, "ab") for _ in range(3)])
                        aTs = Rot([fw.sb([128, 2, 128], BF16, "aTb") for _ in range(3)])
                        ysbs = Rot([fw.sb([128, D], BF16, "ysb") for _ in range(3)])
                        w1v = exp_w1p.rearrange("l e p n -> (l e p) n")
                        w3v = exp_w3p.rearrange("l e p n -> (l e p) n")
                        w2v = exp_w2p.rearrange("l e p n -> (l e p) n")
                        WMAX = fw.E[P].h.alloc_register()
                        fw.E[P].h.reg_mov(WMAX, L * NE * 128 - 1)
                        sched = []
                        for ex in range(NE):
                            for k3 in range(CAPB):
                                sched.append(dict(j=ex * CAPB + k3, ex=ex, first=(k3 == 0), ov=None))
                        for jo in range(NOV):
                            sched.append(dict(j=NSB + jo, ex=None, first=True, ov=jo))
                        cur_w = [None]

                        def S1(b):
                            if b["first"]:
                                (w1b, rw1), (w3b, rw3), (w2b, rw2) = w1bs.next(), w3bs.next(), w2bs.next()
                                f1 = lambda t: t[:].rearrange("p a b -> p (a b)")
                                if b["ov"] is None:
                                    ex = b["ex"]
                                    fw.dma(P, f1(w1b), exp_w1p[l, ex], writes=[rw1])
                                    fw.dma(P, f1(w3b), exp_w3p[l, ex], writes=[rw3])
                                    fw.dma(P, f1(w2b), exp_w2p[l, ex], writes=[rw2])
                                else:
                                    jo = b["ov"]
                                    idma(f1(w1b), None, w1v, idxw[:, jo:jo + 1], reads=[r_idx], writes=[rw1], bounds_check=WMAX, oob_is_err=False)
                                    idma(f1(w3b), None, w3v, idxw[:, jo:jo + 1], reads=[r_idx], writes=[rw3], bounds_check=WMAX, oob_is_err=False)
                                    idma(f1(w2b), None, w2v, idxw[:, jo:jo + 1], reads=[r_idx], writes=[rw2], bounds_check=WMAX, oob_is_err=False)
                                cur_w[0] = (w1b, rw1, w3b, rw3, w2b, rw2)
                            b["w"] = cur_w[0]
                            j = b["j"]
                            xsb, r_xsb = xsbs.next()
                            fw.dma(SY, xsb[:], xs_d[j * 128:(j + 1) * 128, :], writes=[r_xsb])
                            tp, r_tp = ps_tp.next()
                            for kt in range(8):
                                fw.op(T, lambda e, kt=kt: e.transpose(out=tp[:, kt, :], in_=xsb[:, kt * 128:(kt + 1) * 128], identity=identb[:]), reads=[r_xsb, rc_const], writes=[r_tp])
                            xsT, r_xsT = xsTs.next()
                            fw.op(A, lambda e: e.activation(out=xsT[:, 0:4, :], in_=tp[:, 0:4, :], func=AF.Identity), reads=[r_tp], writes=[r_xsT])
                            fw.op(V, lambda e: e.tensor_copy(out=xsT[:, 4:8, :], in_=tp[:, 4:8, :]), reads=[r_tp], writes=[r_xsT])
                            b.update(xsb=xsb, r_xsb=r_xsb, xsT=xsT, r_xsT=r_xsT)

                        def S2(b):
                            w1b, rw1, w3b, rw3, w2b, rw2 = b["w"]
                            xsT, r_xsT, xsb, r_xsb = b["xsT"], b["r_xsT"], b["xsb"], b["r_xsb"]
                            h13, r_h13 = ps_h13.next()
                            for kt in range(8):
                                fw.op(T, lambda e, kt=kt: e.matmul(h13[:, 0:DE], lhsT=xsT[:, kt, :], rhs=w1b[:, kt, :], start=(kt == 0), stop=(kt == 7), skip_group_check=True), reads=[r_xsT, rw1], writes=[r_h13])
                            for kt in range(8):
                                fw.op(T, lambda e, kt=kt: e.matmul(h13[:, DE:2 * DE], lhsT=xsT[:, kt, :], rhs=w3b[:, kt, :], start=False, stop=(kt == 7), skip_group_check=True), reads=[r_xsT, rw3], writes=[r_h13])
                            sg, r_sg = sgs.next()
                            fw.op(A, lambda e: e.activation(out=sg[:], in_=h13[:, 0:DE], func=AF.Silu), reads=[r_h13], writes=[r_sg])
                            ab, r_ab = abs_.next()
                            fw.op(V, lambda e: e.scalar_tensor_tensor(out=ab[:], in0=h13[:, DE:2 * DE], scalar=xsb[:, D:D + 2].bitcast(F32), in1=sg[:], op0=ALU.mult, op1=ALU.mult), reads=[r_h13, r_xsb, r_sg], writes=[r_ab])
                            b.update(ab=ab, r_ab=r_ab)

                        def S3(b):
                            ab, r_ab = b["ab"], b["r_ab"]
                            at, r_at = ps_at.next()
                            for kt in range(2):
                                fw.op(T, lambda e, kt=kt: e.transpose(out=at[:, kt, :], in_=ab[:, kt * 128:(kt + 1) * 128], identity=identb[:]), reads=[r_ab, rc_const], writes=[r_at])
                            aT, r_aT = aTs.next()
                            fw.op(V, lambda e: e.tensor_copy(out=aT[:], in_=at[:]), reads=[r_at], writes=[r_aT])
                            b.update(aT=aT, r_aT=r_aT)

                        def S4(b):
                            w1b, rw1, w3b, rw3, w2b, rw2 = b["w"]
                            aT, r_aT = b["aT"], b["r_aT"]
                            j = b["j"]
                            ysb, r_ysb = ysbs.next()
                            for hh in range(2):
                                y_ps, r_yps = ps_y.next()
                                for kt in range(2):
                                    fw.op(T, lambda e, kt=kt, hh=hh, y_ps=y_ps: e.matmul(y_ps[:], lhsT=aT[:, kt, :], rhs=w2b[:, kt, hh * 512:(hh + 1) * 512], start=(kt == 0), stop=(kt == 1)), reads=[r_aT, rw2], writes=[r_yps])
                                if hh == 0:
                                    fw.op(A, lambda e, y_ps=y_ps: e.activation(out=ysb[:, 0:512], in_=y_ps[:], func=AF.Identity), reads=[r_yps], writes=[r_ysb])
                                else:
                                    fw.op(V, lambda e, y_ps=y_ps: e.tensor_copy(out=ysb[:, 512:1024], in_=y_ps[:]), reads=[r_yps], writes=[r_ysb])
                            fw.dma(A, ys_d[j * 128:(j + 1) * 128, :], ysb[:], reads=[r_ysb], writes=[Reg()])
                            b.clear()

                        nb = len(sched)
                        for i in range(nb + 3):
                            if i < nb:
                                S1(sched[i])
                            if 0 <= i - 1 < nb:
                                S2(sched[i - 1])
                            if 0 <= i - 2 < nb:
                                S3(sched[i - 2])
                            if 0 <= i - 3 < nb:
                                S4(sched[i - 3])
                        fw.E[P].h.free_register(WMAX)
                with contextlib.ExitStack() as esC:
                  if p5_stages >= 3:
                        fw.es = esC
                        fw.barrier()
                        xts = Rot([fw.sb([128, D], F32, "xt5c") for _ in range(2)])
                        g1s = Rot([fw.sb([128, D], BF16, "g1c") for _ in range(3)])
                        g2s = Rot([fw.sb([128, D], BF16, "g2c") for _ in range(3)])
                        accs = Rot([fw.sb([128, D], F32, "acc5c") for _ in range(2)])
                        yss = Rot([fw.sb([128, D], F32, "ysc") for _ in range(2)])
                        xos = Rot([fw.sb([128, D], F32, "xo5") for _ in range(2)])
                        for b in range(NTB):
                            row0 = b * 128
                            xt, r_xt = xts.next()
                            ga, r_ga = g1s.next()
                            gb_, r_gb = g2s.next()
                            ys_, r_ys = yss.next()
                            fw.dma(SY, xt[:], x1_d[row0:row0 + 128, :], writes=[r_xt])
                            fw.dma(SY, ys_[:], ysh_d[row0:row0 + 128, :], writes=[r_ys])
                            idma(ga[:, :], None, ys_d[:, :], idx2[:, b, 0:1], reads=[r_idx], writes=[r_ga])
                            idma(gb_[:, :], None, ys_d[:, :], idx2[:, b, 1:2], reads=[r_idx], writes=[r_gb])
                            acc, r_acc = accs.next()
                            fw.op(V, lambda e, acc=acc, ga=ga, ys_=ys_: e.tensor_tensor(out=acc[:], in0=ga[:], in1=ys_[:], op=ALU.add), reads=[r_ga, r_ys], writes=[r_acc])
                            fw.op(V, lambda e, acc=acc, gb_=gb_: e.tensor_tensor(out=acc[:], in0=acc[:], in1=gb_[:], op=ALU.add), reads=[r_acc, r_gb], writes=[r_acc])
                            fw.op(V, lambda e, acc=acc: e.tensor_tensor(out=acc[:], in0=acc[:], in1=g2b[:], op=ALU.mult), reads=[r_acc, r_c], writes=[r_acc])
                            fw.op(V, lambda e, acc=acc, xt=xt: e.scalar_tensor_tensor(out=acc[:], in0=xt[:], scalar=ALPHA, in1=acc[:], op0=ALU.mult, op1=ALU.add), reads=[r_xt, r_acc], writes=[r_acc])
                            xo, r_xo = xos.next()
                            final_ln(acc, r_acc, stats, mv, rstd, r_st, epst, r_c, lng, lnb, xo, r_xo, eng2=V)
                            fw.dma(A, x_dst[row0:row0 + 128, :], xo[:], reads=[r_xo], writes=[Reg()])
                fw.es = es
            fw.es = es0

        r_xin = Reg()
        for l in range(L):
            x_src, r_xsrc = (x_in, r_xin) if l == 0 else (x2_d, reg("x2"))
            p1(l, x_src, r_xsrc)
            if stop == f"p1_{l}":
                fw.barrier()
                for nm, src in (("hT", hT_d), ("zc", zc_d), ("u", u_d), ("cq", cq_d), ("ckv", ckv_d), ("kr", kr_d)):
                    if "d_" + nm in dbg_aps:
                        fw.dma(SY, dbg_aps["d_" + nm][:, :], src[:, :], reads=[reg(nm)], writes=[reg("dbg")])
                fw.finish(list(R.values()))
                return nc

            p2(l)
            if stop == f"p2_{l}":
                fw.barrier()
                if "d_zs" in dbg_aps:
                    fw.dma(SY, dbg_aps["d_zs"][:, :], zs_d[:, :], reads=[reg("zs")], writes=[reg("dbg")])
                fw.finish(list(R.values()))
                return nc
            p3(l)
            if stop == f"p3_{l}":
                fw.barrier()
                if "d_oT" in dbg_aps:
                    fw.dma(SY, dbg_aps["d_oT"][:, :], oT_d[:, :], reads=[reg("oT")], writes=[reg("dbg")])
                fw.finish(list(R.values()))
                return nc
            p4(l, x_src, r_xsrc)
            if stop == f"p4_{l}":
                fw.barrier()
                if "d_x1" in dbg_aps:
                    fw.dma(SY, dbg_aps["d_x1"][:, :], x1_d[:, :], reads=[reg("x1")], writes=[reg("dbg")])
                fw.finish(list(R.values()))
                return nc
            last = (l == L - 1)
            p5(l, out_d if last else x2_d, reg("out") if last else reg("x2"))
            fw.cut_on = False
            if stop in (f"p5_{l}", f"p5a_{l}", f"p5s_{l}", f"p5b_{l}"):
                fw.barrier()
                if "d_x2" in dbg_aps:
                    fw.dma(SY, dbg_aps["d_x2"][:, :], (out_d if last else x2_d)[:, :], reads=[reg("out") if last else reg("x2")], writes=[reg("dbg")])
                fw.finish(list(R.values()))
                return nc
        fw.finish(list(R.values()))
    return nc


def host_consts():
    tri = np.triu(np.ones((128, 128), np.float32))
    inv_freq = (10000.0 ** (-np.arange(0, ROPE, 2, dtype=np.float32) / ROPE)).astype(np.float32)
    ropec = np.zeros((32, 2), np.float32)
    ropec[:, 0] = np.concatenate([inv_freq, inv_freq])
    ropec[:16, 1] = -1.0
    ropec[16:, 1] = 1.0
    return {
        "ident_bf": np.eye(128, dtype=np.float32).astype(ml_dtypes.bfloat16),
        "ident_f": np.eye(128, dtype=np.float32),
        "ones_bf": np.ones((128, 128), np.float32).astype(ml_dtypes.bfloat16),
        "tri_bf": tri.astype(ml_dtypes.bfloat16),
        "ropec": ropec,
        "iota_f": np.tile(np.arange(512, dtype=np.float32)[None, :], (128, 1)),
        "iota_p": np.arange(128, dtype=np.float32).reshape(128, 1),
        "tris_bf": np.triu(np.ones((128, 128), np.float32), 1).astype(ml_dtypes.bfloat16),
    }


def prep_inputs(inp, S):
    f = lambda a: np.ascontiguousarray(np.asarray(a))
    w_in = f(inp["w_in"])
    kr0 = IN_MIX - ROPE
    w_in_a = np.concatenate([w_in[:, :, :IN_MIX], w_in[:, :, kr0 + 16:kr0 + 32], w_in[:, :, kr0:kr0 + 16]], axis=2)
    w_in_g = w_in[:, :, IN_MIX:]
    w_uq = f(inp["w_uq"])
    Lw = w_uq.shape[0]
    wq4 = w_uq.reshape(Lw, QL, H, DK)
    wsw = np.zeros_like(wq4)
    wsw[..., NOPE:NOPE + 16] = wq4[..., NOPE + 16:NOPE + 32]
    wsw[..., NOPE + 16:NOPE + 32] = wq4[..., NOPE:NOPE + 16]
    shared = {
        "w_in_a": f(w_in_a), "w_in_g": f(w_in_g), "w_uq_sw": f(wsw.reshape(Lw, QL, H * DK)),
        "router_bias": f(inp["router_bias"]).reshape(1, NE),
    }
    for k in ("w_ada", "b_ada", "conv_w", "ssm_a_re", "ssm_a_im", "ssm_b_re", "ssm_b_im", "ssm_c_re", "ssm_c_im", "ssm_d",
              "ssm_log_dt", "ssm_w_glu", "q_norm", "w_uq", "kv_norm", "w_uk", "w_uv", "w_up_conv", "w_up_ssm", "w_up_attn",
              "w_o", "ln_g", "ln_b", "router_w", "shared_w1", "shared_w3", "shared_w2"):
        shared[k] = f(inp[k])
    Le = np.asarray(inp["exp_w1"]).shape[0]
    shared["exp_w1p"] = f(np.asarray(inp["exp_w1"]).reshape(Le, NE, 8, 128, DE).transpose(0, 1, 3, 2, 4).reshape(Le, NE, 128, 8 * DE))
    shared["exp_w3p"] = f(np.asarray(inp["exp_w3"]).reshape(Le, NE, 8, 128, DE).transpose(0, 1, 3, 2, 4).reshape(Le, NE, 128, 8 * DE))
    shared["exp_w2p"] = f(np.asarray(inp["exp_w2"]).reshape(Le, NE, 2, 128, D).transpose(0, 1, 3, 2, 4).reshape(Le, NE, 128, 2 * D))
    shared.update(host_consts())
    x = f(inp["x"])
    c = f(inp["c"])
    pos = f(inp["positions"]).astype(np.int32)
    B = x.shape[0]
    maps = []
    for b in range(B):
        m = dict(shared)
        m["x"] = f(x[b, :S])
        m["c"] = f(c[b:b + 1])
        m["positions"] = f(pos[b:b + 1, :S])
        maps.append(m)
    return maps


def kernel(**inputs):
    S = inputs["x"].shape[1]
    nc = build_program(S)
    maps = prep_inputs(inputs, S)
    res = run_bass_kernel_spmd(nc, maps, core_ids=list(range(len(maps))))
    return np.stack([np.asarray(r["out"]) for r in res.results], axis=0).astype(np.float32)
```
